# Optimizing a Trainium2 kernel written in Bass

```python
import jax, jax.numpy as jnp
from jax import lax
import numpy as np

D_MODEL = 2048
BATCH = 16
SEQ = 2048
DEPTH = 4

MLSTM_HEADS = 4
MLSTM_HEAD_DIM = 256
FOX_HEADS = 8
FOX_HEAD_DIM = 128
RET_HEADS = 4
RET_HEAD_DIM = 256
MLSTM_WIDTH = MLSTM_HEADS * MLSTM_HEAD_DIM
FOX_WIDTH = FOX_HEADS * FOX_HEAD_DIM
RET_WIDTH = RET_HEADS * RET_HEAD_DIM
CONV_WIDTH = 4
CHUNK = 128
Q_BLOCK = 128
ROPE_BASE = 10000.0
NORM_EPS = 1e-6
D_FF = ((8 * D_MODEL // 3 + 255) // 256) * 256
N_MOD = 9
HALF_STEP = 0.5
IN_WIDTHS = (
    2 * MLSTM_WIDTH,
    MLSTM_WIDTH,
    MLSTM_WIDTH,
    MLSTM_HEADS,
    MLSTM_HEADS,
    FOX_WIDTH, FOX_WIDTH, FOX_WIDTH,
    FOX_HEADS,
    RET_WIDTH, RET_WIDTH, RET_WIDTH, RET_WIDTH,
    D_MODEL, D_MODEL, D_MODEL,
)
IN_COLS = sum(IN_WIDTHS)

kernel_name = 'hybrid_mlstm_fox_retention_macaron_block'


def _col_starts():
    return [int(s) for s in np.cumsum((0,) + IN_WIDTHS[:-1])]


def rmsnorm(x, w):
    xf = x.astype(jnp.float32)
    y = xf * lax.rsqrt(jnp.mean(xf * xf, axis=-1, keepdims=True) + NORM_EPS)
    return (y * w.astype(jnp.float32)).astype(x.dtype)


def headnorm(h):
    mu = jnp.mean(h, axis=-1, keepdims=True)
    var = jnp.mean(jnp.square(h - mu), axis=-1, keepdims=True)
    return (h - mu) * lax.rsqrt(var + NORM_EPS)


def swiglu(h, w_gate, w_up, w_down):
    return (jax.nn.silu(h @ w_gate) * (h @ w_up)) @ w_down


def split_heads(t, n_heads):
    b, s, _ = t.shape
    return t.reshape(b, s, n_heads, -1).transpose(0, 2, 1, 3)


def merge_heads(t):
    b, h, s, d = t.shape
    return t.transpose(0, 2, 1, 3).reshape(b, s, h * d)


def to_chunks(t):
    b, h, s = t.shape[:3]
    t = t.reshape((b, h, s // CHUNK, CHUNK) + t.shape[3:])
    return jnp.moveaxis(t, 2, 0)


def from_chunks(t):
    t = jnp.moveaxis(t, 0, 2)
    b, h, nc, l, d = t.shape
    return t.reshape(b, h, nc * l, d)


def causal_depthwise_conv(x, w, b):
    k = w.shape[0]
    y = lax.conv_general_dilated(x, w[:, None, :], window_strides=(1,), padding=[(k - 1, 0)],
                                 dimension_numbers=('NWC', 'WIO', 'NWC'),
                                 feature_group_count=x.shape[-1])
    return y + b


def rotary(t):
    s, d = t.shape[2], t.shape[3]
    half = d // 2
    inv_freq = ROPE_BASE ** (-jnp.arange(half, dtype=jnp.float32) / half)
    ang = jnp.arange(s, dtype=jnp.float32)[:, None] * inv_freq[None, :]
    cos, sin = jnp.cos(ang), jnp.sin(ang)
    t1, t2 = t[..., :half], t[..., half:]
    return jnp.concatenate([t1 * cos - t2 * sin, t1 * sin + t2 * cos], axis=-1)


def mlstm_chunkwise(q, k, v, ig, lf):
    b, h, s, dk = q.shape
    dv = v.shape[-1]
    k = k * dk ** -0.5
    tril = jnp.tril(jnp.ones((CHUNK, CHUNK), dtype=bool))

    def step(carry, xs):
        c_state, n_state, m_state = carry
        qc, kc, vc, igc, lfc = xs
        bcum = jnp.cumsum(lfc, axis=-1)
        logd = jnp.where(tril, bcum[..., :, None] - bcum[..., None, :] + igc[..., None, :], -jnp.inf)
        inter = bcum + m_state[..., None]
        m_t = jnp.maximum(inter, jnp.max(logd, axis=-1))
        w = jnp.einsum('bhld,bhsd->bhls', qc, kc) * jnp.exp(logd - m_t[..., None])
        a = jnp.exp(inter - m_t)
        num = jnp.einsum('bhls,bhse->bhle', w, vc) + a[..., None] * jnp.einsum('bhld,bhde->bhle', qc, c_state)
        den = jnp.sum(w, axis=-1) + a * jnp.einsum('bhld,bhd->bhl', qc, n_state)
        h_out = num / jnp.maximum(jnp.abs(den), jnp.exp(-m_t))[..., None]
        g = bcum[..., -1]
        logw = g[..., None] - bcum + igc
        m_new = jnp.maximum(g + m_state, jnp.max(logw, axis=-1))
        kw = kc * jnp.exp(logw - m_new[..., None])[..., None]
        decay = jnp.exp(g + m_state - m_new)
        c_state = decay[..., None, None] * c_state + jnp.einsum('bhsd,bhse->bhde', kw, vc)
        n_state = decay[..., None] * n_state + jnp.sum(kw, axis=2)
        return (c_state, n_state, m_new), h_out

    init = (jnp.zeros((b, h, dk, dv), jnp.float32), jnp.zeros((b, h, dk), jnp.float32),
            jnp.zeros((b, h), jnp.float32))
    _, hs = lax.scan(step, init, (to_chunks(q), to_chunks(k), to_chunks(v), to_chunks(ig), to_chunks(lf)))
    return from_chunks(hs)


def forgetting_attention(q, k, v, lf):
    s, d = q.shape[2], q.shape[3]
    fcum = jnp.cumsum(lf, axis=-1)
    scale = d ** -0.5
    outs = []
    for blk in range(s // Q_BLOCK):
        lo, hi = blk * Q_BLOCK, (blk + 1) * Q_BLOCK
        logits = jnp.einsum('bhqd,bhkd->bhqk', q[:, :, lo:hi], k[:, :, :hi]).astype(jnp.float32) * scale
        logits = logits + fcum[:, :, lo:hi, None] - fcum[:, :, None, :hi]
        causal = (lo + jnp.arange(Q_BLOCK))[:, None] >= jnp.arange(hi)[None, :]
        p = jax.nn.softmax(jnp.where(causal, logits, -jnp.inf), axis=-1)
        outs.append(jnp.einsum('bhqk,bhkd->bhqd', p.astype(v.dtype), v[:, :, :hi]))
    return jnp.concatenate(outs, axis=2)


def retention_chunkwise(q, k, v):
    b, h, s, dk = q.shape
    dv = v.shape[-1]
    k = k * dk ** -0.5
    log_gamma = jnp.log1p(-(2.0 ** (-5.0 - jnp.arange(h, dtype=jnp.float32))))
    idx = jnp.arange(CHUNK, dtype=jnp.float32)
    rel = idx[:, None] - idx[None, :]
    intra = jnp.where(rel >= 0, jnp.exp(jnp.maximum(rel, 0.0)[None] * log_gamma[:, None, None]), 0.0)
    q_decay = jnp.exp((idx[None, :] + 1.0) * log_gamma[:, None])
    k_decay = jnp.exp((CHUNK - 1.0 - idx[None, :]) * log_gamma[:, None])
    chunk_decay = jnp.exp(CHUNK * log_gamma)

    def step(r_state, xs):
        qc, kc, vc = xs
        scores = jnp.einsum('bhld,bhsd->bhls', qc, kc) * intra
        out = jnp.einsum('bhls,bhse->bhle', scores, vc) + \
            jnp.einsum('bhld,bhde->bhle', qc * q_decay[..., None], r_state)
        r_state = chunk_decay[:, None, None] * r_state + \
            jnp.einsum('bhsd,bhse->bhde', kc * k_decay[..., None], vc)
        return r_state, out

    _, outs = lax.scan(step, jnp.zeros((b, h, dk, dv), jnp.float32), (to_chunks(q), to_chunks(k), to_chunks(v)))
    return from_chunks(outs)


def hybrid_mixer(u, w_in, b_in, conv_w, conv_b, mlstm_norm_w, ret_norm_w, w_bm, w_bf, w_br, w_out):
    dt = u.dtype
    f32 = jnp.float32
    z = u @ w_in + b_in
    (m_qk, m_v, m_o, m_i, m_f, f_q, f_k, f_v, f_f,
     r_q, r_k, r_v, r_g, g_m, g_f, g_r) = jnp.split(z, _col_starts()[1:], axis=-1)

    m_qk = jax.nn.silu(causal_depthwise_conv(m_qk, conv_w, conv_b))
    m_q, m_k = jnp.split(m_qk, 2, axis=-1)
    ig = m_i.astype(f32).transpose(0, 2, 1)
    lf_m = jax.nn.log_sigmoid(m_f.astype(f32)).transpose(0, 2, 1)
    h_m = mlstm_chunkwise(split_heads(m_q, MLSTM_HEADS).astype(f32), split_heads(m_k, MLSTM_HEADS).astype(f32),
                          split_heads(m_v, MLSTM_HEADS).astype(f32), ig, lf_m)
    y_m = (jax.nn.sigmoid(m_o.astype(f32)) * merge_heads(headnorm(h_m)) * mlstm_norm_w.astype(f32)).astype(dt)

    lf_f = jax.nn.log_sigmoid(f_f.astype(f32)).transpose(0, 2, 1)
    y_f = merge_heads(forgetting_attention(split_heads(f_q, FOX_HEADS), split_heads(f_k, FOX_HEADS),
                                           split_heads(f_v, FOX_HEADS), lf_f)).astype(dt)

    h_r = retention_chunkwise(rotary(split_heads(r_q, RET_HEADS).astype(f32)),
                              rotary(split_heads(r_k, RET_HEADS).astype(f32)),
                              split_heads(r_v, RET_HEADS).astype(f32))
    y_r = (jax.nn.silu(r_g.astype(f32)) * merge_heads(headnorm(h_r)) * ret_norm_w.astype(f32)).astype(dt)

    merged = jax.nn.sigmoid(g_m) * (y_m @ w_bm) + jax.nn.sigmoid(g_f) * (y_f @ w_bf) + \
        jax.nn.sigmoid(g_r) * (y_r @ w_br)
    return merged @ w_out


def setup_inputs(seed: int = 0) -> dict:
    key = jax.random.key(seed)
    ks = jax.random.split(key, 24)
    nrm = jax.random.normal
    f32 = jnp.float32
    starts = _col_starts()
    b_in = 0.02 * nrm(ks[9], (DEPTH, IN_COLS), f32)
    b_in = b_in.at[:, starts[4]:starts[4] + MLSTM_HEADS].add(jnp.linspace(3.0, 6.0, MLSTM_HEADS))
    b_in = b_in.at[:, starts[8]:starts[8] + FOX_HEADS].add(jnp.linspace(2.0, 6.0, FOX_HEADS))
    return {
        'x': nrm(ks[0], (BATCH, SEQ, D_MODEL), f32),
        'c': nrm(ks[1], (BATCH, D_MODEL), f32),
        'w_ada': nrm(ks[2], (DEPTH, D_MODEL, N_MOD * D_MODEL), f32) * D_MODEL ** -0.5,
        'b_ada': 0.02 * nrm(ks[3], (DEPTH, N_MOD * D_MODEL), f32),
        'norm_w': 1.0 + 0.02 * nrm(ks[4], (DEPTH, 6, D_MODEL), f32),
        'ffn1_gate': nrm(ks[5], (DEPTH, D_MODEL, D_FF), f32) * D_MODEL ** -0.5,
        'ffn1_up': nrm(ks[6], (DEPTH, D_MODEL, D_FF), f32) * D_MODEL ** -0.5,
        'ffn1_down': nrm(ks[7], (DEPTH, D_FF, D_MODEL), f32) * D_FF ** -0.5,
        'w_in': nrm(ks[8], (DEPTH, D_MODEL, IN_COLS), f32) * D_MODEL ** -0.5,
        'b_in': b_in,
        'conv_w': nrm(ks[10], (DEPTH, CONV_WIDTH, 2 * MLSTM_WIDTH), f32) * CONV_WIDTH ** -0.5,
        'conv_b': 0.02 * nrm(ks[11], (DEPTH, 2 * MLSTM_WIDTH), f32),
        'mlstm_norm_w': 1.0 + 0.02 * nrm(ks[12], (DEPTH, MLSTM_WIDTH), f32),
        'ret_norm_w': 1.0 + 0.02 * nrm(ks[13], (DEPTH, RET_WIDTH), f32),
        'w_branch_m': nrm(ks[14], (DEPTH, MLSTM_WIDTH, D_MODEL), f32) * MLSTM_WIDTH ** -0.5,
        'w_branch_f': nrm(ks[15], (DEPTH, FOX_WIDTH, D_MODEL), f32) * FOX_WIDTH ** -0.5,
        'w_branch_r': nrm(ks[16], (DEPTH, RET_WIDTH, D_MODEL), f32) * RET_WIDTH ** -0.5,
        'w_out': nrm(ks[17], (DEPTH, D_MODEL, D_MODEL), f32) * D_MODEL ** -0.5,
        'ffn2_gate': nrm(ks[18], (DEPTH, D_MODEL, D_FF), f32) * D_MODEL ** -0.5,
        'ffn2_up': nrm(ks[19], (DEPTH, D_MODEL, D_FF), f32) * D_MODEL ** -0.5,
        'ffn2_down': nrm(ks[20], (DEPTH, D_FF, D_MODEL), f32) * D_FF ** -0.5,
    }


def reference(x, c, w_ada, b_ada, norm_w, ffn1_gate, ffn1_up, ffn1_down, w_in, b_in, conv_w, conv_b,
              mlstm_norm_w, ret_norm_w, w_branch_m, w_branch_f, w_branch_r, w_out,
              ffn2_gate, ffn2_up, ffn2_down):
    cond = jax.nn.silu(c)
    for l in range(DEPTH):
        mod = cond @ w_ada[l] + b_ada[l]
        sh1, sc1, gt1, sh2, sc2, gt2, sh3, sc3, gt3 = [m[:, None, :] for m in jnp.split(mod, N_MOD, axis=-1)]
        h = rmsnorm(x, norm_w[l, 0]) * (1.0 + sc1) + sh1
        x = x + HALF_STEP * gt1 * rmsnorm(swiglu(h, ffn1_gate[l], ffn1_up[l], ffn1_down[l]), norm_w[l, 1])
        h = rmsnorm(x, norm_w[l, 2]) * (1.0 + sc2) + sh2
        y = hybrid_mixer(h, w_in[l], b_in[l], conv_w[l], conv_b[l], mlstm_norm_w[l], ret_norm_w[l],
                         w_branch_m[l], w_branch_f[l], w_branch_r[l], w_out[l])
        x = x + gt2 * rmsnorm(y, norm_w[l, 3])
        h = rmsnorm(x, norm_w[l, 4]) * (1.0 + sc3) + sh3
        x = x + HALF_STEP * gt3 * rmsnorm(swiglu(h, ffn2_gate[l], ffn2_up[l], ffn2_down[l]), norm_w[l, 5])
    return x
```

```python
import numpy as np
from contextlib import ExitStack
import concourse.bass as bass
import concourse.mybir as mybir
from concourse.bass_utils import run_bass_kernel_spmd

F32 = mybir.dt.float32
BF16 = mybir.dt.bfloat16
AF = mybir.ActivationFunctionType
ALU = mybir.AluOpType

COMPUTE = ("pe", "act", "dve", "pool")
ENGINES = ("pe", "act", "dve", "pool", "sp")


class Buf:
    __slots__ = ("name", "w", "r")

    def __init__(self, name=""):
        self.name = name
        self.w = {}
        self.r = {}


class Prog:
    def __init__(self, nc):
        self.nc = nc
        self.es = ExitStack()
        self.cnt = {e: 0 for e in COMPUTE}
        self.waited = {e: {} for e in ENGINES}
        self.sems = {}
        self.eng = {"pe": nc.tensor, "act": nc.scalar, "dve": nc.vector, "pool": nc.gpsimd, "sp": nc.sync}
        for e in COMPUTE:
            self.sems[e] = self.es.enter_context(nc.semaphore("s_" + e))
        self.dma_pool, self.dma_cnt, self.dma_rr = {}, {}, {}
        for q, n in (("sp", 16), ("act", 4), ("pool", 8)):
            ks = []
            for i in range(n):
                k = "d_%s_%d" % (q, i)
                self.sems[k] = self.es.enter_context(nc.semaphore(k))
                self.dma_cnt[k] = 0
                ks.append(k)
            self.dma_pool[q] = ks
            self.dma_rr[q] = 0
        self.n_instr = 0

    def _deps(self, eng, reads, writes, nowaw=()):
        deps = {}

        def add(ev):
            if deps.get(ev[0], 0) < ev[1]:
                deps[ev[0]] = ev[1]
        for b in reads:
            for kv in b.w.items():
                add(kv)
        for b in writes:
            for kv in b.w.items():
                add(kv)
            for kv in b.r.items():
                add(kv)
        for b in nowaw:
            for kv in b.r.items():
                add(kv)
        waits = []
        wd = self.waited[eng]
        for k, v in deps.items():
            if k == eng and (eng == "pe" or v > self.cnt[eng]):
                continue
            if wd.get(k, 0) >= v:
                continue
            wd[k] = v
            waits.append((k, v))
        return waits

    @staticmethod
    def _mark(ev, reads, writes, nowaw=()):
        k, v = ev
        for b in reads:
            if b.r.get(k, 0) < v:
                b.r[k] = v
        for b in writes:
            b.w = {k: v}
            b.r = {}
        for b in nowaw:
            if b.w.get(k, 0) < v:
                b.w[k] = v

    def _emit(self, eng, waits, fn, ev):
        e = self.eng[eng]
        for k, v in waits:
            e.wait_ge(self.sems[k], v)
        if fn is None:
            return
        ins = fn(e)
        if ev is not None:
            ins.then_inc(self.sems[ev[0]], 1 if ev[0] in COMPUTE else 16)
        self.n_instr += 1

    def op(self, eng, fn, reads=(), writes=(), inc=True):
        waits = self._deps(eng, reads, writes)
        ev = (eng, self.cnt[eng] + 1)
        if inc:
            self.cnt[eng] += 1
        self._mark(ev, reads, writes)
        self._emit(eng, waits, fn, ev if inc else None)
        return ev

    def dma(self, q, out, in_, reads=(), writes=(), nowaw=()):
        pool = self.dma_pool[q]
        k = pool[self.dma_rr[q] % len(pool)]
        self.dma_rr[q] += 1
        waits = self._deps(q, reads, writes, nowaw)
        prev = self.dma_cnt[k] * 16
        if prev > 0 and self.waited[q].get(k, 0) < prev:
            self.waited[q][k] = prev
            waits.append((k, prev))
        self.dma_cnt[k] += 1
        ev = (k, self.dma_cnt[k] * 16)
        self._mark(ev, reads, writes, nowaw)
        self._emit(q, waits, lambda e: e.dma_start(out=out, in_=in_), ev)
        return ev

    def barrier(self):
        evs = [(e, self.cnt[e]) for e in COMPUTE if self.cnt[e] > 0]
        evs += [(k, c * 16) for k, c in self.dma_cnt.items() if c > 0]
        for eng in ENGINES:
            waits = []
            for k, v in evs:
                if k == eng and eng == "pe":
                    continue
                if self.waited[eng].get(k, 0) >= v:
                    continue
                self.waited[eng][k] = v
                waits.append((k, v))
            self._emit(eng, waits, None, None)

    def close(self):
        self.es.close()


class Cfg:
    pass


def mkcfg(full=True):
    c = Cfg()
    if full is True:
        c.D, c.DFF, c.S, c.NB, c.HM, c.HF, c.HR, c.L = 2048, 5632, 2048, 2, 4, 8, 4, 4
    elif full == "medium":
        c.D, c.DFF, c.S, c.NB, c.HM, c.HF, c.HR, c.L = 256, 768, 1024, 2, 4, 8, 4, 2
    else:
        c.D, c.DFF, c.S, c.NB, c.HM, c.HF, c.HR, c.L = 256, 512, 1024, 1, 1, 2, 1, 2
    c.T = 512
    c.N = c.NB * c.S
    c.DC, c.FC = c.D // 128, c.DFF // 128
    c.NT = c.N // c.T
    c.TPS = c.S // c.T
    c.MW, c.FW, c.RW = c.HM * 256, c.HF * 128, c.HR * 256
    c.NCH = c.N // 128
    c.CPS = c.S // 128
    widths = (2 * c.MW, c.MW, c.MW, c.HM, c.HM, c.FW, c.FW, c.FW, c.HF, c.RW, c.RW, c.RW, c.RW, c.D, c.D, c.D)
    names = ("m_qk", "m_v", "m_o", "m_i", "m_f", "f_q", "f_k", "f_v", "f_f", "r_q", "r_k", "r_v", "r_g", "g_m", "g_f", "g_r")
    st = np.cumsum((0,) + widths[:-1])
    c.col = {n: (int(s), int(w)) for n, s, w in zip(names, st, widths)}
    c.INC = int(sum(widths))
    c.frange = ("m_qk", "f_q", "f_k", "r_q", "r_k", "g_m", "g_f", "g_r")
    c.foff = {}
    o = 0
    for n in c.frange:
        c.foff[n] = o
        o += c.col[n][1] // 128
    c.NCF = o
    c.krange = ("m_v", "m_o", "f_v", "r_v", "r_g")
    return c


EPS = 1e-6
NEG = -60000.0


def build_program(cfg):
    D, DFF, S, NB, HM, HF, HR, L, T, N = cfg.D, cfg.DFF, cfg.S, cfg.NB, cfg.HM, cfg.HF, cfg.HR, cfg.L, cfg.T, cfg.N
    DC, FC, NT, TPS, MW, FW, RW, NCH, CPS = cfg.DC, cfg.FC, cfg.NT, cfg.TPS, cfg.MW, cfg.FW, cfg.RW, cfg.NCH, cfg.CPS
    KMAX = max(FC, 2 * DC)
    nc = bass.Bass("TRN2", target_bir_lowering=False)

    def din(name, shape, dt=F32):
        return nc.dram_tensor(name, list(shape), dt, kind="ExternalInput").ap()

    def dscr(name, shape, dt):
        return nc.dram_tensor(name, list(shape), dt).ap()

    x_in = din("x", [N, D])
    c_pc = din("c_pc", [128, DC, NB])
    w_ada = din("w_ada", [L, D, 9 * D])
    bada_pc = din("bada_pc", [128, L, 9 * DC])
    nw_pc = din("nw_pc", [128, L, 6, DC])
    ffn_w = {}
    for nm in ("ffn1_gate", "ffn1_up", "ffn2_gate", "ffn2_up"):
        ffn_w[nm] = din(nm, [L, D, DFF])
    for nm in ("ffn1_down", "ffn2_down"):
        ffn_w[nm] = din(nm, [L, DFF, D])
    w_in = din("w_in", [L, D, cfg.INC])
    b_in = din("b_in", [L, cfg.INC])
    bin_pc = din("bin_pc", [128, L, cfg.NCF])
    bsm_i = din("bsm_i", [HM, L])
    bsm_f = din("bsm_f", [HM, L])
    bsm_ff = din("bsm_ff", [HF, L])
    convw_pc = din("convw_pc", [128, L, 2 * MW // 128, 4])
    convb_pc = din("convb_pc", [128, L, 2 * MW // 128])
    mnw_b = din("mnw_b", [128, L, MW])
    rnw_b = din("rnw_b", [128, L, RW])
    w_bm = din("w_branch_m", [L, MW, D])
    w_bf = din("w_branch_f", [L, FW, D])
    w_br = din("w_branch_r", [L, RW, D])
    w_out = din("w_out", [L, D, D])
    ident_in = din("ident", [128, 128])
    maskT_in = din("maskT", [128, 128])
    sel_in = din("sel", [16, 16, 128])
    intra_in = din("intra", [128, HR, 128])
    qdec_in = din("qdec", [128, HR, 128])
    kdec_in = din("kdec", [128, HR])
    cos_in = din("cos", [128, S])
    sin_in = din("sin", [128, S])
    out = nc.dram_tensor("out", [N, D], F32, kind="ExternalOutput").ap()

    XT = dscr("XT", [D, N], F32)
    QKM = dscr("QKM", [2 * MW, N], BF16)
    VM = dscr("VM", [N, MW], BF16)
    OM = dscr("OM", [N, MW], BF16)
    G_I = dscr("G_I", [HM, N], F32)
    G_F = dscr("G_F", [HM, N], F32)
    G_FF = dscr("G_FF", [HF, N], F32)
    QF = dscr("QF", [FW, N], BF16)
    KF = dscr("KF", [FW, N], BF16)
    VF = dscr("VF", [N, FW], BF16)
    QR = dscr("QR", [RW, N], BF16)
    KR = dscr("KR", [RW, N], BF16)
    VR = dscr("VR", [N, RW], BF16)
    GR = dscr("GR", [N, RW], BF16)
    GATES = dscr("GATES", [3 * D, N], BF16)
    YM = dscr("YM", [MW, N], BF16)
    YF = dscr("YF", [FW, N], BF16)
    YR = dscr("YR", [RW, N], BF16)

    P = Prog(nc)
    glob = ExitStack()
    glob.enter_context(nc.allow_non_contiguous_dma(reason="tiny strided gate/bias loads"))

    uid = [0]

    def sb(es, name, shape, dt):
        uid[0] += 1
        return es.enter_context(nc.sbuf_tensor("s%d_%s" % (uid[0], name), list(shape), dt))

    banks = [glob.enter_context(nc.psum_tensor("pb%d" % i, [128, 512], F32)) for i in range(8)]
    bbuf = [Buf("pb%d" % i) for i in range(8)]
    ring = [0]

    def ps():
        i = ring[0] % 5
        ring[0] += 1
        return banks[i], bbuf[i]

    ident_f = sb(glob, "ident_f", [128, 128], F32)
    ident_b = sb(glob, "ident_b", [128, 128], BF16)
    ones_b = sb(glob, "ones_b", [128, 128], BF16)
    cst = sb(glob, "cst", [128, 4], F32)
    maskT = sb(glob, "maskT", [128, 128], F32)
    sel = sb(glob, "sel", [16, 16, 128], F32)
    MOD = sb(glob, "MOD", [128, 9 * DC, NB], F32)
    SCE = sb(glob, "SCE", [128, 3, DC, NB], F32)
    GTE = sb(glob, "GTE", [128, 3, DC, NB], F32)
    badap = sb(glob, "badap", [128, L, 9 * DC], F32)
    nwp = sb(glob, "nwp", [128, L, 6, DC], F32)
    binp = sb(glob, "binp", [128, L, cfg.NCF], F32)
    bsi = sb(glob, "bsi", [HM, L], F32)
    bsf = sb(glob, "bsf", [HM, L], F32)
    bsff = sb(glob, "bsff", [HF, L], F32)
    cwp = sb(glob, "cwp", [128, L, 2 * MW // 128, 4], F32)
    cbp = sb(glob, "cbp", [128, L, 2 * MW // 128], F32)
    condT = sb(glob, "condT", [128, DC, NB], BF16)
    c32 = sb(glob, "c32", [128, DC, NB], F32)
    b_const = Buf("const")
    b_mod = Buf("mod")
    for dst, src in ((ident_f, ident_in), (maskT, maskT_in), (sel, sel_in), (badap, bada_pc), (nwp, nw_pc),
                     (binp, bin_pc), (bsi, bsm_i), (bsf, bsm_f), (bsff, bsm_ff), (cwp, convw_pc), (cbp, convb_pc),
                     (c32, c_pc)):
        P.dma("sp", dst[:], src, writes=[b_const])
    P.op("dve", lambda e: e.tensor_copy(out=ident_b[:], in_=ident_f[:]), reads=[b_const], writes=[b_const])
    P.op("dve", lambda e: e.memset(ones_b[:], 1.0), writes=[b_const])
    P.op("dve", lambda e: e.memset(cst[:, 0:1], EPS * D), writes=[b_const])
    P.op("dve", lambda e: e.memset(cst[:, 1:2], EPS), writes=[b_const])
    P.op("dve", lambda e: e.memset(cst[:, 2:3], 1.0), writes=[b_const])
    P.op("act", lambda e: e.activation(out=condT[:], in_=c32[:], func=AF.Silu), reads=[b_const], writes=[b_const])

    b_XT = [[Buf("XT%d_%d" % (i, c)) for c in range(DC)] for i in range(NT)]
    b_scr = Buf("scr")
    b_y = Buf("ybranch")
    b_out = Buf("out")

    def tok(tt):
        return slice(tt * T, (tt + 1) * T)

    with ExitStack() as es:
        xin = [sb(es, "xin%d" % i, [128, T // 128, D], F32) for i in range(2)]
        xo = [sb(es, "xo%d" % i, [128, T], F32) for i in range(3)]
        b_xin = [Buf() for _ in range(2)]
        b_xo = [Buf() for _ in range(3)]
        k = 0
        for tt in range(NT):
            s = tt % 2
            P.dma("sp", xin[s][:], x_in[tok(tt), :].rearrange("(c p) d -> p c d", p=128), writes=[b_xin[s]])
            for dc in range(DC):
                pt, pb = ps()
                for tc in range(T // 128):
                    P.op("pe", lambda e, pt=pt, s=s, tc=tc, dc=dc: e.matmul(
                        pt[:, tc * 128:(tc + 1) * 128], lhsT=xin[s][:, tc, dc * 128:(dc + 1) * 128], rhs=ident_f[:],
                        start=True, stop=True), reads=[b_xin[s], b_const], writes=[pb], inc=(tc == T // 128 - 1))
                o = k % 3
                eng = "act" if k % 2 == 0 else "dve"
                if eng == "act":
                    P.op("act", lambda e, o=o, pt=pt: e.copy(out=xo[o][:], in_=pt[:]), reads=[pb], writes=[b_xo[o]])
                else:
                    P.op("dve", lambda e, o=o, pt=pt: e.tensor_copy(out=xo[o][:], in_=pt[:]), reads=[pb], writes=[b_xo[o]])
                P.dma("sp", XT[dc * 128:(dc + 1) * 128, tok(tt)], xo[o][:], reads=[b_xo[o]], writes=[b_XT[tt][dc]])
                k += 1
        P.barrier()

    class G:
        pass

    def alloc_gemm(es, kmax):
        g = G()
        g.xy = sb(es, "xy", [128, DC, T], F32)
        g.hT = sb(es, "hT", [128, DC, T], BF16)
        g.wb = [sb(es, "wb%d" % i, [128, kmax, 256], BF16) for i in range(3)]
        g.b_wb = [[Buf("wb%d_%d" % (i, p_)) for p_ in range(8)] for i in range(3)]
        g.wi = [0]
        g.sq = [sb(es, "sq%d" % i, [128, T], BF16) for i in range(2)]
        g.b_sq = [Buf() for _ in range(2)]
        g.tmp = [sb(es, "tmp%d" % i, [128, T], F32) for i in range(3)]
        g.b_tmp = [Buf() for _ in range(3)]
        g.ti = [0]
        g.rstd = sb(es, "rstd", [128, T], F32)
        g.b_rstd = Buf()
        g.xr = [sb(es, "xr%d" % i, [128, T], F32) for i in range(3)]
        g.b_xr = [Buf() for _ in range(3)]
        g.xo = [sb(es, "xo%d" % i, [128, T], F32) for i in range(3)]
        g.b_xo = [Buf() for _ in range(3)]
        g.ob = [sb(es, "ob%d" % i, [128, T], BF16) for i in range(4)]
        g.b_ob = [Buf() for _ in range(4)]
        g.oi = [0]
        g.b_xy = Buf("xy")
        g.b_hT = Buf("hT")
        return g

    def wslot(g):
        s = g.wi[0] % 3
        g.wi[0] += 1
        return s

    def tmpslot(g):
        s = g.ti[0] % 3
        g.ti[0] += 1
        return s

    def obslot(g):
        s = g.oi[0] % 4
        g.oi[0] += 1
        return s

    def load_w(g, s, W2d, k0, kc_n, c0, ncols, kdst=0):
        for a in range(0, kc_n, 8):
            n = min(8, kc_n - a)
            src = W2d[(k0 + a) * 128:(k0 + a + n) * 128, c0:c0 + ncols].rearrange("(kc p) n -> p kc n", p=128)
            P.dma("pool", g.wb[s][:, kdst + a:kdst + a + n, 0:ncols], src, writes=[g.b_wb[s][(kdst + a) // 8]])

    def sumsq_rstd(g, src_fn, nchunks, rbuf):
        pt, pb = ps()
        for c in range(nchunks):
            q = c % 2
            P.op("act", lambda e, q=q, c=c: e.activation(out=g.sq[q][:], in_=src_fn(c), func=AF.Square),
                 reads=[rbuf], writes=[g.b_sq[q]])
            P.op("pe", lambda e, q=q, c=c, pt=pt: e.matmul(pt[:], lhsT=ones_b[:], rhs=g.sq[q][:], start=(c == 0),
                                                         stop=(c == nchunks - 1)),
                 reads=[g.b_sq[q], b_const], writes=[pb], inc=True)
        P.op("act", lambda e, pt=pt: e.activation(out=g.rstd[:], in_=pt[:], func=AF.Sqrt, bias=cst[:, 0:1]),
             reads=[pb, b_const], writes=[g.b_rstd])
        P.op("dve", lambda e: e.reciprocal(out=g.rstd[:], in_=g.rstd[:]), reads=[g.b_rstd], writes=[g.b_rstd])

    def load_norm(g, tt, j):
        b = tt // TPS
        P.dma("sp", g.xy[:], XT[:, tok(tt)].rearrange("(c p) t -> p c t", p=128), reads=b_XT[tt], writes=[g.b_xy])
        import os
        LNP = os.environ.get('LN_PARTS', 'sa')
        if 's' in LNP:
            sumsq_rstd(g, lambda c: g.xy[:, c, :], DC, g.b_xy)
        for c in range(DC if 'a' in LNP else 0):
            s = tmpslot(g)
            P.op("dve", lambda e, s=s, c=c: e.tensor_tensor(out=g.tmp[s][:], in0=g.xy[:, c, :], in1=g.rstd[:], op=ALU.mult),
                 reads=[g.b_xy, g.b_rstd], writes=[g.b_tmp[s]])
            P.op("act", lambda e, s=s, c=c: e.activation(out=g.hT[:, c, :], in_=g.tmp[s][:], func=AF.Identity,
                                                          scale=SCE[:, j, c, b:b + 1], bias=MOD[:, (3 * j) * DC + c, b:b + 1]),
                 reads=[g.b_tmp[s], b_mod], writes=[g.b_hT])

    def post_res(g, tt, j):
        b = tt // TPS
        sumsq_rstd(g, lambda c: g.xy[:, c, :], DC, g.b_xy)
        for c in range(DC):
            r = c % 3
            P.dma("sp", g.xr[r][:], XT[c * 128:(c + 1) * 128, tok(tt)], reads=[b_XT[tt][c]], writes=[g.b_xr[r]])
            s = tmpslot(g)
            P.op("dve", lambda e, s=s, c=c: e.tensor_tensor(out=g.tmp[s][:], in0=g.xy[:, c, :], in1=g.rstd[:], op=ALU.mult),
                 reads=[g.b_xy, g.b_rstd], writes=[g.b_tmp[s]])
            P.op("dve", lambda e, s=s, c=c, r=r: e.scalar_tensor_tensor(
                out=g.xo[r][:], in0=g.tmp[s][:], scalar=GTE[:, j, c, b:b + 1], in1=g.xr[r][:], op0=ALU.mult, op1=ALU.add),
                reads=[g.b_tmp[s], g.b_xr[r], b_mod], writes=[g.b_xo[r]])
            P.dma("sp", XT[c * 128:(c + 1) * 128, tok(tt)], g.xo[r][:], reads=[g.b_xo[r]], writes=[b_XT[tt][c]])

    def gemm_F(g, W2d, c0, ncols, KCn, rhs_fn, rbufs, epi, M=128):
        for t0 in range(0, ncols, 256):
            nw = min(256, ncols - t0)
            s = wslot(g)
            load_w(g, s, W2d, 0, KCn, c0 + t0, nw)
            for jn in range(0, nw, 128):
                m = min(M, nw - jn)
                pt, pb = ps()
                for kc in range(KCn):
                    P.op("pe", lambda e, pt=pt, s=s, kc=kc, jn=jn, m=m: e.matmul(
                        pt[0:m, :], lhsT=g.wb[s][:, kc, jn:jn + m], rhs=rhs_fn(kc), start=(kc == 0), stop=(kc == KCn - 1)),
                        reads=[g.b_wb[s][kc // 8]] + rbufs, writes=[pb], inc=(kc == KCn - 1))
                epi((t0 + jn) // 128, pt, pb)

    def compute_mod(l, g):
        pt, pb = banks[5], bbuf[5]
        ncols = 9 * D
        for t0 in range(0, ncols, 256):
            s = wslot(g)
            load_w(g, s, w_ada[l], 0, DC, t0, 256)
            for jn in (0, 128):
                ci = (t0 + jn) // 128
                for kc in range(DC):
                    P.op("pe", lambda e, s=s, kc=kc, jn=jn, ci=ci: e.matmul(
                        pt[:, ci * NB:(ci + 1) * NB], lhsT=g.wb[s][:, kc, jn:jn + 128], rhs=condT[:, kc, :],
                        start=(kc == 0), stop=(kc == DC - 1)),
                        reads=[g.b_wb[s][kc // 8], b_const], writes=[pb], inc=(kc == DC - 1 and jn == 128))
        pv = pt[:, 0:9 * DC * NB].rearrange("p (c b) -> p c b", b=NB)
        for b in range(NB):
            P.op("dve", lambda e, b=b: e.tensor_tensor(out=MOD[:, :, b], in0=pv[:, :, b], in1=badap[:, l, :], op=ALU.add),
                 reads=[pb, b_const], writes=[b_mod])
        sqD = float(np.sqrt(D))
        for j in range(3):
            step = 1.0 if j == 1 else 0.5
            for b in range(NB):
                P.op("dve", lambda e, j=j, b=b: e.scalar_tensor_tensor(
                    out=SCE[:, j, :, b], in0=MOD[:, (3 * j + 1) * DC:(3 * j + 2) * DC, b], scalar=1.0,
                    in1=nwp[:, l, 2 * j, :], op0=ALU.add, op1=ALU.mult), reads=[b_mod, b_const], writes=[b_mod])
                P.op("dve", lambda e, j=j, b=b: e.tensor_scalar(
                    out=SCE[:, j, :, b], in0=SCE[:, j, :, b], scalar1=sqD, scalar2=None, op0=ALU.mult),
                    reads=[b_mod], writes=[b_mod])
                P.op("dve", lambda e, j=j, b=b, step=step: e.scalar_tensor_tensor(
                    out=GTE[:, j, :, b], in0=MOD[:, (3 * j + 2) * DC:(3 * j + 3) * DC, b], scalar=step * sqD,
                    in1=nwp[:, l, 2 * j + 1, :], op0=ALU.mult, op1=ALU.mult), reads=[b_mod, b_const], writes=[b_mod])

    def ffn(g, aT, b_aT, l, j, Wg, Wu, Wd):
        import os
        PARTS = os.environ.get('FFN_PARTS', 'ngdp')
        for tt in range(int(os.environ.get('FFN_NT', NT))):
            if 'n' in PARTS:
                load_norm(g, tt, j)
            for ft in range(DFF // 256 if 'g' in PARTS else 0):
                s = wslot(g)
                load_w(g, s, Wg[l], 0, DC, ft * 256, 256, kdst=0)
                load_w(g, s, Wu[l], 0, DC, ft * 256, 256, kdst=DC)
                for jn in (0, 128):
                    pa, ba = ps()
                    pu, bu = ps()
                    for (pt, pb, ko) in ((pa, ba, 0), (pu, bu, DC)):
                        for kc in range(DC):
                            P.op("pe", lambda e, pt=pt, s=s, kc=kc, ko=ko, jn=jn: e.matmul(
                                pt[:], lhsT=g.wb[s][:, ko + kc, jn:jn + 128], rhs=g.hT[:, kc, :],
                                start=(kc == 0), stop=(kc == DC - 1)),
                                reads=[g.b_wb[s][(ko + kc) // 8], g.b_hT], writes=[pb], inc=(kc == DC - 1))
                    ts_ = tmpslot(g)
                    fc = ft * 2 + jn // 128
                    P.op("act", lambda e, ts_=ts_, pa=pa: e.activation(out=g.tmp[ts_][:], in_=pa[:], func=AF.Silu),
                         reads=[ba], writes=[g.b_tmp[ts_]])
                    P.op("dve", lambda e, ts_=ts_, pu=pu, fc=fc: e.tensor_tensor(out=aT[:, fc, :], in0=g.tmp[ts_][:], in1=pu[:],
                                                                                op=ALU.mult),
                         reads=[g.b_tmp[ts_], bu], writes=[b_aT])

            kk = [0]

            def epi(ci, pt, pb):
                if kk[0] % 2 == 0:
                    P.op("act", lambda e: e.copy(out=g.xy[:, ci, :], in_=pt[:]), reads=[pb], writes=[g.b_xy])
                else:
                    P.op("dve", lambda e: e.tensor_copy(out=g.xy[:, ci, :], in_=pt[:]), reads=[pb], writes=[g.b_xy])
                kk[0] += 1
            if 'd' in PARTS:
                gemm_F(g, Wd[l], 0, D, FC, lambda kc: aT[:, kc, :], [b_aT], epi)
            if 'p' in PARTS:
                post_res(g, tt, j)

    def mixer_in(g, l, zb, b_zb, halo, b_halo, brow, b_brow, wsm, b_wsm, osm, b_osm, cs, sn):
        W = w_in[l]
        for i, nm in enumerate(cfg.krange):
            c0, wdt = cfg.col[nm]
            P.dma("pool", brow[0:1, i, 0:wdt], b_in[l:l + 1, c0:c0 + wdt], writes=[b_brow])
        for i, (nm, h) in enumerate((("m_i", HM), ("m_f", HM), ("f_f", HF))):
            c0, wdt = cfg.col[nm]
            for k0 in range(0, DC, 8):
                k1 = min(DC, k0 + 8)
                P.dma("pool", wsm[:, k0:k1, i, 0:wdt], W[k0 * 128:k1 * 128, c0:c0 + wdt].rearrange("(kc p) n -> p kc n", p=128),
                      writes=[b_wsm])
        for tt in range(NT):
            b = tt // TPS
            first = (tt % TPS == 0)
            load_norm(g, tt, 1)
            hfn = lambda kc: g.hT[:, kc, :]
            c0, wdt = cfg.col["m_qk"]

            def epi_qk(ci, pt, pb):
                q = ci % 2
                if first:
                    P.op("dve", lambda e: e.memset(zb[q][:, 0:3], 0.0), writes=[b_zb[q]])
                else:
                    P.op("dve", lambda e: e.tensor_copy(out=zb[q][:, 0:3], in_=halo[:, ci, :]), reads=[b_halo], writes=[b_zb[q]])
                P.op("act", lambda e: e.activation(out=zb[q][:, 3:3 + T], in_=pt[:], func=AF.Identity,
                                                   bias=binp[:, l, cfg.foff["m_qk"] + ci:cfg.foff["m_qk"] + ci + 1]),
                     reads=[pb, b_const], writes=[b_zb[q]])
                P.op("dve", lambda e: e.tensor_copy(out=halo[:, ci, :], in_=zb[q][:, T:T + 3]), reads=[b_zb[q]], writes=[b_halo])
                s = tmpslot(g)
                P.op("dve", lambda e: e.tensor_scalar(out=g.tmp[s][:], in0=zb[q][:, 0:T], scalar1=cwp[:, l, ci, 0:1],
                                                      scalar2=None, op0=ALU.mult), reads=[b_zb[q], b_const], writes=[g.b_tmp[s]])
                for jj in (1, 2, 3):
                    P.op("dve", lambda e, jj=jj: e.scalar_tensor_tensor(
                        out=g.tmp[s][:], in0=zb[q][:, jj:jj + T], scalar=cwp[:, l, ci, jj:jj + 1], in1=g.tmp[s][:],
                        op0=ALU.mult, op1=ALU.add), reads=[b_zb[q], b_const, g.b_tmp[s]], writes=[g.b_tmp[s]])
                o = obslot(g)
                P.op("act", lambda e: e.activation(out=g.ob[o][:], in_=g.tmp[s][:], func=AF.Silu, bias=cbp[:, l, ci:ci + 1]),
                     reads=[g.b_tmp[s], b_const], writes=[g.b_ob[o]])
                P.dma("sp", QKM[ci * 128:(ci + 1) * 128, tok(tt)], g.ob[o][:], reads=[g.b_ob[o]], nowaw=[b_scr])
            gemm_F(g, W, c0, wdt, DC, hfn, [g.b_hT], epi_qk)

            def mk_epi(nm, dst, func, rowoff=0):
                def epi(ci, pt, pb):
                    o = obslot(g)
                    P.op("act", lambda e: e.activation(out=g.ob[o][:], in_=pt[:], func=func,
                                                       bias=binp[:, l, cfg.foff[nm] + ci:cfg.foff[nm] + ci + 1]),
                         reads=[pb, b_const], writes=[g.b_ob[o]])
                    P.dma("sp", dst[rowoff + ci * 128:rowoff + (ci + 1) * 128, tok(tt)], g.ob[o][:], reads=[g.b_ob[o]],
                          nowaw=[b_scr])
                return epi
            for nm, dst, func, ro in (("f_q", QF, AF.Identity, 0), ("f_k", KF, AF.Identity, 0),
                                      ("g_m", GATES, AF.Sigmoid, 0), ("g_f", GATES, AF.Sigmoid, D),
                                      ("g_r", GATES, AF.Sigmoid, 2 * D)):
                c0, wdt = cfg.col[nm]
                gemm_F(g, W, c0, wdt, DC, hfn, [g.b_hT], mk_epi(nm, dst, func, ro))

            pos = slice((tt % TPS) * T, (tt % TPS + 1) * T)
            for nm, dst in (("r_q", QR), ("r_k", KR)):
                c0, wdt = cfg.col[nm]
                st = {}

                def epi_rot(ci, pt, pb, nm=nm, dst=dst, st=st):
                    s = tmpslot(g)
                    P.op("act", lambda e: e.activation(out=g.tmp[s][:], in_=pt[:], func=AF.Identity,
                                                       bias=binp[:, l, cfg.foff[nm] + ci:cfg.foff[nm] + ci + 1]),
                         reads=[pb, b_const], writes=[g.b_tmp[s]])
                    if ci % 2 == 0:
                        st["a"] = s
                        return
                    s1, s2 = st["a"], s
                    for which in (0, 1):
                        s3 = tmpslot(g)
                        o = obslot(g)
                        ta, tb = (cs, sn) if which == 0 else (sn, cs)
                        P.op("dve", lambda e, ta=ta, s3=s3: e.tensor_tensor(out=g.tmp[s3][:], in0=g.tmp[s1][:], in1=ta[:, pos],
                                                                               op=ALU.mult),
                             reads=[g.b_tmp[s1], b_const], writes=[g.b_tmp[s3]])
                        P.op("dve", lambda e, tb=tb, o=o: e.tensor_tensor(out=g.ob[o][:], in0=g.tmp[s2][:], in1=tb[:, pos],
                                                                          op=ALU.mult),
                             reads=[g.b_tmp[s2], b_const], writes=[g.b_ob[o]])
                        P.op("dve", lambda e, o=o, s3=s3, which=which: e.tensor_tensor(
                            out=g.ob[o][:], in0=g.tmp[s3][:], in1=g.ob[o][:],
                            op=(ALU.subtract if which == 0 else ALU.add)),
                            reads=[g.b_tmp[s3], g.b_ob[o]], writes=[g.b_ob[o]])
                        cio = ci - 1 + which
                        P.dma("sp", dst[cio * 128:(cio + 1) * 128, tok(tt)], g.ob[o][:], reads=[g.b_ob[o]], nowaw=[b_scr])
                gemm_F(g, W, c0, wdt, DC, hfn, [g.b_hT], epi_rot)

            for i, (nm, hn, dst, bias) in enumerate((("m_i", HM, G_I, bsi), ("m_f", HM, G_F, bsf), ("f_f", HF, G_FF, bsff))):
                pt, pb = ps()
                for kc in range(DC):
                    P.op("pe", lambda e, pt=pt, kc=kc, i=i, hn=hn: e.matmul(
                        pt[0:hn, :], lhsT=wsm[:, kc, i, 0:hn], rhs=g.hT[:, kc, :], start=(kc == 0), stop=(kc == DC - 1)),
                        reads=[b_wsm, g.b_hT], writes=[pb], inc=(kc == DC - 1))
                q = i
                P.op("act", lambda e, pt=pt, hn=hn, bias=bias, q=q: e.activation(
                    out=osm[q][0:hn, :], in_=pt[0:hn, :], func=AF.Identity, bias=bias[0:hn, l:l + 1]),
                    reads=[pb, b_const], writes=[b_osm[q]])
                P.dma("sp", dst[0:hn, tok(tt)], osm[q][0:hn, :], reads=[b_osm[q]], nowaw=[b_scr])

            for i, (nm, dst, func) in enumerate((("m_v", VM, AF.Identity), ("m_o", OM, AF.Sigmoid), ("f_v", VF, AF.Identity),
                                                 ("r_v", VR, AF.Identity), ("r_g", GR, AF.Silu))):
                c0, wdt = cfg.col[nm]
                for t0 in range(0, wdt, 256):
                    s = wslot(g)
                    load_w(g, s, W, 0, DC, c0 + t0, 256)
                    for tc in range(T // 128):
                        pt, pb = ps()
                        for kc in range(DC):
                            P.op("pe", lambda e, pt=pt, s=s, kc=kc, tc=tc: e.matmul(
                                pt[:, 0:256], lhsT=g.hT[:, kc, tc * 128:(tc + 1) * 128], rhs=g.wb[s][:, kc, 0:256],
                                start=(kc == 0), stop=False), reads=[g.b_wb[s][kc // 8], g.b_hT], writes=[pb], inc=False)
                        P.op("pe", lambda e, pt=pt, i=i, t0=t0: e.matmul(
                            pt[:, 0:256], lhsT=ones_b[0:1, :], rhs=brow[0:1, i, t0:t0 + 256], start=False, stop=True),
                            reads=[b_brow, b_const], writes=[pb], inc=True)
                        o = obslot(g)
                        P.op("act", lambda e, o=o, pt=pt, func=func: e.activation(out=g.ob[o][:, 0:256], in_=pt[:, 0:256], func=func),
                             reads=[pb], writes=[g.b_ob[o]])
                        r0 = tt * T + tc * 128
                        P.dma("sp", dst[r0:r0 + 128, t0:t0 + 256], g.ob[o][:, 0:256], reads=[g.b_ob[o]], nowaw=[b_scr])

    def mixer_out(g, l, yb, b_yb, macc, b_macc, gt, b_gt):
        for tt in range(NT):
            for i, (Y, Wd_) in enumerate(((YM, MW), (YF, FW), (YR, RW))):
                P.dma("sp", yb[i][:, 0:Wd_ // 128, :], Y[:, tok(tt)].rearrange("(c p) t -> p c t", p=128),
                      reads=[b_y], writes=[b_yb[i]])
            for i, (Wb, Wd_) in enumerate(((w_bm, MW), (w_bf, FW), (w_br, RW))):
                def epi(ci, pt, pb, i=i):
                    q = ci % 2
                    P.dma("sp", gt[q][:], GATES[i * D + ci * 128:i * D + (ci + 1) * 128, tok(tt)], reads=[b_scr], writes=[b_gt[q]])
                    if i == 0:
                        P.op("dve", lambda e: e.tensor_tensor(out=macc[:, ci, :], in0=pt[:], in1=gt[q][:], op=ALU.mult),
                             reads=[pb, b_gt[q]], writes=[b_macc])
                    else:
                        s = tmpslot(g)
                        P.op("dve", lambda e: e.tensor_tensor(out=g.tmp[s][:], in0=pt[:], in1=gt[q][:], op=ALU.mult),
                             reads=[pb, b_gt[q]], writes=[g.b_tmp[s]])
                        if i == 1:
                            P.op("dve", lambda e: e.tensor_tensor(out=macc[:, ci, :], in0=macc[:, ci, :], in1=g.tmp[s][:], op=ALU.add),
                                 reads=[g.b_tmp[s], b_macc], writes=[b_macc])
                        else:
                            P.op("dve", lambda e: e.tensor_tensor(out=g.hT[:, ci, :], in0=macc[:, ci, :], in1=g.tmp[s][:], op=ALU.add),
                                 reads=[g.b_tmp[s], b_macc], writes=[g.b_hT])
                gemm_F(g, Wb[l], 0, D, Wd_ // 128, lambda kc, i=i: yb[i][:, kc, :], [b_yb[i]], epi)
            kk = [0]

            def epi_o(ci, pt, pb):
                if kk[0] % 2 == 0:
                    P.op("act", lambda e: e.copy(out=g.xy[:, ci, :], in_=pt[:]), reads=[pb], writes=[g.b_xy])
                else:
                    P.op("dve", lambda e: e.tensor_copy(out=g.xy[:, ci, :], in_=pt[:]), reads=[pb], writes=[g.b_xy])
                kk[0] += 1
            gemm_F(g, w_out[l], 0, D, DC, lambda kc: g.hT[:, kc, :], [g.b_hT], epi_o)
            post_res(g, tt, 1)

    def seq_mixers(l):
        RM, RF = NB * HM, NB * HF
        with ExitStack() as es:
            GMN = sb(es, "GMN", [RM, S], F32)
            FS = sb(es, "FS", [RF, S], F32)
            MS = sb(es, "MS", [128, CPS, 4, RM], F32)
            DECB = sb(es, "DECB", [128, RM, CPS], F32)
            FNT = sb(es, "FNT", [128, CPS, RF], F32)
            br = Buf("rows")
            bt = Buf("tokmaj")
            with ExitStack() as es1:
                UU = sb(es1, "UU", [RM, S], F32)
                GM = sb(es1, "GM", [RM, S], F32)
                BN = sb(es1, "BN", [RM, S], F32)
                RA = sb(es1, "RA", [RM, S], F32)
                RK = sb(es1, "RK", [RM, S], F32)
                GC = sb(es1, "GC", [RM, CPS], F32)
                FFp = sb(es1, "FFp", [RF, S], F32)
                FN = sb(es1, "FN", [RF, S], F32)
                onesr = sb(es1, "onesr", [max(RM, RF), S], F32)
                for b in range(NB):
                    sq_ = slice(b * S, (b + 1) * S)
                    P.dma("sp", UU[b * HM:(b + 1) * HM, :], G_I[:, sq_], reads=[b_scr], writes=[br])
                    P.dma("sp", GM[b * HM:(b + 1) * HM, :], G_F[:, sq_], reads=[b_scr], writes=[br])
                    P.dma("sp", FFp[b * HF:(b + 1) * HF, :], G_FF[:, sq_], reads=[b_scr], writes=[br])
                P.op("dve", lambda e: e.memset(onesr[:], 1.0), writes=[br])
                for tl in (GM, FFp):
                    P.op("act", lambda e, tl=tl: e.activation(out=tl[:], in_=tl[:], func=AF.Exp, scale=-1.0), reads=[br], writes=[br])
                    P.op("act", lambda e, tl=tl: e.activation(out=tl[:], in_=tl[:], func=AF.Ln, bias=cst[0:tl.shape[0], 2:3]), reads=[br], writes=[br])
                P.op("dve", lambda e: e.tensor_tensor_scan(out=BN[:], data0=onesr[0:RM, :], data1=GM[:], initial=0.0,
                                                           op0=ALU.mult, op1=ALU.add), reads=[br], writes=[br])
                P.op("dve", lambda e: e.tensor_tensor_scan(out=FN[:], data0=onesr[0:RF, :], data1=FFp[:], initial=0.0,
                                                           op0=ALU.mult, op1=ALU.add), reads=[br], writes=[br])
                P.op("dve", lambda e: e.tensor_tensor(out=UU[:], in0=UU[:], in1=BN[:], op=ALU.add), reads=[br], writes=[br])
                P.op("dve", lambda e: e.tensor_tensor_scan(out=GM[:], data0=UU[:], data1=UU[:], initial=0.0,
                                                           op0=ALU.max, op1=ALU.max), reads=[br], writes=[br])
                P.op("dve", lambda e: e.tensor_scalar(out=GMN[:], in0=GM[:], scalar1=-1.0, scalar2=None, op0=ALU.mult), reads=[br], writes=[br])
                P.op("dve", lambda e: e.tensor_scalar(out=FS[:], in0=FN[:], scalar1=-1.0, scalar2=None, op0=ALU.mult),
                     reads=[br], writes=[br])
                GMv = GM[:].rearrange("p (c l) -> p c l", l=128)
                P.op("dve", lambda e: e.memset(GC[:, 0:1], 0.0), reads=[br], writes=[br])
                P.op("dve", lambda e: e.tensor_copy(out=GC[:, 1:CPS], in_=GMv[:, 0:CPS - 1, 127]), reads=[br], writes=[br])
                P.op("dve", lambda e: e.tensor_tensor(out=BN[:], in0=BN[:], in1=GM[:], op=ALU.subtract), reads=[br], writes=[br])
                P.op("act", lambda e: e.activation(out=BN[:], in_=BN[:], func=AF.Exp), reads=[br], writes=[br])
                for c in range(CPS):
                    cl = slice(c * 128, (c + 1) * 128)
                    P.op("dve", lambda e, c=c, cl=cl: e.tensor_scalar(out=RA[:, cl], in0=GMN[:, cl], scalar1=GC[:, c:c + 1], scalar2=None,
                                                                      op0=ALU.add), reads=[br], writes=[br])
                    P.op("dve", lambda e, c=c, cl=cl: e.tensor_scalar(out=RK[:, cl], in0=UU[:, cl], scalar1=GM[:, c * 128 + 127:c * 128 + 128],
                                                                      scalar2=float(np.log(16.0)), op0=ALU.subtract, op1=ALU.subtract),
                         reads=[br], writes=[br])
                P.op("act", lambda e: e.activation(out=RA[:], in_=RA[:], func=AF.Exp), reads=[br], writes=[br])
                P.op("act", lambda e: e.activation(out=RK[:], in_=RK[:], func=AF.Exp), reads=[br], writes=[br])
                pms, bms = banks[5], bbuf[5]
                pdc, bdc = banks[6], bbuf[6]
                pfn, bfn = banks[7], bbuf[7]
                nq = 4 * RM
                for c in range(CPS):
                    cl = slice(c * 128, (c + 1) * 128)
                    for qi, R_ in enumerate((UU, RA, BN, RK)):
                        last = (c == CPS - 1 and qi == 3)
                        P.op("pe", lambda e, c=c, qi=qi, R_=R_, cl=cl: e.matmul(
                            pms[:, c * nq + qi * RM:c * nq + (qi + 1) * RM], lhsT=R_[:, cl], rhs=ident_f[0:RM, 0:RM], start=True, stop=True),
                            reads=[br, b_const], writes=[bms], inc=last)
                    P.op("pe", lambda e, c=c, cl=cl: e.matmul(pfn[:, c * RF:(c + 1) * RF], lhsT=FN[:, cl], rhs=ident_f[0:RF, 0:RF],
                                                              start=True, stop=True), reads=[br, b_const], writes=[bfn], inc=(c == CPS - 1))
                RAv = RA[:].rearrange("p (c l) -> p c l", l=128)
                for h in range(RM):
                    P.op("pe", lambda e, h=h: e.matmul(pdc[:, h * CPS:(h + 1) * CPS], lhsT=sel[0:RM, h, :], rhs=RAv[:, :, 127],
                                                       start=True, stop=True), reads=[br, b_const], writes=[bdc], inc=(h == RM - 1))
                P.op("dve", lambda e: e.tensor_copy(out=MS[:].rearrange("p c q h -> p (c q h)"), in_=pms[:, 0:CPS * nq]),
                     reads=[bms], writes=[bt])
                P.op("dve", lambda e: e.tensor_copy(out=DECB[:].rearrange("p h c -> p (h c)"), in_=pdc[:, 0:RM * CPS]), reads=[bdc], writes=[bt])
                P.op("dve", lambda e: e.tensor_copy(out=FNT[:].rearrange("p c h -> p (c h)"), in_=pfn[:, 0:CPS * RF]), reads=[bfn], writes=[bt])
                P.barrier()

            with ExitStack() as es2:
                kT = [sb(es2, "fkT%d" % i, [128, S], BF16) for i in range(2)]
                qT = [sb(es2, "fqT%d" % i, [128, S], BF16) for i in range(2)]
                Vt = [sb(es2, "fV%d" % i, [128, CPS, 128], BF16) for i in range(2)]
                b_in_ = [Buf() for _ in range(2)]
                PT = [sb(es2, "fPT%d" % i, [128, T], BF16) for i in range(3)]
                b_PT = [Buf() for _ in range(3)]
                rden = sb(es2, "frden", [128, T], F32)
                b_rden = Buf()
                Fb = [sb(es2, "fFb%d" % i, [128, T], F32) for i in range(2)]
                b_Fb = [Buf() for _ in range(2)]
                ftmp = [sb(es2, "ftmp%d" % i, [128, T], F32) for i in range(3)]
                b_ftmp = [Buf() for _ in range(3)]
                fbi = 0
                yo = [sb(es2, "fyo%d" % i, [128, T], BF16) for i in range(2)]
                b_yo = [Buf() for _ in range(2)]
                pO, bO = banks[5], bbuf[5]
                pD, bD = banks[6], bbuf[6]
                scale = float(128.0 ** -0.5)
                it = 0
                pi = 0
                oi = 0
                for b in range(NB):
                    for h in range(HF):
                        s = it % 2
                        it += 1
                        bh = b * HF + h
                        sq_ = slice(b * S, (b + 1) * S)
                        P.dma("sp", kT[s][:], KF[h * 128:(h + 1) * 128, sq_], reads=[b_scr], writes=[b_in_[s]])
                        P.dma("sp", qT[s][:], QF[h * 128:(h + 1) * 128, sq_], reads=[b_scr], writes=[b_in_[s]])
                        P.dma("sp", Vt[s][:], VF[sq_, h * 128:(h + 1) * 128].rearrange("(c p) e -> p c e", p=128),
                              reads=[b_scr], writes=[b_in_[s]])
                        for gq in range(TPS):
                            nkb = 4 * gq + 4
                            pF, bF = ps()
                            P.op("pe", lambda e: e.matmul(pF[:, 0:T], lhsT=sel[0:RF, bh, :], rhs=FS[:, gq * T:(gq + 1) * T],
                                                          start=True, stop=True), reads=[br, b_const], writes=[bF], inc=True)
                            fb = fbi % 2
                            fbi += 1
                            P.op("act", lambda e: e.copy(out=Fb[fb][:], in_=pF[:, 0:T]), reads=[bF], writes=[b_Fb[fb]])
                            pending = None
                            for j in range(nkb):
                                lo = max(0, j - 4 * gq) * 128
                                q0, q1 = gq * T + lo, (gq + 1) * T
                                ncol = T - lo
                                pt, pb = ps()
                                diag = (j >= 4 * gq)
                                P.op("pe", lambda e: e.matmul(
                                    pt[:, 0:ncol], lhsT=kT[s][:, j * 128:(j + 1) * 128], rhs=qT[s][:, q0:q1], start=True, stop=(not diag)),
                                    reads=[b_in_[s]], writes=[pb], inc=(not diag))
                                if diag:
                                    P.op("pe", lambda e: e.matmul(pt[:, 0:128], lhsT=ident_f[:], rhs=maskT[:], start=False, stop=True),
                                         reads=[b_const], writes=[pb], inc=True)
                                if pending is not None:
                                    pending()
                                p3 = pi % 3
                                pi += 1
                                P.op("dve", lambda e: e.scalar_tensor_tensor(
                                    out=ftmp[p3][:, 0:ncol], in0=pt[:, 0:ncol], scalar=scale, in1=Fb[fb][:, lo:T], op0=ALU.mult, op1=ALU.add),
                                    reads=[pb, b_Fb[fb]], writes=[b_ftmp[p3]])
                                P.op("act", lambda e: e.activation(
                                    out=PT[p3][:, 0:ncol], in_=ftmp[p3][:, 0:ncol], func=AF.Exp, bias=FNT[:, j, bh:bh + 1]),
                                    reads=[b_ftmp[p3], bt], writes=[b_PT[p3]])
                                def pv(j=j, p3=p3, lo=lo, ncol=ncol, s=s, nkb=nkb):
                                    P.op("pe", lambda e: e.matmul(
                                        pO[:, lo:T], lhsT=Vt[s][:, j, :], rhs=PT[p3][:, 0:ncol], start=(j == 0), stop=(j == nkb - 1)),
                                        reads=[b_PT[p3], b_in_[s]], writes=[bO], inc=(j == nkb - 1))
                                    P.op("pe", lambda e: e.matmul(
                                        pD[:, lo:T], lhsT=ones_b[:], rhs=PT[p3][:, 0:ncol], start=(j == 0), stop=(j == nkb - 1)),
                                        reads=[b_PT[p3], b_const], writes=[bD], inc=(j == nkb - 1))
                                pending = pv
                            pending()
                            P.op("dve", lambda e: e.reciprocal(out=rden[:], in_=pD[:]), reads=[bD], writes=[b_rden])
                            o = oi % 2
                            oi += 1
                            P.op("dve", lambda e: e.tensor_tensor(out=yo[o][:], in0=pO[:], in1=rden[:], op=ALU.mult),
                                 reads=[bO, b_rden], writes=[b_yo[o]])
                            P.dma("sp", YF[h * 128:(h + 1) * 128, b * S + gq * T:b * S + (gq + 1) * T], yo[o][:],
                                  reads=[b_yo[o]], nowaw=[b_y])
                P.barrier()

            def chunk_branch(kind):
                H = HM if kind == "m" else HR
                QT_src, KT_src = (QKM, QKM) if kind == "m" else (QR, KR)
                koff = MW if kind == "m" else 0
                Vsrc = VM if kind == "m" else VR
                Gsrc = OM if kind == "m" else GR
                NWsrc = mnw_b if kind == "m" else rnw_b
                Ydst = YM if kind == "m" else YR
                VW = 257 if kind == "m" else 256
                HG = min(2, H)
                with ExitStack() as es2:
                    nwb = sb(es2, "nwb", [128, H * 256], F32)
                    b_nwb = Buf()
                    P.dma("sp", nwb[:], NWsrc[:, l, :], writes=[b_nwb])
                    if kind == "r":
                        intra = sb(es2, "intra", [128, HR, 128], F32)
                        qdec = sb(es2, "qdec", [128, HR, 128], F32)
                        kdec = sb(es2, "kdec", [128, HR], F32)
                        for d_, s_ in ((intra, intra_in), (qdec, qdec_in), (kdec, kdec_in)):
                            P.dma("sp", d_[:], s_, writes=[b_nwb])
                    qT = [sb(es2, "cq%d" % h, [128, 2, S], BF16) for h in range(HG)]
                    kT = [sb(es2, "ck%d" % h, [128, 2, S], BF16) for h in range(HG)]
                    Va = [sb(es2, "cv%d" % h, [128, CPS, VW], BF16) for h in range(HG)]
                    Gt = [sb(es2, "cg%d" % h, [128, CPS, 256], BF16) for h in range(HG)]
                    Cst = [sb(es2, "cC%d" % h, [128, 2, VW], F32) for h in range(HG)]
                    Cbf = [sb(es2, "cCb%d" % h, [128, 2, VW], BF16) for h in range(HG)]
                    b_ld = [Buf() for _ in range(HG)]
                    b_C = [Buf() for _ in range(HG)]
                    b_Cb = [Buf() for _ in range(HG)]
                    NR = 4
                    wT = [sb(es2, "cw%d" % i, [128, 128], BF16) for i in range(NR)]
                    Dt = [sb(es2, "cD%d" % i, [128, 128], F32) for i in range(NR)]
                    qd = [sb(es2, "cqd%d" % i, [128, 2, 128], BF16) for i in range(NR)]
                    kw = [sb(es2, "ckw%d" % i, [128, 256], BF16) for i in range(NR)]
                    o1 = [sb(es2, "co1%d" % i, [128, VW], F32) for i in range(NR)]
                    na = [sb(es2, "cna%d" % i, [128, VW], F32) for i in range(NR)]
                    hh = [sb(es2, "chh%d" % i, [128, 256], F32) for i in range(NR)]
                    yb_ = [sb(es2, "cyb%d" % i, [128, 256], BF16) for i in range(NR)]
                    st6 = [sb(es2, "cst%d" % i, [128, 8], F32) for i in range(NR)]
                    mv = [sb(es2, "cmv%d" % i, [128, 4], F32) for i in range(NR)]
                    sc1 = [sb(es2, "csc%d" % i, [128, 4], F32) for i in range(NR)]
                    yT = [sb(es2, "cyT%d" % h, [128, 2, T], BF16) for h in range(HG)]
                    b_r = [Buf() for _ in range(NR)]
                    b_yT = [Buf() for _ in range(HG)]
                    ri = 0
                    for b in range(NB):
                        sq_ = slice(b * S, (b + 1) * S)
                        for h0 in range(0, H, HG):
                            for hi in range(HG):
                                h = h0 + hi
                                P.dma("sp", qT[hi][:], QT_src[h * 256:(h + 1) * 256, sq_].rearrange("(c p) t -> p c t", p=128),
                                      reads=[b_scr], writes=[b_ld[hi]])
                                P.dma("sp", kT[hi][:], KT_src[koff + h * 256:koff + (h + 1) * 256, sq_].rearrange("(c p) t -> p c t", p=128),
                                      reads=[b_scr], writes=[b_ld[hi]])
                                P.dma("sp", Va[hi][:, :, 0:256], Vsrc[sq_, h * 256:(h + 1) * 256].rearrange("(c p) e -> p c e", p=128),
                                      reads=[b_scr], writes=[b_ld[hi]])
                                if kind == "m":
                                    P.op("dve", lambda e: e.memset(Va[hi][:, :, 256:257], 1.0), writes=[b_ld[hi]])
                                P.dma("sp", Gt[hi][:], Gsrc[sq_, h * 256:(h + 1) * 256].rearrange("(c p) e -> p c e", p=128),
                                      reads=[b_scr], writes=[b_ld[hi]])
                                P.op("dve", lambda e: e.memset(Cst[hi][:], 0.0), writes=[b_C[hi]])
                                P.op("dve", lambda e: e.memset(Cbf[hi][:], 0.0), writes=[b_Cb[hi]])
                            for c in range(CPS):
                                cl = slice(c * 128, (c + 1) * 128)
                                for hi in range(HG):
                                    h = h0 + hi
                                    bh = b * H + h
                                    r = ri % NR
                                    ri += 1
                                    R = [b_r[r]]
                                    pS, bS = ps()
                                    for dc in (0, 1):
                                        P.op("pe", lambda e: e.matmul(
                                            pS[:, 0:128], lhsT=kT[hi][:, dc, cl], rhs=qT[hi][:, dc, cl], start=(dc == 0), stop=(dc == 1)),
                                            reads=[b_ld[hi]], writes=[bS], inc=(dc == 1))
                                    if kind == "m":
                                        pDm, bDm = ps()
                                        P.op("pe", lambda e: e.matmul(
                                            pDm[:, 0:128], lhsT=sel[0:RM, bh, :], rhs=GMN[:, cl], start=True, stop=False),
                                            reads=[br, b_const], writes=[bDm], inc=False)
                                        P.op("pe", lambda e: e.matmul(pDm[:, 0:128], lhsT=ident_f[:], rhs=maskT[:], start=False, stop=True),
                                             reads=[b_const], writes=[bDm], inc=True)
                                        P.op("act", lambda e: e.activation(
                                            out=Dt[r][:], in_=pDm[:, 0:128], func=AF.Exp, bias=MS[:, c, 0, bh:bh + 1]),
                                            reads=[bDm, bt], writes=R)
                                        P.op("dve", lambda e: e.scalar_tensor_tensor(
                                            out=wT[r][:], in0=pS[:, 0:128], scalar=0.0625, in1=Dt[r][:], op0=ALU.mult, op1=ALU.mult),
                                            reads=[bS] + R, writes=R)
                                    else:
                                        P.op("dve", lambda e: e.tensor_tensor(
                                            out=wT[r][:], in0=pS[:, 0:128], in1=intra[:, h, :], op=ALU.mult),
                                            reads=[bS, b_nwb], writes=R)
                                        for dc in (0, 1):
                                            P.op("pool", lambda e: e.tensor_tensor(
                                                out=qd[r][:, dc, :], in0=qT[hi][:, dc, cl], in1=qdec[:, h, :], op=ALU.mult),
                                                reads=[b_ld[hi], b_nwb], writes=R)
                                    pO_, bO_ = ps()
                                    if kind == "m":
                                        P.op("pe", lambda e: e.matmul(
                                            pO_[:, 0:VW], lhsT=wT[r][:], rhs=Va[hi][:, c, :], start=True, stop=True),
                                            reads=R + [b_ld[hi]], writes=[bO_], inc=True)
                                        pI, bI = ps()
                                        for dc in (0, 1):
                                            P.op("pe", lambda e: e.matmul(
                                                pI[:, 0:VW], lhsT=qT[hi][:, dc, cl], rhs=Cbf[hi][:, dc, :], start=(dc == 0), stop=(dc == 1)),
                                                reads=[b_ld[hi], b_Cb[hi]], writes=[bI], inc=(dc == 1))
                                        P.op("act", lambda e: e.copy(out=o1[r][:], in_=pO_[:, 0:VW]), reads=[bO_], writes=R)
                                        P.op("dve", lambda e: e.scalar_tensor_tensor(
                                            out=na[r][:], in0=pI[:, 0:VW], scalar=MS[:, c, 1, bh:bh + 1], in1=o1[r][:], op0=ALU.mult, op1=ALU.add),
                                            reads=[bI, bt] + R, writes=R)
                                        P.op("dve", lambda e: e.tensor_scalar(
                                            out=sc1[r][:, 2:3], in0=na[r][:, 256:257], scalar1=-1.0, scalar2=None, op0=ALU.mult),
                                            reads=R, writes=R)
                                        P.op("dve", lambda e: e.tensor_tensor(
                                            out=sc1[r][:, 0:1], in0=na[r][:, 256:257], in1=sc1[r][:, 2:3], op=ALU.max), reads=R, writes=R)
                                        P.op("dve", lambda e: e.tensor_scalar(
                                            out=sc1[r][:, 0:1], in0=sc1[r][:, 0:1], scalar1=MS[:, c, 2, bh:bh + 1], scalar2=None, op0=ALU.max),
                                            reads=R + [bt], writes=R)
                                        P.op("dve", lambda e: e.reciprocal(out=sc1[r][:, 1:2], in_=sc1[r][:, 0:1]), reads=R, writes=R)
                                        P.op("dve", lambda e: e.tensor_scalar(out=hh[r][:], in0=na[r][:, 0:256], scalar1=sc1[r][:, 1:2],
                                                                              scalar2=None, op0=ALU.mult), reads=R, writes=R)
                                    else:
                                        P.op("pe", lambda e: e.matmul(
                                            pO_[:, 0:256], lhsT=wT[r][:], rhs=Va[hi][:, c, :], start=True, stop=False),
                                            reads=R + [b_ld[hi]], writes=[bO_], inc=False)
                                        for dc in (0, 1):
                                            P.op("pe", lambda e: e.matmul(
                                                pO_[:, 0:256], lhsT=qd[r][:, dc, :], rhs=Cbf[hi][:, dc, :], start=False, stop=(dc == 1)),
                                                reads=R + [b_Cb[hi]], writes=[bO_], inc=(dc == 1))
                                        P.op("act", lambda e: e.copy(out=hh[r][:], in_=pO_[:, 0:256]), reads=[bO_], writes=R)
                                    P.op("dve", lambda e: e.bn_stats(out=st6[r][:, 0:6], in_=hh[r][:]), reads=R, writes=R)
                                    P.op("dve", lambda e: e.bn_aggr(out=mv[r][:, 0:2], in_=st6[r][:, 0:6]), reads=R, writes=R)
                                    P.op("act", lambda e: e.activation(out=mv[r][:, 2:3], in_=mv[r][:, 1:2], func=AF.Sqrt, bias=cst[:, 1:2]),
                                         reads=R + [b_const], writes=R)
                                    P.op("dve", lambda e: e.reciprocal(out=mv[r][:, 2:3], in_=mv[r][:, 2:3]), reads=R, writes=R)
                                    P.op("dve", lambda e: e.tensor_scalar(out=hh[r][:], in0=hh[r][:], scalar1=mv[r][:, 0:1],
                                                                          scalar2=mv[r][:, 2:3], op0=ALU.subtract, op1=ALU.mult),
                                         reads=R, writes=R)
                                    P.op("pool", lambda e: e.tensor_tensor(out=hh[r][:], in0=hh[r][:], in1=nwb[:, h * 256:(h + 1) * 256],
                                                                            op=ALU.mult), reads=R + [b_nwb], writes=R)
                                    P.op("dve", lambda e: e.tensor_tensor(out=yb_[r][:], in0=hh[r][:], in1=Gt[hi][:, c, :], op=ALU.mult),
                                         reads=R + [b_ld[hi]], writes=R)
                                    tq = c % 4
                                    for dc in (0, 1):
                                        pY, bY = ps()
                                        P.op("pe", lambda e: e.matmul(
                                            pY[:, 0:128], lhsT=yb_[r][:, dc * 128:(dc + 1) * 128], rhs=ident_b[:], start=True, stop=True),
                                            reads=R + [b_const], writes=[bY], inc=True)
                                        P.op("act", lambda e: e.copy(out=yT[hi][:, dc, tq * 128:(tq + 1) * 128], in_=pY[:, 0:128]),
                                             reads=[bY], writes=[b_yT[hi]])
                                    if tq == 3:
                                        t0_ = b * S + (c - 3) * 128
                                        P.dma("sp", Ydst[h * 256:(h + 1) * 256, t0_:t0_ + T].rearrange("(c p) t -> p c t", p=128), yT[hi][:],
                                              reads=[b_yT[hi]], nowaw=[b_y])
                                    pK, bK = ps()
                                    for dc in (0, 1):
                                        P.op("pe", lambda e: e.matmul(
                                            pK[:, dc * 128:(dc + 1) * 128], lhsT=kT[hi][:, dc, cl], rhs=ident_b[:], start=True, stop=True),
                                            reads=[b_ld[hi], b_const], writes=[bK], inc=(dc == 1))
                                    if kind == "m":
                                        P.op("dve", lambda e: e.tensor_scalar(
                                            out=kw[r][:], in0=pK[:, 0:256], scalar1=MS[:, c, 3, bh:bh + 1], scalar2=None, op0=ALU.mult),
                                            reads=[bK, bt], writes=R)
                                    else:
                                        P.op("dve", lambda e: e.tensor_scalar(
                                            out=kw[r][:], in0=pK[:, 0:256], scalar1=kdec[:, h:h + 1], scalar2=None, op0=ALU.mult),
                                            reads=[bK, b_nwb], writes=R)
                                    for dc in (0, 1):
                                        pC, bC = ps()
                                        P.op("pe", lambda e: e.matmul(
                                            pC[:, 0:VW], lhsT=kw[r][:, dc * 128:(dc + 1) * 128], rhs=Va[hi][:, c, :], start=True, stop=True),
                                            reads=R + [b_ld[hi]], writes=[bC], inc=True)
                                        if kind == "m":
                                            dscal = DECB[:, bh, c:c + 1]
                                            rd = [bt]
                                        else:
                                            dscal = float((1.0 - 2.0 ** (-5.0 - h)) ** 128)
                                            rd = []
                                        P.op("dve", lambda e: e.scalar_tensor_tensor(
                                            out=Cst[hi][:, dc, :], in0=Cst[hi][:, dc, :], scalar=dscal, in1=pC[:, 0:VW], op0=ALU.mult, op1=ALU.add),
                                            reads=[bC, b_C[hi]] + rd, writes=[b_C[hi]])
                                        P.op("act", lambda e: e.copy(out=Cbf[hi][:, dc, :], in_=Cst[hi][:, dc, :]),
                                             reads=[b_C[hi]], writes=[b_Cb[hi]])
                    P.barrier()

            chunk_branch("r")
            chunk_branch("m")

    PH = getattr(cfg, 'phases', (1, 2, 3, 4, 5))
    for l in range(L):
        with ExitStack() as es:
            g = alloc_gemm(es, KMAX)
            aT = sb(es, "aT", [128, FC, T], BF16)
            b_aT = Buf("aT")
            compute_mod(l, g)
            if 1 in PH:
                ffn(g, aT, b_aT, l, 0, ffn_w["ffn1_gate"], ffn_w["ffn1_up"], ffn_w["ffn1_down"])
            P.barrier()
        with ExitStack() as es:
            g = alloc_gemm(es, max(DC, MW // 128, FW // 128, RW // 128))
            zb = [sb(es, "zb%d" % i, [128, T + 3], F32) for i in range(2)]
            b_zb = [Buf() for _ in range(2)]
            halo = sb(es, "halo", [128, 2 * MW // 128, 3], F32)
            b_halo = Buf()
            brow = sb(es, "brow", [1, 5, max(MW, FW, RW)], BF16)
            b_brow = Buf()
            wsm = sb(es, "wsm", [128, DC, 3, 8], BF16)
            b_wsm = Buf()
            osm = [sb(es, "osm%d" % i, [8, T], F32) for i in range(3)]
            b_osm = [Buf() for _ in range(3)]
            cs = sb(es, "cs", [128, S], F32)
            sn = sb(es, "sn", [128, S], F32)
            P.dma("sp", cs[:], cos_in, writes=[b_const])
            P.dma("sp", sn[:], sin_in, writes=[b_const])
            if 2 in PH:
                mixer_in(g, l, zb, b_zb, halo, b_halo, brow, b_brow, wsm, b_wsm, osm, b_osm, cs, sn)
            P.barrier()
        if 3 in PH:
            seq_mixers(l)
        with ExitStack() as es:
            g = alloc_gemm(es, max(DC, MW // 128, FW // 128, RW // 128))
            yb = [sb(es, "yb%d" % i, [128, max(MW, FW, RW) // 128, T], BF16) for i in range(3)]
            b_yb = [Buf() for _ in range(3)]
            macc = sb(es, "macc", [128, DC, T], F32)
            b_macc = Buf()
            gt = [sb(es, "gt%d" % i, [128, T], BF16) for i in range(2)]
            b_gt = [Buf() for _ in range(2)]
            if 4 in PH:
                mixer_out(g, l, yb, b_yb, macc, b_macc, gt, b_gt)
            P.barrier()
        with ExitStack() as es:
            g = alloc_gemm(es, KMAX)
            aT = sb(es, "aT", [128, FC, T], BF16)
            b_aT = Buf("aT")
            if 5 in PH:
                ffn(g, aT, b_aT, l, 2, ffn_w["ffn2_gate"], ffn_w["ffn2_up"], ffn_w["ffn2_down"])
            P.barrier()

    with ExitStack() as es:
        xt = [sb(es, "fxt%d" % i, [128, DC, T], F32) for i in range(2)]
        b_xt = [Buf() for _ in range(2)]
        ot = [sb(es, "fot%d" % i, [128, 512], F32) for i in range(4)]
        b_ot = [Buf() for _ in range(4)]
        k = 0
        for tt in range(NT):
            s = tt % 2
            P.dma("sp", xt[s][:], XT[:, tok(tt)].rearrange("(c p) t -> p c t", p=128), reads=b_XT[tt], writes=[b_xt[s]])
            for tc in range(T // 128):
                for d0 in range(0, DC, 4):
                    nd = min(4, DC - d0)
                    pt, pb = ps()
                    for dd in range(nd):
                        P.op("pe", lambda e, pt=pt, s=s, tc=tc, d0=d0, dd=dd: e.matmul(
                            pt[:, dd * 128:(dd + 1) * 128], lhsT=xt[s][:, d0 + dd, tc * 128:(tc + 1) * 128], rhs=ident_f[:],
                            start=True, stop=True), reads=[b_xt[s], b_const], writes=[pb], inc=(dd == nd - 1))
                    o = k % 4
                    if k % 2 == 0:
                        P.op("act", lambda e, o=o, pt=pt, nd=nd: e.copy(out=ot[o][:, 0:nd * 128], in_=pt[:, 0:nd * 128]), reads=[pb], writes=[b_ot[o]])
                    else:
                        P.op("dve", lambda e, o=o, pt=pt, nd=nd: e.tensor_copy(out=ot[o][:, 0:nd * 128], in_=pt[:, 0:nd * 128]), reads=[pb], writes=[b_ot[o]])
                    r0 = tt * T + tc * 128
                    P.dma("sp", out[r0:r0 + 128, d0 * 128:(d0 + nd) * 128], ot[o][:, 0:nd * 128], reads=[b_ot[o]], nowaw=[b_out])
                    k += 1
    P.barrier()
    glob.close()
    P.close()
    return nc, P.n_instr


def host_inputs(cfg, inp, core):
    D, S, NB, L, HM, HF, HR = cfg.D, cfg.S, cfg.NB, cfg.L, cfg.HM, cfg.HF, cfg.HR
    f = np.float32
    m = {}
    xs = inp["x"][core * NB:(core + 1) * NB]
    m["x"] = np.ascontiguousarray(xs.reshape(NB * S, D))
    cc = inp["c"][core * NB:(core + 1) * NB]
    m["c_pc"] = np.ascontiguousarray(cc.reshape(NB, cfg.DC, 128).transpose(2, 1, 0))
    return m


def shared_inputs(cfg, inp):
    D, S, NB, L, HM, HF, HR, MW = cfg.D, cfg.S, cfg.NB, cfg.L, cfg.HM, cfg.HF, cfg.HR, cfg.MW
    f = np.float32
    m = {}
    for k in ("w_ada", "ffn1_gate", "ffn1_up", "ffn1_down", "ffn2_gate", "ffn2_up", "ffn2_down", "w_in", "b_in",
              "w_branch_m", "w_branch_f", "w_branch_r", "w_out"):
        m[k] = np.ascontiguousarray(inp[k], dtype=f)

    def pc(v):
        v = np.asarray(v, dtype=f)
        sh = v.shape[:-1]
        v = v.reshape(sh + (v.shape[-1] // 128, 128))
        return np.ascontiguousarray(np.moveaxis(v, -1, 0))
    m["bada_pc"] = pc(inp["b_ada"])
    m["nw_pc"] = pc(inp["norm_w"])
    parts = []
    for nm in cfg.frange:
        c0, w = cfg.col[nm]
        parts.append(pc(inp["b_in"][:, c0:c0 + w]))
    m["bin_pc"] = np.ascontiguousarray(np.concatenate(parts, axis=2))
    for key, nm in (("bsm_i", "m_i"), ("bsm_f", "m_f"), ("bsm_ff", "f_f")):
        c0, w = cfg.col[nm]
        m[key] = np.ascontiguousarray(np.asarray(inp["b_in"][:, c0:c0 + w], dtype=f).T)
    cw = np.asarray(inp["conv_w"], dtype=f)
    m["convw_pc"] = np.ascontiguousarray(np.moveaxis(pc(cw), 2, 3))
    m["convb_pc"] = pc(inp["conv_b"])
    m["mnw_b"] = np.ascontiguousarray(np.broadcast_to(np.asarray(inp["mlstm_norm_w"], dtype=f)[None], (128, L, MW)))
    m["rnw_b"] = np.ascontiguousarray(np.broadcast_to(np.asarray(inp["ret_norm_w"], dtype=f)[None], (128, L, cfg.RW)))
    m["ident"] = np.eye(128, dtype=f)
    s_ = np.arange(128)
    m["maskT"] = np.where(s_[:, None] <= s_[None, :], 0.0, NEG).astype(f)
    sel = np.zeros((16, 16, 128), f)
    for h in range(16):
        sel[h, h, :] = 1.0
    m["sel"] = sel
    lg = np.log1p(-(2.0 ** (-5.0 - np.arange(HR, dtype=np.float64))))
    rel = (s_[None, :] - s_[:, None]).astype(np.float64)
    intra = np.where(rel[:, None, :] >= 0, np.exp(np.maximum(rel, 0)[:, None, :] * lg[None, :, None]), 0.0) * (256.0 ** -0.5)
    m["intra"] = np.ascontiguousarray(intra.astype(f))
    qd = np.exp((s_[None, :] + 1.0) * lg[:, None])
    m["qdec"] = np.ascontiguousarray(np.broadcast_to(qd[None], (128, HR, 128)).astype(f))
    kd = np.exp((127.0 - s_[:, None]) * lg[None, :]) * (256.0 ** -0.5)
    m["kdec"] = np.ascontiguousarray(kd.astype(f))
    half = 128
    inv_freq = (10000.0 ** (-np.arange(half, dtype=f) / f(half))).astype(f)
    ang = (np.arange(S, dtype=f)[None, :] * inv_freq[:, None]).astype(f)
    m["cos"] = np.cos(ang.astype(np.float64)).astype(f)
    m["sin"] = np.sin(ang.astype(np.float64)).astype(f)
    return m


_CACHE = {}


def run(cfg, inp, ncores, trace=False):
    nc, n_instr = build_program(cfg)
    sh = shared_inputs(cfg, inp)
    in_maps = []
    for c in range(ncores):
        d = dict(sh)
        d.update(host_inputs(cfg, inp, c))
        in_maps.append(d)
    res = run_bass_kernel_spmd(nc, in_maps, core_ids=list(range(ncores)), trace=trace)
    outs = [r["out"].reshape(cfg.NB, cfg.S, cfg.D) for r in res.results]
    return np.concatenate(outs, axis=0), res, n_instr


def kernel(**inputs):
    cfg = mkcfg(True)
    inp = {k: np.asarray(v) for k, v in inputs.items()}
    out, _, _ = run(cfg, inp, 8)
    return out.astype(np.float32)
```

```python
import numpy as np
from contextlib import ExitStack
import concourse.bass as bass
import concourse.mybir as mybir
from concourse.bass_utils import run_bass_kernel_spmd

F32 = mybir.dt.float32
BF16 = mybir.dt.bfloat16
AF = mybir.ActivationFunctionType
ALU = mybir.AluOpType

COMPUTE = ("pe", "act", "dve", "pool")
ENGINES = ("pe", "act", "dve", "pool", "sp")


class Buf:
    __slots__ = ("name", "w", "r")

    def __init__(self, name=""):
        self.name = name
        self.w = {}
        self.r = {}


class Prog:
    def __init__(self, nc):
        self.nc = nc
        self.es = ExitStack()
        self.cnt = {e: 0 for e in COMPUTE}
        self.waited = {e: {} for e in ENGINES}
        self.sems = {}
        self.eng = {"pe": nc.tensor, "act": nc.scalar, "dve": nc.vector, "pool": nc.gpsimd, "sp": nc.sync}
        for e in COMPUTE:
            self.sems[e] = self.es.enter_context(nc.semaphore("s_" + e))
        self.dma_pool, self.dma_cnt, self.dma_rr = {}, {}, {}
        for q, n in (("sp", 16), ("act", 4), ("pool", 8)):
            ks = []
            for i in range(n):
                k = "d_%s_%d" % (q, i)
                self.sems[k] = self.es.enter_context(nc.semaphore(k))
                self.dma_cnt[k] = 0
                ks.append(k)
            self.dma_pool[q] = ks
            self.dma_rr[q] = 0
        self.n_instr = 0

    def _deps(self, eng, reads, writes, nowaw=()):
        deps = {}

        def add(ev):
            if deps.get(ev[0], 0) < ev[1]:
                deps[ev[0]] = ev[1]
        for b in reads:
            for kv in b.w.items():
                add(kv)
        for b in writes:
            for kv in b.w.items():
                add(kv)
            for kv in b.r.items():
                add(kv)
        for b in nowaw:
            for kv in b.r.items():
                add(kv)
        waits = []
        wd = self.waited[eng]
        for k, v in deps.items():
            if k == eng and (eng == "pe" or v > self.cnt[eng]):
                continue
            if wd.get(k, 0) >= v:
                continue
            wd[k] = v
            waits.append((k, v))
        return waits

    @staticmethod
    def _mark(ev, reads, writes, nowaw=()):
        k, v = ev
        for b in reads:
            if b.r.get(k, 0) < v:
                b.r[k] = v
        for b in writes:
            b.w = {k: v}
            b.r = {}
        for b in nowaw:
            if b.w.get(k, 0) < v:
                b.w[k] = v

    def _emit(self, eng, waits, fn, ev):
        e = self.eng[eng]
        for k, v in waits:
            e.wait_ge(self.sems[k], v)
        if fn is None:
            return
        ins = fn(e)
        if ev is not None:
            ins.then_inc(self.sems[ev[0]], 1 if ev[0] in COMPUTE else 16)
        self.n_instr += 1

    def op(self, eng, fn, reads=(), writes=(), inc=True):
        waits = self._deps(eng, reads, writes)
        ev = (eng, self.cnt[eng] + 1)
        if inc:
            self.cnt[eng] += 1
        self._mark(ev, reads, writes)
        self._emit(eng, waits, fn, ev if inc else None)
        return ev

    def dma(self, q, out, in_, reads=(), writes=(), nowaw=()):
        pool = self.dma_pool[q]
        k = pool[self.dma_rr[q] % len(pool)]
        self.dma_rr[q] += 1
        waits = self._deps(q, reads, writes, nowaw)
        prev = self.dma_cnt[k] * 16
        if prev > 0 and self.waited[q].get(k, 0) < prev:
            self.waited[q][k] = prev
            waits.append((k, prev))
        self.dma_cnt[k] += 1
        ev = (k, self.dma_cnt[k] * 16)
        self._mark(ev, reads, writes, nowaw)
        self._emit(q, waits, lambda e: e.dma_start(out=out, in_=in_), ev)
        return ev

    def barrier(self):
        evs = [(e, self.cnt[e]) for e in COMPUTE if self.cnt[e] > 0]
        evs += [(k, c * 16) for k, c in self.dma_cnt.items() if c > 0]
        for eng in ENGINES:
            waits = []
            for k, v in evs:
                if k == eng and eng == "pe":
                    continue
                if self.waited[eng].get(k, 0) >= v:
                    continue
                self.waited[eng][k] = v
                waits.append((k, v))
            self._emit(eng, waits, None, None)

    def close(self):
        self.es.close()


class Cfg:
    pass


def mkcfg(full=True):
    c = Cfg()
    if full is True:
        c.D, c.DFF, c.S, c.NB, c.HM, c.HF, c.HR, c.L = 2048, 5632, 2048, 2, 4, 8, 4, 4
    elif full == "medium":
        c.D, c.DFF, c.S, c.NB, c.HM, c.HF, c.HR, c.L = 256, 768, 1024, 2, 4, 8, 4, 2
    else:
        c.D, c.DFF, c.S, c.NB, c.HM, c.HF, c.HR, c.L = 256, 512, 1024, 1, 1, 2, 1, 2
    c.T = 512
    c.N = c.NB * c.S
    c.DC, c.FC = c.D // 128, c.DFF // 128
    c.NT = c.N // c.T
    c.TPS = c.S // c.T
    c.MW, c.FW, c.RW = c.HM * 256, c.HF * 128, c.HR * 256
    c.NCH = c.N // 128
    c.CPS = c.S // 128
    widths = (2 * c.MW, c.MW, c.MW, c.HM, c.HM, c.FW, c.FW, c.FW, c.HF, c.RW, c.RW, c.RW, c.RW, c.D, c.D, c.D)
    names = ("m_qk", "m_v", "m_o", "m_i", "m_f", "f_q", "f_k", "f_v", "f_f", "r_q", "r_k", "r_v", "r_g", "g_m", "g_f", "g_r")
    st = np.cumsum((0,) + widths[:-1])
    c.col = {n: (int(s), int(w)) for n, s, w in zip(names, st, widths)}
    c.INC = int(sum(widths))
    c.frange = ("m_qk", "f_q", "f_k", "r_q", "r_k", "g_m", "g_f", "g_r")
    c.foff = {}
    o = 0
    for n in c.frange:
        c.foff[n] = o
        o += c.col[n][1] // 128
    c.NCF = o
    c.krange = ("m_v", "m_o", "f_v", "r_v", "r_g")
    return c


EPS = 1e-6
NEG = -60000.0


def build_program(cfg):
    D, DFF, S, NB, HM, HF, HR, L, T, N = cfg.D, cfg.DFF, cfg.S, cfg.NB, cfg.HM, cfg.HF, cfg.HR, cfg.L, cfg.T, cfg.N
    DC, FC, NT, TPS, MW, FW, RW, NCH, CPS = cfg.DC, cfg.FC, cfg.NT, cfg.TPS, cfg.MW, cfg.FW, cfg.RW, cfg.NCH, cfg.CPS
    KMAX = max(FC, 2 * DC)
    nc = bass.Bass("TRN2", target_bir_lowering=False)

    def din(name, shape, dt=F32):
        return nc.dram_tensor(name, list(shape), dt, kind="ExternalInput").ap()

    def dscr(name, shape, dt):
        return nc.dram_tensor(name, list(shape), dt).ap()

    x_in = din("x", [N, D])
    c_pc = din("c_pc", [128, DC, NB])
    w_ada = din("w_ada", [L, D, 9 * D])
    bada_pc = din("bada_pc", [128, L, 9 * DC])
    nw_pc = din("nw_pc", [128, L, 6, DC])
    ffn_w = {}
    for nm in ("ffn1_gate", "ffn1_up", "ffn2_gate", "ffn2_up"):
        ffn_w[nm] = din(nm, [L, D, DFF])
    for nm in ("ffn1_down", "ffn2_down"):
        ffn_w[nm] = din(nm, [L, DFF, D])
    w_in = din("w_in", [L, D, cfg.INC])
    b_in = din("b_in", [L, cfg.INC])
    bin_pc = din("bin_pc", [128, L, cfg.NCF])
    bsm_i = din("bsm_i", [HM, L])
    bsm_f = din("bsm_f", [HM, L])
    bsm_ff = din("bsm_ff", [HF, L])
    convw_pc = din("convw_pc", [128, L, 2 * MW // 128, 4])
    convb_pc = din("convb_pc", [128, L, 2 * MW // 128])
    mnw_b = din("mnw_b", [128, L, MW])
    rnw_b = din("rnw_b", [128, L, RW])
    w_bm = din("w_branch_m", [L, MW, D])
    w_bf = din("w_branch_f", [L, FW, D])
    w_br = din("w_branch_r", [L, RW, D])
    w_out = din("w_out", [L, D, D])
    ident_in = din("ident", [128, 128])
    maskT_in = din("maskT", [128, 128])
    sel_in = din("sel", [16, 16, 128])
    intra_in = din("intra", [128, HR, 128])
    qdec_in = din("qdec", [128, HR, 128])
    kdec_in = din("kdec", [128, HR])
    cos_in = din("cos", [128, S])
    sin_in = din("sin", [128, S])
    out = nc.dram_tensor("out", [N, D], F32, kind="ExternalOutput").ap()

    XT = dscr("XT", [D, N], F32)
    QKM = dscr("QKM", [2 * MW, N], BF16)
    VM = dscr("VM", [N, MW], BF16)
    OM = dscr("OM", [N, MW], BF16)
    G_I = dscr("G_I", [HM, N], F32)
    G_F = dscr("G_F", [HM, N], F32)
    G_FF = dscr("G_FF", [HF, N], F32)
    QF = dscr("QF", [FW, N], BF16)
    KF = dscr("KF", [FW, N], BF16)
    VF = dscr("VF", [N, FW], BF16)
    QR = dscr("QR", [RW, N], BF16)
    KR = dscr("KR", [RW, N], BF16)
    VR = dscr("VR", [N, RW], BF16)
    GR = dscr("GR", [N, RW], BF16)
    GATES = dscr("GATES", [3 * D, N], BF16)
    YM = dscr("YM", [MW, N], BF16)
    YF = dscr("YF", [FW, N], BF16)
    YR = dscr("YR", [RW, N], BF16)

    P = Prog(nc)
    glob = ExitStack()
    glob.enter_context(nc.allow_non_contiguous_dma(reason="tiny strided gate/bias loads"))

    uid = [0]

    def sb(es, name, shape, dt):
        uid[0] += 1
        return es.enter_context(nc.sbuf_tensor("s%d_%s" % (uid[0], name), list(shape), dt))

    banks = [glob.enter_context(nc.psum_tensor("pb%d" % i, [128, 512], F32)) for i in range(8)]
    bbuf = [Buf("pb%d" % i) for i in range(8)]
    ring = [0]
    ring_n = [5]

    def ps():
        i = ring[0] % ring_n[0]
        ring[0] += 1
        return banks[i], bbuf[i]

    ident_f = sb(glob, "ident_f", [128, 128], F32)
    ident_b = sb(glob, "ident_b", [128, 128], BF16)
    ones_b = sb(glob, "ones_b", [128, 128], BF16)
    cst = sb(glob, "cst", [128, 4], F32)
    maskT = sb(glob, "maskT", [128, 128], F32)
    sel = sb(glob, "sel", [16, 16, 128], F32)
    MOD = sb(glob, "MOD", [128, 9 * DC, NB], F32)
    SCE = sb(glob, "SCE", [128, 3, DC, NB], F32)
    GTE = sb(glob, "GTE", [128, 3, DC, NB], F32)
    badap = sb(glob, "badap", [128, L, 9 * DC], F32)
    nwp = sb(glob, "nwp", [128, L, 6, DC], F32)
    binp = sb(glob, "binp", [128, L, cfg.NCF], F32)
    bsi = sb(glob, "bsi", [HM, L], F32)
    bsf = sb(glob, "bsf", [HM, L], F32)
    bsff = sb(glob, "bsff", [HF, L], F32)
    cwp = sb(glob, "cwp", [128, L, 2 * MW // 128, 4], F32)
    cbp = sb(glob, "cbp", [128, L, 2 * MW // 128], F32)
    condT = sb(glob, "condT", [128, DC, NB], BF16)
    c32 = sb(glob, "c32", [128, DC, NB], F32)
    b_const = Buf("const")
    b_mod = Buf("mod")
    for dst, src in ((ident_f, ident_in), (maskT, maskT_in), (sel, sel_in), (badap, bada_pc), (nwp, nw_pc),
                     (binp, bin_pc), (bsi, bsm_i), (bsf, bsm_f), (bsff, bsm_ff), (cwp, convw_pc), (cbp, convb_pc),
                     (c32, c_pc)):
        P.dma("sp", dst[:], src, writes=[b_const])
    P.op("dve", lambda e: e.tensor_copy(out=ident_b[:], in_=ident_f[:]), reads=[b_const], writes=[b_const])
    P.op("dve", lambda e: e.memset(ones_b[:], 1.0), writes=[b_const])
    P.op("dve", lambda e: e.memset(cst[:, 0:1], EPS * D), writes=[b_const])
    P.op("dve", lambda e: e.memset(cst[:, 1:2], EPS), writes=[b_const])
    P.op("dve", lambda e: e.memset(cst[:, 2:3], 1.0), writes=[b_const])
    P.op("act", lambda e: e.activation(out=condT[:], in_=c32[:], func=AF.Silu), reads=[b_const], writes=[b_const])

    b_XT = [[Buf("XT%d_%d" % (i, c)) for c in range(DC)] for i in range(NT)]
    b_scr = Buf("scr")
    b_y = Buf("ybranch")
    b_out = Buf("out")

    def tok(tt):
        return slice(tt * T, (tt + 1) * T)

    with ExitStack() as es:
        xin = [sb(es, "xin%d" % i, [128, T // 128, D], F32) for i in range(2)]
        xo = [sb(es, "xo%d" % i, [128, T], F32) for i in range(3)]
        b_xin = [Buf() for _ in range(2)]
        b_xo = [Buf() for _ in range(3)]
        k = 0
        for tt in range(NT):
            s = tt % 2
            P.dma("sp", xin[s][:], x_in[tok(tt), :].rearrange("(c p) d -> p c d", p=128), writes=[b_xin[s]])
            for dc in range(DC):
                pt, pb = ps()
                for tc in range(T // 128):
                    P.op("pe", lambda e, pt=pt, s=s, tc=tc, dc=dc: e.matmul(
                        pt[:, tc * 128:(tc + 1) * 128], lhsT=xin[s][:, tc, dc * 128:(dc + 1) * 128], rhs=ident_f[:],
                        start=True, stop=True), reads=[b_xin[s], b_const], writes=[pb], inc=(tc == T // 128 - 1))
                o = k % 3
                eng = "act" if k % 2 == 0 else "dve"
                if eng == "act":
                    P.op("act", lambda e, o=o, pt=pt: e.copy(out=xo[o][:], in_=pt[:]), reads=[pb], writes=[b_xo[o]])
                else:
                    P.op("dve", lambda e, o=o, pt=pt: e.tensor_copy(out=xo[o][:], in_=pt[:]), reads=[pb], writes=[b_xo[o]])
                P.dma("sp", XT[dc * 128:(dc + 1) * 128, tok(tt)], xo[o][:], reads=[b_xo[o]], writes=[b_XT[tt][dc]])
                k += 1
        P.barrier()

    class G:
        pass

    def alloc_gemm(es, kmax):
        g = G()
        g.xy = sb(es, "xy", [128, DC, T], F32)
        g.hT = sb(es, "hT", [128, DC, T], BF16)
        g.wb = [sb(es, "wb%d" % i, [128, kmax, 256], BF16) for i in range(3)]
        g.b_wb = [[Buf("wb%d_%d" % (i, p_)) for p_ in range(8)] for i in range(3)]
        g.wi = [0]
        g.sq = [sb(es, "sq%d" % i, [128, T], BF16) for i in range(2)]
        g.b_sq = [Buf() for _ in range(2)]
        g.tmp = [sb(es, "tmp%d" % i, [128, T], F32) for i in range(3)]
        g.b_tmp = [Buf() for _ in range(3)]
        g.ti = [0]
        g.rstd = sb(es, "rstd", [128, T], F32)
        g.b_rstd = Buf()
        g.xr = [sb(es, "xr%d" % i, [128, T], F32) for i in range(3)]
        g.b_xr = [Buf() for _ in range(3)]
        g.xo = [sb(es, "xo%d" % i, [128, T], F32) for i in range(3)]
        g.b_xo = [Buf() for _ in range(3)]
        g.ob = [sb(es, "ob%d" % i, [128, T], BF16) for i in range(4)]
        g.b_ob = [Buf() for _ in range(4)]
        g.oi = [0]
        g.b_xy = Buf("xy")
        g.b_hT = Buf("hT")
        return g

    def wslot(g):
        s = g.wi[0] % 3
        g.wi[0] += 1
        return s

    def tmpslot(g):
        s = g.ti[0] % 3
        g.ti[0] += 1
        return s

    def obslot(g):
        s = g.oi[0] % 4
        g.oi[0] += 1
        return s

    def load_w(g, s, W2d, k0, kc_n, c0, ncols, kdst=0):
        for a in range(0, kc_n, 8):
            n = min(8, kc_n - a)
            src = W2d[(k0 + a) * 128:(k0 + a + n) * 128, c0:c0 + ncols].rearrange("(kc p) n -> p kc n", p=128)
            P.dma("pool", g.wb[s][:, kdst + a:kdst + a + n, 0:ncols], src, writes=[g.b_wb[s][(kdst + a) // 8]])

    def sumsq_rstd(g, src_fn, nchunks, rbuf):
        pt, pb = ps()
        for c in range(nchunks):
            q = c % 2
            P.op("act", lambda e, q=q, c=c: e.activation(out=g.sq[q][:], in_=src_fn(c), func=AF.Square),
                 reads=[rbuf], writes=[g.b_sq[q]])
            P.op("pe", lambda e, q=q, c=c, pt=pt: e.matmul(pt[:], lhsT=ones_b[:], rhs=g.sq[q][:], start=(c == 0),
                                                         stop=(c == nchunks - 1)),
                 reads=[g.b_sq[q], b_const], writes=[pb], inc=True)
        P.op("act", lambda e, pt=pt: e.activation(out=g.rstd[:], in_=pt[:], func=AF.Sqrt, bias=cst[:, 0:1]),
             reads=[pb, b_const], writes=[g.b_rstd])
        P.op("dve", lambda e: e.reciprocal(out=g.rstd[:], in_=g.rstd[:]), reads=[g.b_rstd], writes=[g.b_rstd])

    def load_norm(g, tt, j):
        b = tt // TPS
        P.dma("sp", g.xy[:], XT[:, tok(tt)].rearrange("(c p) t -> p c t", p=128), reads=b_XT[tt], writes=[g.b_xy])
        import os
        LNP = os.environ.get('LN_PARTS', 'sa')
        if 's' in LNP:
            sumsq_rstd(g, lambda c: g.xy[:, c, :], DC, g.b_xy)
        for c in range(DC if 'a' in LNP else 0):
            s = tmpslot(g)
            P.op("dve", lambda e, s=s, c=c: e.tensor_tensor(out=g.tmp[s][:], in0=g.xy[:, c, :], in1=g.rstd[:], op=ALU.mult),
                 reads=[g.b_xy, g.b_rstd], writes=[g.b_tmp[s]])
            P.op("act", lambda e, s=s, c=c: e.activation(out=g.hT[:, c, :], in_=g.tmp[s][:], func=AF.Identity,
                                                          scale=SCE[:, j, c, b:b + 1], bias=MOD[:, (3 * j) * DC + c, b:b + 1]),
                 reads=[g.b_tmp[s], b_mod], writes=[g.b_hT])

    def post_res(g, tt, j):
        b = tt // TPS
        sumsq_rstd(g, lambda c: g.xy[:, c, :], DC, g.b_xy)
        for c in range(DC):
            r = c % 3
            P.dma("sp", g.xr[r][:], XT[c * 128:(c + 1) * 128, tok(tt)], reads=[b_XT[tt][c]], writes=[g.b_xr[r]])
            s = tmpslot(g)
            P.op("dve", lambda e, s=s, c=c: e.tensor_tensor(out=g.tmp[s][:], in0=g.xy[:, c, :], in1=g.rstd[:], op=ALU.mult),
                 reads=[g.b_xy, g.b_rstd], writes=[g.b_tmp[s]])
            P.op("dve", lambda e, s=s, c=c, r=r: e.scalar_tensor_tensor(
                out=g.xo[r][:], in0=g.tmp[s][:], scalar=GTE[:, j, c, b:b + 1], in1=g.xr[r][:], op0=ALU.mult, op1=ALU.add),
                reads=[g.b_tmp[s], g.b_xr[r], b_mod], writes=[g.b_xo[r]])
            P.dma("sp", XT[c * 128:(c + 1) * 128, tok(tt)], g.xo[r][:], reads=[g.b_xo[r]], writes=[b_XT[tt][c]])

    def gemm_F(g, W2d, c0, ncols, KCn, rhs_fn, rbufs, epi, M=128):
        for t0 in range(0, ncols, 256):
            nw = min(256, ncols - t0)
            s = wslot(g)
            load_w(g, s, W2d, 0, KCn, c0 + t0, nw)
            for jn in range(0, nw, 128):
                m = min(M, nw - jn)
                pt, pb = ps()
                for kc in range(KCn):
                    P.op("pe", lambda e, pt=pt, s=s, kc=kc, jn=jn, m=m: e.matmul(
                        pt[0:m, :], lhsT=g.wb[s][:, kc, jn:jn + m], rhs=rhs_fn(kc), start=(kc == 0), stop=(kc == KCn - 1)),
                        reads=[g.b_wb[s][kc // 8]] + rbufs, writes=[pb], inc=(kc == KCn - 1))
                epi((t0 + jn) // 128, pt, pb)

    def compute_mod(l, g):
        pt, pb = banks[5], bbuf[5]
        ncols = 9 * D
        for t0 in range(0, ncols, 256):
            s = wslot(g)
            load_w(g, s, w_ada[l], 0, DC, t0, 256)
            for jn in (0, 128):
                ci = (t0 + jn) // 128
                for kc in range(DC):
                    P.op("pe", lambda e, s=s, kc=kc, jn=jn, ci=ci: e.matmul(
                        pt[:, ci * NB:(ci + 1) * NB], lhsT=g.wb[s][:, kc, jn:jn + 128], rhs=condT[:, kc, :],
                        start=(kc == 0), stop=(kc == DC - 1)),
                        reads=[g.b_wb[s][kc // 8], b_const], writes=[pb], inc=(kc == DC - 1 and jn == 128))
        pv = pt[:, 0:9 * DC * NB].rearrange("p (c b) -> p c b", b=NB)
        for b in range(NB):
            P.op("dve", lambda e, b=b: e.tensor_tensor(out=MOD[:, :, b], in0=pv[:, :, b], in1=badap[:, l, :], op=ALU.add),
                 reads=[pb, b_const], writes=[b_mod])
        sqD = float(np.sqrt(D))
        for j in range(3):
            step = 1.0 if j == 1 else 0.5
            for b in range(NB):
                P.op("dve", lambda e, j=j, b=b: e.scalar_tensor_tensor(
                    out=SCE[:, j, :, b], in0=MOD[:, (3 * j + 1) * DC:(3 * j + 2) * DC, b], scalar=1.0,
                    in1=nwp[:, l, 2 * j, :], op0=ALU.add, op1=ALU.mult), reads=[b_mod, b_const], writes=[b_mod])
                P.op("dve", lambda e, j=j, b=b: e.tensor_scalar(
                    out=SCE[:, j, :, b], in0=SCE[:, j, :, b], scalar1=sqD, scalar2=None, op0=ALU.mult),
                    reads=[b_mod], writes=[b_mod])
                P.op("dve", lambda e, j=j, b=b, step=step: e.scalar_tensor_tensor(
                    out=GTE[:, j, :, b], in0=MOD[:, (3 * j + 2) * DC:(3 * j + 3) * DC, b], scalar=step * sqD,
                    in1=nwp[:, l, 2 * j + 1, :], op0=ALU.mult, op1=ALU.mult), reads=[b_mod, b_const], writes=[b_mod])

    def ffn(g, aT, b_aT, l, j, Wg, Wu, Wd):
        import os
        PARTS = os.environ.get('FFN_PARTS', 'ngdp')
        for tt in range(int(os.environ.get('FFN_NT', NT))):
            if 'n' in PARTS:
                load_norm(g, tt, j)
            for ft in range(DFF // 256 if 'g' in PARTS else 0):
                s = wslot(g)
                load_w(g, s, Wg[l], 0, DC, ft * 256, 256, kdst=0)
                load_w(g, s, Wu[l], 0, DC, ft * 256, 256, kdst=DC)
                for jn in (0, 128):
                    pa, ba = ps()
                    pu, bu = ps()
                    for (pt, pb, ko) in ((pa, ba, 0), (pu, bu, DC)):
                        for kc in range(DC):
                            P.op("pe", lambda e, pt=pt, s=s, kc=kc, ko=ko, jn=jn: e.matmul(
                                pt[:], lhsT=g.wb[s][:, ko + kc, jn:jn + 128], rhs=g.hT[:, kc, :],
                                start=(kc == 0), stop=(kc == DC - 1)),
                                reads=[g.b_wb[s][(ko + kc) // 8], g.b_hT], writes=[pb], inc=(kc == DC - 1))
                    ts_ = tmpslot(g)
                    fc = ft * 2 + jn // 128
                    P.op("act", lambda e, ts_=ts_, pa=pa: e.activation(out=g.tmp[ts_][:], in_=pa[:], func=AF.Silu),
                         reads=[ba], writes=[g.b_tmp[ts_]])
                    P.op("dve", lambda e, ts_=ts_, pu=pu, fc=fc: e.tensor_tensor(out=aT[:, fc, :], in0=g.tmp[ts_][:], in1=pu[:],
                                                                                op=ALU.mult),
                         reads=[g.b_tmp[ts_], bu], writes=[b_aT])

            kk = [0]

            def epi(ci, pt, pb):
                if kk[0] % 2 == 0:
                    P.op("act", lambda e: e.copy(out=g.xy[:, ci, :], in_=pt[:]), reads=[pb], writes=[g.b_xy])
                else:
                    P.op("dve", lambda e: e.tensor_copy(out=g.xy[:, ci, :], in_=pt[:]), reads=[pb], writes=[g.b_xy])
                kk[0] += 1
            if 'd' in PARTS:
                gemm_F(g, Wd[l], 0, D, FC, lambda kc: aT[:, kc, :], [b_aT], epi)
            if 'p' in PARTS:
                post_res(g, tt, j)

    def mixer_in(g, l, zb, b_zb, halo, b_halo, brow, b_brow, wsm, b_wsm, osm, b_osm, cs, sn):
        W = w_in[l]
        for i, nm in enumerate(cfg.krange):
            c0, wdt = cfg.col[nm]
            P.dma("pool", brow[0:1, i, 0:wdt], b_in[l:l + 1, c0:c0 + wdt], writes=[b_brow])
        for i, (nm, h) in enumerate((("m_i", HM), ("m_f", HM), ("f_f", HF))):
            c0, wdt = cfg.col[nm]
            for k0 in range(0, DC, 8):
                k1 = min(DC, k0 + 8)
                P.dma("pool", wsm[:, k0:k1, i, 0:wdt], W[k0 * 128:k1 * 128, c0:c0 + wdt].rearrange("(kc p) n -> p kc n", p=128),
                      writes=[b_wsm])
        for tt in range(NT):
            b = tt // TPS
            first = (tt % TPS == 0)
            load_norm(g, tt, 1)
            hfn = lambda kc: g.hT[:, kc, :]
            c0, wdt = cfg.col["m_qk"]

            def epi_qk(ci, pt, pb):
                q = ci % 2
                if first:
                    P.op("dve", lambda e: e.memset(zb[q][:, 0:3], 0.0), writes=[b_zb[q]])
                else:
                    P.op("dve", lambda e: e.tensor_copy(out=zb[q][:, 0:3], in_=halo[:, ci, :]), reads=[b_halo], writes=[b_zb[q]])
                P.op("act", lambda e: e.activation(out=zb[q][:, 3:3 + T], in_=pt[:], func=AF.Identity,
                                                   bias=binp[:, l, cfg.foff["m_qk"] + ci:cfg.foff["m_qk"] + ci + 1]),
                     reads=[pb, b_const], writes=[b_zb[q]])
                P.op("dve", lambda e: e.tensor_copy(out=halo[:, ci, :], in_=zb[q][:, T:T + 3]), reads=[b_zb[q]], writes=[b_halo])
                s = tmpslot(g)
                P.op("dve", lambda e: e.tensor_scalar(out=g.tmp[s][:], in0=zb[q][:, 0:T], scalar1=cwp[:, l, ci, 0:1],
                                                      scalar2=None, op0=ALU.mult), reads=[b_zb[q], b_const], writes=[g.b_tmp[s]])
                for jj in (1, 2, 3):
                    P.op("dve", lambda e, jj=jj: e.scalar_tensor_tensor(
                        out=g.tmp[s][:], in0=zb[q][:, jj:jj + T], scalar=cwp[:, l, ci, jj:jj + 1], in1=g.tmp[s][:],
                        op0=ALU.mult, op1=ALU.add), reads=[b_zb[q], b_const, g.b_tmp[s]], writes=[g.b_tmp[s]])
                o = obslot(g)
                P.op("act", lambda e: e.activation(out=g.ob[o][:], in_=g.tmp[s][:], func=AF.Silu, bias=cbp[:, l, ci:ci + 1]),
                     reads=[g.b_tmp[s], b_const], writes=[g.b_ob[o]])
                P.dma("sp", QKM[ci * 128:(ci + 1) * 128, tok(tt)], g.ob[o][:], reads=[g.b_ob[o]], nowaw=[b_scr])
            gemm_F(g, W, c0, wdt, DC, hfn, [g.b_hT], epi_qk)

            def mk_epi(nm, dst, func, rowoff=0):
                def epi(ci, pt, pb):
                    o = obslot(g)
                    P.op("act", lambda e: e.activation(out=g.ob[o][:], in_=pt[:], func=func,
                                                       bias=binp[:, l, cfg.foff[nm] + ci:cfg.foff[nm] + ci + 1]),
                         reads=[pb, b_const], writes=[g.b_ob[o]])
                    P.dma("sp", dst[rowoff + ci * 128:rowoff + (ci + 1) * 128, tok(tt)], g.ob[o][:], reads=[g.b_ob[o]],
                          nowaw=[b_scr])
                return epi
            for nm, dst, func, ro in (("f_q", QF, AF.Identity, 0), ("f_k", KF, AF.Identity, 0),
                                      ("g_m", GATES, AF.Sigmoid, 0), ("g_f", GATES, AF.Sigmoid, D),
                                      ("g_r", GATES, AF.Sigmoid, 2 * D)):
                c0, wdt = cfg.col[nm]
                gemm_F(g, W, c0, wdt, DC, hfn, [g.b_hT], mk_epi(nm, dst, func, ro))

            pos = slice((tt % TPS) * T, (tt % TPS + 1) * T)
            for nm, dst in (("r_q", QR), ("r_k", KR)):
                c0, wdt = cfg.col[nm]
                st = {}

                def epi_rot(ci, pt, pb, nm=nm, dst=dst, st=st):
                    s = tmpslot(g)
                    P.op("act", lambda e: e.activation(out=g.tmp[s][:], in_=pt[:], func=AF.Identity,
                                                       bias=binp[:, l, cfg.foff[nm] + ci:cfg.foff[nm] + ci + 1]),
                         reads=[pb, b_const], writes=[g.b_tmp[s]])
                    if ci % 2 == 0:
                        st["a"] = s
                        return
                    s1, s2 = st["a"], s
                    for which in (0, 1):
                        s3 = tmpslot(g)
                        o = obslot(g)
                        ta, tb = (cs, sn) if which == 0 else (sn, cs)
                        P.op("dve", lambda e, ta=ta, s3=s3: e.tensor_tensor(out=g.tmp[s3][:], in0=g.tmp[s1][:], in1=ta[:, pos],
                                                                               op=ALU.mult),
                             reads=[g.b_tmp[s1], b_const], writes=[g.b_tmp[s3]])
                        P.op("dve", lambda e, tb=tb, o=o: e.tensor_tensor(out=g.ob[o][:], in0=g.tmp[s2][:], in1=tb[:, pos],
                                                                          op=ALU.mult),
                             reads=[g.b_tmp[s2], b_const], writes=[g.b_ob[o]])
                        P.op("dve", lambda e, o=o, s3=s3, which=which: e.tensor_tensor(
                            out=g.ob[o][:], in0=g.tmp[s3][:], in1=g.ob[o][:],
                            op=(ALU.subtract if which == 0 else ALU.add)),
                            reads=[g.b_tmp[s3], g.b_ob[o]], writes=[g.b_ob[o]])
                        cio = ci - 1 + which
                        P.dma("sp", dst[cio * 128:(cio + 1) * 128, tok(tt)], g.ob[o][:], reads=[g.b_ob[o]], nowaw=[b_scr])
                gemm_F(g, W, c0, wdt, DC, hfn, [g.b_hT], epi_rot)

            for i, (nm, hn, dst, bias) in enumerate((("m_i", HM, G_I, bsi), ("m_f", HM, G_F, bsf), ("f_f", HF, G_FF, bsff))):
                pt, pb = ps()
                for kc in range(DC):
                    P.op("pe", lambda e, pt=pt, kc=kc, i=i, hn=hn: e.matmul(
                        pt[0:hn, :], lhsT=wsm[:, kc, i, 0:hn], rhs=g.hT[:, kc, :], start=(kc == 0), stop=(kc == DC - 1)),
                        reads=[b_wsm, g.b_hT], writes=[pb], inc=(kc == DC - 1))
                q = i
                P.op("act", lambda e, pt=pt, hn=hn, bias=bias, q=q: e.activation(
                    out=osm[q][0:hn, :], in_=pt[0:hn, :], func=AF.Identity, bias=bias[0:hn, l:l + 1]),
                    reads=[pb, b_const], writes=[b_osm[q]])
                P.dma("sp", dst[0:hn, tok(tt)], osm[q][0:hn, :], reads=[b_osm[q]], nowaw=[b_scr])

            for i, (nm, dst, func) in enumerate((("m_v", VM, AF.Identity), ("m_o", OM, AF.Sigmoid), ("f_v", VF, AF.Identity),
                                                 ("r_v", VR, AF.Identity), ("r_g", GR, AF.Silu))):
                c0, wdt = cfg.col[nm]
                for t0 in range(0, wdt, 256):
                    s = wslot(g)
                    load_w(g, s, W, 0, DC, c0 + t0, 256)
                    for tc in range(T // 128):
                        pt, pb = ps()
                        for kc in range(DC):
                            P.op("pe", lambda e, pt=pt, s=s, kc=kc, tc=tc: e.matmul(
                                pt[:, 0:256], lhsT=g.hT[:, kc, tc * 128:(tc + 1) * 128], rhs=g.wb[s][:, kc, 0:256],
                                start=(kc == 0), stop=False), reads=[g.b_wb[s][kc // 8], g.b_hT], writes=[pb], inc=False)
                        P.op("pe", lambda e, pt=pt, i=i, t0=t0: e.matmul(
                            pt[:, 0:256], lhsT=ones_b[0:1, :], rhs=brow[0:1, i, t0:t0 + 256], start=False, stop=True),
                            reads=[b_brow, b_const], writes=[pb], inc=True)
                        o = obslot(g)
                        P.op("act", lambda e, o=o, pt=pt, func=func: e.activation(out=g.ob[o][:, 0:256], in_=pt[:, 0:256], func=func),
                             reads=[pb], writes=[g.b_ob[o]])
                        r0 = tt * T + tc * 128
                        P.dma("sp", dst[r0:r0 + 128, t0:t0 + 256], g.ob[o][:, 0:256], reads=[g.b_ob[o]], nowaw=[b_scr])

    def mixer_out(g, l, yb, b_yb, macc, b_macc, gt, b_gt):
        for tt in range(NT):
            for i, (Y, Wd_) in enumerate(((YM, MW), (YF, FW), (YR, RW))):
                P.dma("sp", yb[i][:, 0:Wd_ // 128, :], Y[:, tok(tt)].rearrange("(c p) t -> p c t", p=128),
                      reads=[b_y], writes=[b_yb[i]])
            for i, (Wb, Wd_) in enumerate(((w_bm, MW), (w_bf, FW), (w_br, RW))):
                def epi(ci, pt, pb, i=i):
                    q = ci % 2
                    P.dma("sp", gt[q][:], GATES[i * D + ci * 128:i * D + (ci + 1) * 128, tok(tt)], reads=[b_scr], writes=[b_gt[q]])
                    if i == 0:
                        P.op("dve", lambda e: e.tensor_tensor(out=macc[:, ci, :], in0=pt[:], in1=gt[q][:], op=ALU.mult),
                             reads=[pb, b_gt[q]], writes=[b_macc])
                    else:
                        s = tmpslot(g)
                        P.op("dve", lambda e: e.tensor_tensor(out=g.tmp[s][:], in0=pt[:], in1=gt[q][:], op=ALU.mult),
                             reads=[pb, b_gt[q]], writes=[g.b_tmp[s]])
                        if i == 1:
                            P.op("dve", lambda e: e.tensor_tensor(out=macc[:, ci, :], in0=macc[:, ci, :], in1=g.tmp[s][:], op=ALU.add),
                                 reads=[g.b_tmp[s], b_macc], writes=[b_macc])
                        else:
                            P.op("dve", lambda e: e.tensor_tensor(out=g.hT[:, ci, :], in0=macc[:, ci, :], in1=g.tmp[s][:], op=ALU.add),
                                 reads=[g.b_tmp[s], b_macc], writes=[g.b_hT])
                gemm_F(g, Wb[l], 0, D, Wd_ // 128, lambda kc, i=i: yb[i][:, kc, :], [b_yb[i]], epi)
            kk = [0]

            def epi_o(ci, pt, pb):
                if kk[0] % 2 == 0:
                    P.op("act", lambda e: e.copy(out=g.xy[:, ci, :], in_=pt[:]), reads=[pb], writes=[g.b_xy])
                else:
                    P.op("dve", lambda e: e.tensor_copy(out=g.xy[:, ci, :], in_=pt[:]), reads=[pb], writes=[g.b_xy])
                kk[0] += 1
            gemm_F(g, w_out[l], 0, D, DC, lambda kc: g.hT[:, kc, :], [g.b_hT], epi_o)
            post_res(g, tt, 1)

    def seq_mixers(l):
        RM, RF = NB * HM, NB * HF
        with ExitStack() as es:
            GMN = sb(es, "GMN", [RM, S], F32)
            FS = sb(es, "FS", [RF, S], F32)
            MS = sb(es, "MS", [128, CPS, 4, RM], F32)
            DECB = sb(es, "DECB", [128, RM, CPS], F32)
            FNT = sb(es, "FNT", [128, CPS, RF], F32)
            br = Buf("rows")
            bt = Buf("tokmaj")
            with ExitStack() as es1:
                UU = sb(es1, "UU", [RM, S], F32)
                GM = sb(es1, "GM", [RM, S], F32)
                BN = sb(es1, "BN", [RM, S], F32)
                RA = sb(es1, "RA", [RM, S], F32)
                RK = sb(es1, "RK", [RM, S], F32)
                GC = sb(es1, "GC", [RM, CPS], F32)
                FFp = sb(es1, "FFp", [RF, S], F32)
                FN = sb(es1, "FN", [RF, S], F32)
                onesr = sb(es1, "onesr", [max(RM, RF), S], F32)
                for b in range(NB):
                    sq_ = slice(b * S, (b + 1) * S)
                    P.dma("sp", UU[b * HM:(b + 1) * HM, :], G_I[:, sq_], reads=[b_scr], writes=[br])
                    P.dma("sp", GM[b * HM:(b + 1) * HM, :], G_F[:, sq_], reads=[b_scr], writes=[br])
                    P.dma("sp", FFp[b * HF:(b + 1) * HF, :], G_FF[:, sq_], reads=[b_scr], writes=[br])
                P.op("dve", lambda e: e.memset(onesr[:], 1.0), writes=[br])
                for tl in (GM, FFp):
                    P.op("act", lambda e, tl=tl: e.activation(out=tl[:], in_=tl[:], func=AF.Exp, scale=-1.0), reads=[br], writes=[br])
                    P.op("act", lambda e, tl=tl: e.activation(out=tl[:], in_=tl[:], func=AF.Ln, bias=cst[0:tl.shape[0], 2:3]), reads=[br], writes=[br])
                P.op("dve", lambda e: e.tensor_tensor_scan(out=BN[:], data0=onesr[0:RM, :], data1=GM[:], initial=0.0,
                                                           op0=ALU.mult, op1=ALU.add), reads=[br], writes=[br])
                P.op("dve", lambda e: e.tensor_tensor_scan(out=FN[:], data0=onesr[0:RF, :], data1=FFp[:], initial=0.0,
                                                           op0=ALU.mult, op1=ALU.add), reads=[br], writes=[br])
                P.op("dve", lambda e: e.tensor_tensor(out=UU[:], in0=UU[:], in1=BN[:], op=ALU.add), reads=[br], writes=[br])
                P.op("dve", lambda e: e.tensor_tensor_scan(out=GM[:], data0=UU[:], data1=UU[:], initial=0.0,
                                                           op0=ALU.max, op1=ALU.max), reads=[br], writes=[br])
                P.op("dve", lambda e: e.tensor_scalar(out=GMN[:], in0=GM[:], scalar1=-1.0, scalar2=None, op0=ALU.mult), reads=[br], writes=[br])
                P.op("dve", lambda e: e.tensor_scalar(out=FS[:], in0=FN[:], scalar1=-1.0, scalar2=None, op0=ALU.mult),
                     reads=[br], writes=[br])
                GMv = GM[:].rearrange("p (c l) -> p c l", l=128)
                P.op("dve", lambda e: e.memset(GC[:, 0:1], 0.0), reads=[br], writes=[br])
                P.op("dve", lambda e: e.tensor_copy(out=GC[:, 1:CPS], in_=GMv[:, 0:CPS - 1, 127]), reads=[br], writes=[br])
                P.op("dve", lambda e: e.tensor_tensor(out=BN[:], in0=BN[:], in1=GM[:], op=ALU.subtract), reads=[br], writes=[br])
                P.op("act", lambda e: e.activation(out=BN[:], in_=BN[:], func=AF.Exp), reads=[br], writes=[br])
                for c in range(CPS):
                    cl = slice(c * 128, (c + 1) * 128)
                    P.op("dve", lambda e, c=c, cl=cl: e.tensor_scalar(out=RA[:, cl], in0=GMN[:, cl], scalar1=GC[:, c:c + 1], scalar2=None,
                                                                      op0=ALU.add), reads=[br], writes=[br])
                    P.op("dve", lambda e, c=c, cl=cl: e.tensor_scalar(out=RK[:, cl], in0=UU[:, cl], scalar1=GM[:, c * 128 + 127:c * 128 + 128],
                                                                      scalar2=float(np.log(16.0)), op0=ALU.subtract, op1=ALU.subtract),
                         reads=[br], writes=[br])
                P.op("act", lambda e: e.activation(out=RA[:], in_=RA[:], func=AF.Exp), reads=[br], writes=[br])
                P.op("act", lambda e: e.activation(out=RK[:], in_=RK[:], func=AF.Exp), reads=[br], writes=[br])
                pms, bms = banks[5], bbuf[5]
                pdc, bdc = banks[6], bbuf[6]
                pfn, bfn = banks[7], bbuf[7]
                nq = 4 * RM
                for c in range(CPS):
                    cl = slice(c * 128, (c + 1) * 128)
                    for qi, R_ in enumerate((UU, RA, BN, RK)):
                        last = (c == CPS - 1 and qi == 3)
                        P.op("pe", lambda e, c=c, qi=qi, R_=R_, cl=cl: e.matmul(
                            pms[:, c * nq + qi * RM:c * nq + (qi + 1) * RM], lhsT=R_[:, cl], rhs=ident_f[0:RM, 0:RM], start=True, stop=True),
                            reads=[br, b_const], writes=[bms], inc=last)
                    P.op("pe", lambda e, c=c, cl=cl: e.matmul(pfn[:, c * RF:(c + 1) * RF], lhsT=FN[:, cl], rhs=ident_f[0:RF, 0:RF],
                                                              start=True, stop=True), reads=[br, b_const], writes=[bfn], inc=(c == CPS - 1))
                RAv = RA[:].rearrange("p (c l) -> p c l", l=128)
                for h in range(RM):
                    P.op("pe", lambda e, h=h: e.matmul(pdc[:, h * CPS:(h + 1) * CPS], lhsT=sel[0:RM, h, :], rhs=RAv[:, :, 127],
                                                       start=True, stop=True), reads=[br, b_const], writes=[bdc], inc=(h == RM - 1))
                P.op("dve", lambda e: e.tensor_copy(out=MS[:].rearrange("p c q h -> p (c q h)"), in_=pms[:, 0:CPS * nq]),
                     reads=[bms], writes=[bt])
                P.op("dve", lambda e: e.tensor_copy(out=DECB[:].rearrange("p h c -> p (h c)"), in_=pdc[:, 0:RM * CPS]), reads=[bdc], writes=[bt])
                P.op("dve", lambda e: e.tensor_copy(out=FNT[:].rearrange("p c h -> p (c h)"), in_=pfn[:, 0:CPS * RF]), reads=[bfn], writes=[bt])
                P.barrier()

            with ExitStack() as es2:
                kT = [sb(es2, "fkT%d" % i, [128, S], BF16) for i in range(2)]
                qT = [sb(es2, "fqT%d" % i, [128, S], BF16) for i in range(2)]
                Vt = [sb(es2, "fV%d" % i, [128, CPS, 128], BF16) for i in range(2)]
                b_in_ = [Buf() for _ in range(2)]
                PT = [sb(es2, "fPT%d" % i, [128, T], BF16) for i in range(3)]
                b_PT = [Buf() for _ in range(3)]
                rden = sb(es2, "frden", [128, T], F32)
                b_rden = Buf()
                Fb = [sb(es2, "fFb%d" % i, [128, T], F32) for i in range(2)]
                b_Fb = [Buf() for _ in range(2)]
                ftmp = [sb(es2, "ftmp%d" % i, [128, T], F32) for i in range(3)]
                b_ftmp = [Buf() for _ in range(3)]
                fbi = 0
                yo = [sb(es2, "fyo%d" % i, [128, T], BF16) for i in range(2)]
                b_yo = [Buf() for _ in range(2)]
                pO, bO = banks[5], bbuf[5]
                pD, bD = banks[6], bbuf[6]
                scale = float(128.0 ** -0.5)
                it = 0
                pi = 0
                oi = 0
                for b in range(NB):
                    for h in range(HF):
                        s = it % 2
                        it += 1
                        bh = b * HF + h
                        sq_ = slice(b * S, (b + 1) * S)
                        P.dma("sp", kT[s][:], KF[h * 128:(h + 1) * 128, sq_], reads=[b_scr], writes=[b_in_[s]])
                        P.dma("sp", qT[s][:], QF[h * 128:(h + 1) * 128, sq_], reads=[b_scr], writes=[b_in_[s]])
                        P.dma("sp", Vt[s][:], VF[sq_, h * 128:(h + 1) * 128].rearrange("(c p) e -> p c e", p=128),
                              reads=[b_scr], writes=[b_in_[s]])
                        for gq in range(TPS):
                            nkb = 4 * gq + 4
                            pF, bF = ps()
                            P.op("pe", lambda e: e.matmul(pF[:, 0:T], lhsT=sel[0:RF, bh, :], rhs=FS[:, gq * T:(gq + 1) * T],
                                                          start=True, stop=True), reads=[br, b_const], writes=[bF], inc=True)
                            fb = fbi % 2
                            fbi += 1
                            P.op("act", lambda e: e.copy(out=Fb[fb][:], in_=pF[:, 0:T]), reads=[bF], writes=[b_Fb[fb]])
                            pending = None
                            for j in range(nkb):
                                lo = max(0, j - 4 * gq) * 128
                                q0, q1 = gq * T + lo, (gq + 1) * T
                                ncol = T - lo
                                pt, pb = ps()
                                diag = (j >= 4 * gq)
                                P.op("pe", lambda e: e.matmul(
                                    pt[:, 0:ncol], lhsT=kT[s][:, j * 128:(j + 1) * 128], rhs=qT[s][:, q0:q1], start=True, stop=(not diag)),
                                    reads=[b_in_[s]], writes=[pb], inc=(not diag))
                                if diag:
                                    P.op("pe", lambda e: e.matmul(pt[:, 0:128], lhsT=ident_f[:], rhs=maskT[:], start=False, stop=True),
                                         reads=[b_const], writes=[pb], inc=True)
                                if pending is not None:
                                    pending()
                                p3 = pi % 3
                                pi += 1
                                P.op("dve", lambda e: e.scalar_tensor_tensor(
                                    out=ftmp[p3][:, 0:ncol], in0=pt[:, 0:ncol], scalar=scale, in1=Fb[fb][:, lo:T], op0=ALU.mult, op1=ALU.add),
                                    reads=[pb, b_Fb[fb]], writes=[b_ftmp[p3]])
                                P.op("act", lambda e: e.activation(
                                    out=PT[p3][:, 0:ncol], in_=ftmp[p3][:, 0:ncol], func=AF.Exp, bias=FNT[:, j, bh:bh + 1]),
                                    reads=[b_ftmp[p3], bt], writes=[b_PT[p3]])
                                def pv(j=j, p3=p3, lo=lo, ncol=ncol, s=s, nkb=nkb):
                                    P.op("pe", lambda e: e.matmul(
                                        pO[:, lo:T], lhsT=Vt[s][:, j, :], rhs=PT[p3][:, 0:ncol], start=(j == 0), stop=(j == nkb - 1)),
                                        reads=[b_PT[p3], b_in_[s]], writes=[bO], inc=(j == nkb - 1))
                                    P.op("pe", lambda e: e.matmul(
                                        pD[:, lo:T], lhsT=ones_b[:], rhs=PT[p3][:, 0:ncol], start=(j == 0), stop=(j == nkb - 1)),
                                        reads=[b_PT[p3], b_const], writes=[bD], inc=(j == nkb - 1))
                                pending = pv
                            pending()
                            P.op("dve", lambda e: e.reciprocal(out=rden[:], in_=pD[:]), reads=[bD], writes=[b_rden])
                            o = oi % 2
                            oi += 1
                            P.op("dve", lambda e: e.tensor_tensor(out=yo[o][:], in0=pO[:], in1=rden[:], op=ALU.mult),
                                 reads=[bO, b_rden], writes=[b_yo[o]])
                            P.dma("sp", YF[h * 128:(h + 1) * 128, b * S + gq * T:b * S + (gq + 1) * T], yo[o][:],
                                  reads=[b_yo[o]], nowaw=[b_y])
                P.barrier()

            def chunk_branch(kind):
                H = HM if kind == "m" else HR
                QT_src, KT_src = (QKM, QKM) if kind == "m" else (QR, KR)
                koff = MW if kind == "m" else 0
                Vsrc = VM if kind == "m" else VR
                Gsrc = OM if kind == "m" else GR
                NWsrc = mnw_b if kind == "m" else rnw_b
                Ydst = YM if kind == "m" else YR
                VW = 257 if kind == "m" else 256
                ring_n[0] = 8
                HG = min(4, H)
                HALF = CPS // 2
                SH = HALF * 128
                with ExitStack() as es2:
                    nwb = sb(es2, "nwb", [128, H * 256], F32)
                    b_nwb = Buf()
                    P.dma("sp", nwb[:], NWsrc[:, l, :], writes=[b_nwb])
                    if kind == "r":
                        intra = sb(es2, "intra", [128, HR, 128], F32)
                        qdec = sb(es2, "qdec", [128, HR, 128], F32)
                        kdec = sb(es2, "kdec", [128, HR], F32)
                        for d_, s_ in ((intra, intra_in), (qdec, qdec_in), (kdec, kdec_in)):
                            P.dma("sp", d_[:], s_, writes=[b_nwb])
                    qT = [sb(es2, "cq%d" % h, [128, 2, SH], BF16) for h in range(HG)]
                    kT = [sb(es2, "ck%d" % h, [128, 2, SH], BF16) for h in range(HG)]
                    Va = [sb(es2, "cv%d" % h, [128, HALF, VW], BF16) for h in range(HG)]
                    Gt = [sb(es2, "cg%d" % h, [128, HALF, 256], BF16) for h in range(HG)]
                    Cst = [sb(es2, "cC%d" % h, [128, 2, VW], F32) for h in range(HG)]
                    Cbf = [sb(es2, "cCb%d" % h, [128, 2, VW], BF16) for h in range(HG)]
                    b_ld = [Buf() for _ in range(HG)]
                    b_C = [Buf() for _ in range(HG)]
                    b_Cb = [Buf() for _ in range(HG)]
                    NR = 2 * HG
                    wT = [sb(es2, "cw%d" % i, [128, 128], BF16) for i in range(NR)]
                    Dt = [sb(es2, "cD%d" % i, [128, 128], F32) for i in range(NR)]
                    qd = [sb(es2, "cqd%d" % i, [128, 2, 128], BF16) for i in range(NR)]
                    kw = [sb(es2, "ckw%d" % i, [128, 256], BF16) for i in range(NR)]
                    o1 = [sb(es2, "co1%d" % i, [128, VW], F32) for i in range(NR)]
                    na = [sb(es2, "cna%d" % i, [128, VW], F32) for i in range(NR)]
                    hh = [sb(es2, "chh%d" % i, [128, 256], F32) for i in range(NR)]
                    yb_ = [sb(es2, "cyb%d" % i, [128, 256], BF16) for i in range(NR)]
                    st6 = [sb(es2, "cst%d" % i, [128, 8], F32) for i in range(NR)]
                    mv = [sb(es2, "cmv%d" % i, [128, 4], F32) for i in range(NR)]
                    sc1 = [sb(es2, "csc%d" % i, [128, 4], F32) for i in range(NR)]
                    yT = [sb(es2, "cyT%d" % h, [128, 2, T], BF16) for h in range(HG)]
                    b_r = [Buf() for _ in range(NR)]
                    b_yT = [Buf() for _ in range(HG)]
                    ri = [0]
                    for b, hf, h0 in [(b_, hf_, h0_) for b_ in range(NB) for h0_ in range(0, H, HG) for hf_ in range(2)]:
                        if True:
                            sq_ = slice(b * S + hf * SH, b * S + (hf + 1) * SH)
                            for hi in range(HG):
                                h = h0 + hi
                                P.dma("sp", qT[hi][:], QT_src[h * 256:(h + 1) * 256, sq_].rearrange("(c p) t -> p c t", p=128),
                                      reads=[b_scr], writes=[b_ld[hi]])
                                P.dma("sp", kT[hi][:], KT_src[koff + h * 256:koff + (h + 1) * 256, sq_].rearrange("(c p) t -> p c t", p=128),
                                      reads=[b_scr], writes=[b_ld[hi]])
                                P.dma("sp", Va[hi][:, :, 0:256], Vsrc[sq_, h * 256:(h + 1) * 256].rearrange("(c p) e -> p c e", p=128),
                                      reads=[b_scr], writes=[b_ld[hi]])
                                if kind == "m":
                                    P.op("dve", lambda e: e.memset(Va[hi][:, :, 256:257], 1.0), writes=[b_ld[hi]])
                                P.dma("sp", Gt[hi][:], Gsrc[sq_, h * 256:(h + 1) * 256].rearrange("(c p) e -> p c e", p=128),
                                      reads=[b_scr], writes=[b_ld[hi]])
                                if hf == 0:
                                    P.op("dve", lambda e: e.memset(Cst[hi][:], 0.0), writes=[b_C[hi]])
                                    P.op("dve", lambda e: e.memset(Cbf[hi][:], 0.0), writes=[b_Cb[hi]])
                            for cc in range(HALF):
                                c = hf * HALF + cc
                                cl = slice(cc * 128, (cc + 1) * 128)
                                gl = slice(c * 128, (c + 1) * 128)
                                def item(hi):
                                    h = h0 + hi
                                    bh = b * H + h
                                    r = ri[0] % NR
                                    ri[0] += 1
                                    R = [b_r[r]]
                                    pS, bS = ps()
                                    for dc in (0, 1):
                                        P.op("pe", lambda e: e.matmul(
                                            pS[:, 0:128], lhsT=kT[hi][:, dc, cl], rhs=qT[hi][:, dc, cl], start=(dc == 0), stop=(dc == 1)),
                                            reads=[b_ld[hi]], writes=[bS], inc=(dc == 1))
                                        yield
                                    if kind == "m":
                                        pDm, bDm = ps()
                                        P.op("pe", lambda e: e.matmul(
                                            pDm[:, 0:128], lhsT=sel[0:RM, bh, :], rhs=GMN[:, gl], start=True, stop=False),
                                            reads=[br, b_const], writes=[bDm], inc=False)
                                        yield
                                        P.op("pe", lambda e: e.matmul(pDm[:, 0:128], lhsT=ident_f[:], rhs=maskT[:], start=False, stop=True),
                                             reads=[b_const], writes=[bDm], inc=True)
                                        yield
                                        P.op("act", lambda e: e.activation(
                                            out=Dt[r][:], in_=pDm[:, 0:128], func=AF.Exp, bias=MS[:, c, 0, bh:bh + 1]),
                                            reads=[bDm, bt], writes=R)
                                        yield
                                        P.op("dve", lambda e: e.scalar_tensor_tensor(
                                            out=wT[r][:], in0=pS[:, 0:128], scalar=0.0625, in1=Dt[r][:], op0=ALU.mult, op1=ALU.mult),
                                            reads=[bS] + R, writes=R)
                                        yield
                                    else:
                                        P.op("dve", lambda e: e.tensor_tensor(
                                            out=wT[r][:], in0=pS[:, 0:128], in1=intra[:, h, :], op=ALU.mult),
                                            reads=[bS, b_nwb], writes=R)
                                        yield
                                        for dc in (0, 1):
                                            P.op("pool", lambda e: e.tensor_tensor(
                                                out=qd[r][:, dc, :], in0=qT[hi][:, dc, cl], in1=qdec[:, h, :], op=ALU.mult),
                                                reads=[b_ld[hi], b_nwb], writes=R)
                                            yield
                                    pO_, bO_ = ps()
                                    if kind == "m":
                                        P.op("pe", lambda e: e.matmul(
                                            pO_[:, 0:VW], lhsT=wT[r][:], rhs=Va[hi][:, cc, :], start=True, stop=True),
                                            reads=R + [b_ld[hi]], writes=[bO_], inc=True)
                                        yield
                                        pI, bI = ps()
                                        for dc in (0, 1):
                                            P.op("pe", lambda e: e.matmul(
                                                pI[:, 0:VW], lhsT=qT[hi][:, dc, cl], rhs=Cbf[hi][:, dc, :], start=(dc == 0), stop=(dc == 1)),
                                                reads=[b_ld[hi], b_Cb[hi]], writes=[bI], inc=(dc == 1))
                                            yield
                                        P.op("act", lambda e: e.copy(out=o1[r][:], in_=pO_[:, 0:VW]), reads=[bO_], writes=R)
                                        yield
                                        P.op("dve", lambda e: e.scalar_tensor_tensor(
                                            out=na[r][:], in0=pI[:, 0:VW], scalar=MS[:, c, 1, bh:bh + 1], in1=o1[r][:], op0=ALU.mult, op1=ALU.add),
                                            reads=[bI, bt] + R, writes=R)
                                        yield
                                        P.op("dve", lambda e: e.tensor_scalar(
                                            out=sc1[r][:, 2:3], in0=na[r][:, 256:257], scalar1=-1.0, scalar2=None, op0=ALU.mult),
                                            reads=R, writes=R)
                                        yield
                                        P.op("dve", lambda e: e.tensor_tensor(
                                            out=sc1[r][:, 0:1], in0=na[r][:, 256:257], in1=sc1[r][:, 2:3], op=ALU.max), reads=R, writes=R)
                                        yield
                                        P.op("dve", lambda e: e.tensor_scalar(
                                            out=sc1[r][:, 0:1], in0=sc1[r][:, 0:1], scalar1=MS[:, c, 2, bh:bh + 1], scalar2=None, op0=ALU.max),
                                            reads=R + [bt], writes=R)
                                        yield
                                        P.op("dve", lambda e: e.reciprocal(out=sc1[r][:, 1:2], in_=sc1[r][:, 0:1]), reads=R, writes=R)
                                        yield
                                        P.op("dve", lambda e: e.tensor_scalar(out=hh[r][:], in0=na[r][:, 0:256], scalar1=sc1[r][:, 1:2],
                                                                              scalar2=None, op0=ALU.mult), reads=R, writes=R)
                                        yield
                                    else:
                                        P.op("pe", lambda e: e.matmul(
                                            pO_[:, 0:256], lhsT=wT[r][:], rhs=Va[hi][:, cc, :], start=True, stop=False),
                                            reads=R + [b_ld[hi]], writes=[bO_], inc=False)
                                        yield
                                        for dc in (0, 1):
                                            P.op("pe", lambda e: e.matmul(
                                                pO_[:, 0:256], lhsT=qd[r][:, dc, :], rhs=Cbf[hi][:, dc, :], start=False, stop=(dc == 1)),
                                                reads=R + [b_Cb[hi]], writes=[bO_], inc=(dc == 1))
                                            yield
                                        P.op("act", lambda e: e.copy(out=hh[r][:], in_=pO_[:, 0:256]), reads=[bO_], writes=R)
                                        yield
                                    P.op("dve", lambda e: e.bn_stats(out=st6[r][:, 0:6], in_=hh[r][:]), reads=R, writes=R)
                                    yield
                                    P.op("dve", lambda e: e.bn_aggr(out=mv[r][:, 0:2], in_=st6[r][:, 0:6]), reads=R, writes=R)
                                    yield
                                    P.op("act", lambda e: e.activation(out=mv[r][:, 2:3], in_=mv[r][:, 1:2], func=AF.Sqrt, bias=cst[:, 1:2]),
                                         reads=R + [b_const], writes=R)
                                    yield
                                    P.op("dve", lambda e: e.reciprocal(out=mv[r][:, 2:3], in_=mv[r][:, 2:3]), reads=R, writes=R)
                                    yield
                                    P.op("dve", lambda e: e.tensor_scalar(out=hh[r][:], in0=hh[r][:], scalar1=mv[r][:, 0:1],
                                                                          scalar2=mv[r][:, 2:3], op0=ALU.subtract, op1=ALU.mult),
                                         reads=R, writes=R)
                                    yield
                                    P.op("pool", lambda e: e.tensor_tensor(out=hh[r][:], in0=hh[r][:], in1=nwb[:, h * 256:(h + 1) * 256],
                                                                            op=ALU.mult), reads=R + [b_nwb], writes=R)
                                    yield
                                    P.op("dve", lambda e: e.tensor_tensor(out=yb_[r][:], in0=hh[r][:], in1=Gt[hi][:, cc, :], op=ALU.mult),
                                         reads=R + [b_ld[hi]], writes=R)
                                    yield
                                    tq = c % 4
                                    for dc in (0, 1):
                                        pY, bY = ps()
                                        P.op("pe", lambda e: e.matmul(
                                            pY[:, 0:128], lhsT=yb_[r][:, dc * 128:(dc + 1) * 128], rhs=ident_b[:], start=True, stop=True),
                                            reads=R + [b_const], writes=[bY], inc=True)
                                        yield
                                        P.op("act", lambda e: e.copy(out=yT[hi][:, dc, tq * 128:(tq + 1) * 128], in_=pY[:, 0:128]),
                                             reads=[bY], writes=[b_yT[hi]])
                                        yield
                                    if tq == 3:
                                        t0_ = b * S + (c - 3) * 128
                                        P.dma("sp", Ydst[h * 256:(h + 1) * 256, t0_:t0_ + T].rearrange("(c p) t -> p c t", p=128), yT[hi][:],
                                              reads=[b_yT[hi]], nowaw=[b_y])
                                        yield
                                    pK, bK = ps()
                                    for dc in (0, 1):
                                        P.op("pe", lambda e: e.matmul(
                                            pK[:, dc * 128:(dc + 1) * 128], lhsT=kT[hi][:, dc, cl], rhs=ident_b[:], start=True, stop=True),
                                            reads=[b_ld[hi], b_const], writes=[bK], inc=(dc == 1))
                                        yield
                                    if kind == "m":
                                        P.op("dve", lambda e: e.tensor_scalar(
                                            out=kw[r][:], in0=pK[:, 0:256], scalar1=MS[:, c, 3, bh:bh + 1], scalar2=None, op0=ALU.mult),
                                            reads=[bK, bt], writes=R)
                                        yield
                                    else:
                                        P.op("dve", lambda e: e.tensor_scalar(
                                            out=kw[r][:], in0=pK[:, 0:256], scalar1=kdec[:, h:h + 1], scalar2=None, op0=ALU.mult),
                                            reads=[bK, b_nwb], writes=R)
                                        yield
                                    for dc in (0, 1):
                                        pC, bC = ps()
                                        P.op("pe", lambda e: e.matmul(
                                            pC[:, 0:VW], lhsT=kw[r][:, dc * 128:(dc + 1) * 128], rhs=Va[hi][:, cc, :], start=True, stop=True),
                                            reads=R + [b_ld[hi]], writes=[bC], inc=True)
                                        yield
                                        if kind == "m":
                                            dscal = DECB[:, bh, c:c + 1]
                                            rd = [bt]
                                        else:
                                            dscal = float((1.0 - 2.0 ** (-5.0 - h)) ** 128)
                                            rd = []
                                        P.op("dve", lambda e: e.scalar_tensor_tensor(
                                            out=Cst[hi][:, dc, :], in0=Cst[hi][:, dc, :], scalar=dscal, in1=pC[:, 0:VW], op0=ALU.mult, op1=ALU.add),
                                            reads=[bC, b_C[hi]] + rd, writes=[b_C[hi]])
                                        yield
                                        P.op("act", lambda e: e.copy(out=Cbf[hi][:, dc, :], in_=Cst[hi][:, dc, :]),
                                             reads=[b_C[hi]], writes=[b_Cb[hi]])
                                        yield
                                gens = [item(hi_) for hi_ in range(HG)]
                                while gens:
                                    nxt = []
                                    for g_ in gens:
                                        try:
                                            next(g_)
                                            nxt.append(g_)
                                        except StopIteration:
                                            pass
                                    gens = nxt
                    P.barrier()
                    ring_n[0] = 5

            chunk_branch("r")
            chunk_branch("m")

    PH = getattr(cfg, 'phases', (1, 2, 3, 4, 5))
    for l in range(L):
        with ExitStack() as es:
            g = alloc_gemm(es, KMAX)
            aT = sb(es, "aT", [128, FC, T], BF16)
            b_aT = Buf("aT")
            compute_mod(l, g)
            if 1 in PH:
                ffn(g, aT, b_aT, l, 0, ffn_w["ffn1_gate"], ffn_w["ffn1_up"], ffn_w["ffn1_down"])
            P.barrier()
        with ExitStack() as es:
            g = alloc_gemm(es, max(DC, MW // 128, FW // 128, RW // 128))
            zb = [sb(es, "zb%d" % i, [128, T + 3], F32) for i in range(2)]
            b_zb = [Buf() for _ in range(2)]
            halo = sb(es, "halo", [128, 2 * MW // 128, 3], F32)
            b_halo = Buf()
            brow = sb(es, "brow", [1, 5, max(MW, FW, RW)], BF16)
            b_brow = Buf()
            wsm = sb(es, "wsm", [128, DC, 3, 8], BF16)
            b_wsm = Buf()
            osm = [sb(es, "osm%d" % i, [8, T], F32) for i in range(3)]
            b_osm = [Buf() for _ in range(3)]
            cs = sb(es, "cs", [128, S], F32)
            sn = sb(es, "sn", [128, S], F32)
            P.dma("sp", cs[:], cos_in, writes=[b_const])
            P.dma("sp", sn[:], sin_in, writes=[b_const])
            if 2 in PH:
                mixer_in(g, l, zb, b_zb, halo, b_halo, brow, b_brow, wsm, b_wsm, osm, b_osm, cs, sn)
            P.barrier()
        if 3 in PH:
            seq_mixers(l)
        with ExitStack() as es:
            g = alloc_gemm(es, max(DC, MW // 128, FW // 128, RW // 128))
            yb = [sb(es, "yb%d" % i, [128, max(MW, FW, RW) // 128, T], BF16) for i in range(3)]
            b_yb = [Buf() for _ in range(3)]
            macc = sb(es, "macc", [128, DC, T], F32)
            b_macc = Buf()
            gt = [sb(es, "gt%d" % i, [128, T], BF16) for i in range(2)]
            b_gt = [Buf() for _ in range(2)]
            if 4 in PH:
                mixer_out(g, l, yb, b_yb, macc, b_macc, gt, b_gt)
            P.barrier()
        with ExitStack() as es:
            g = alloc_gemm(es, KMAX)
            aT = sb(es, "aT", [128, FC, T], BF16)
            b_aT = Buf("aT")
            if 5 in PH:
                ffn(g, aT, b_aT, l, 2, ffn_w["ffn2_gate"], ffn_w["ffn2_up"], ffn_w["ffn2_down"])
            P.barrier()

    with ExitStack() as es:
        xt = [sb(es, "fxt%d" % i, [128, DC, T], F32) for i in range(2)]
        b_xt = [Buf() for _ in range(2)]
        ot = [sb(es, "fot%d" % i, [128, 512], F32) for i in range(4)]
        b_ot = [Buf() for _ in range(4)]
        k = 0
        for tt in range(NT):
            s = tt % 2
            P.dma("sp", xt[s][:], XT[:, tok(tt)].rearrange("(c p) t -> p c t", p=128), reads=b_XT[tt], writes=[b_xt[s]])
            for tc in range(T // 128):
                for d0 in range(0, DC, 4):
                    nd = min(4, DC - d0)
                    pt, pb = ps()
                    for dd in range(nd):
                        P.op("pe", lambda e, pt=pt, s=s, tc=tc, d0=d0, dd=dd: e.matmul(
                            pt[:, dd * 128:(dd + 1) * 128], lhsT=xt[s][:, d0 + dd, tc * 128:(tc + 1) * 128], rhs=ident_f[:],
                            start=True, stop=True), reads=[b_xt[s], b_const], writes=[pb], inc=(dd == nd - 1))
                    o = k % 4
                    if k % 2 == 0:
                        P.op("act", lambda e, o=o, pt=pt, nd=nd: e.copy(out=ot[o][:, 0:nd * 128], in_=pt[:, 0:nd * 128]), reads=[pb], writes=[b_ot[o]])
                    else:
                        P.op("dve", lambda e, o=o, pt=pt, nd=nd: e.tensor_copy(out=ot[o][:, 0:nd * 128], in_=pt[:, 0:nd * 128]), reads=[pb], writes=[b_ot[o]])
                    r0 = tt * T + tc * 128
                    P.dma("sp", out[r0:r0 + 128, d0 * 128:(d0 + nd) * 128], ot[o][:, 0:nd * 128], reads=[b_ot[o]], nowaw=[b_out])
                    k += 1
    P.barrier()
    glob.close()
    P.close()
    return nc, P.n_instr


def host_inputs(cfg, inp, core):
    D, S, NB, L, HM, HF, HR = cfg.D, cfg.S, cfg.NB, cfg.L, cfg.HM, cfg.HF, cfg.HR
    f = np.float32
    m = {}
    xs = inp["x"][core * NB:(core + 1) * NB]
    m["x"] = np.ascontiguousarray(xs.reshape(NB * S, D))
    cc = inp["c"][core * NB:(core + 1) * NB]
    m["c_pc"] = np.ascontiguousarray(cc.reshape(NB, cfg.DC, 128).transpose(2, 1, 0))
    return m


def shared_inputs(cfg, inp):
    D, S, NB, L, HM, HF, HR, MW = cfg.D, cfg.S, cfg.NB, cfg.L, cfg.HM, cfg.HF, cfg.HR, cfg.MW
    f = np.float32
    m = {}
    for k in ("w_ada", "ffn1_gate", "ffn1_up", "ffn1_down", "ffn2_gate", "ffn2_up", "ffn2_down", "w_in", "b_in",
              "w_branch_m", "w_branch_f", "w_branch_r", "w_out"):
        m[k] = np.ascontiguousarray(inp[k], dtype=f)

    def pc(v):
        v = np.asarray(v, dtype=f)
        sh = v.shape[:-1]
        v = v.reshape(sh + (v.shape[-1] // 128, 128))
        return np.ascontiguousarray(np.moveaxis(v, -1, 0))
    m["bada_pc"] = pc(inp["b_ada"])
    m["nw_pc"] = pc(inp["norm_w"])
    parts = []
    for nm in cfg.frange:
        c0, w = cfg.col[nm]
        parts.append(pc(inp["b_in"][:, c0:c0 + w]))
    m["bin_pc"] = np.ascontiguousarray(np.concatenate(parts, axis=2))
    for key, nm in (("bsm_i", "m_i"), ("bsm_f", "m_f"), ("bsm_ff", "f_f")):
        c0, w = cfg.col[nm]
        m[key] = np.ascontiguousarray(np.asarray(inp["b_in"][:, c0:c0 + w], dtype=f).T)
    cw = np.asarray(inp["conv_w"], dtype=f)
    m["convw_pc"] = np.ascontiguousarray(np.moveaxis(pc(cw), 2, 3))
    m["convb_pc"] = pc(inp["conv_b"])
    m["mnw_b"] = np.ascontiguousarray(np.broadcast_to(np.asarray(inp["mlstm_norm_w"], dtype=f)[None], (128, L, MW)))
    m["rnw_b"] = np.ascontiguousarray(np.broadcast_to(np.asarray(inp["ret_norm_w"], dtype=f)[None], (128, L, cfg.RW)))
    m["ident"] = np.eye(128, dtype=f)
    s_ = np.arange(128)
    m["maskT"] = np.where(s_[:, None] <= s_[None, :], 0.0, NEG).astype(f)
    sel = np.zeros((16, 16, 128), f)
    for h in range(16):
        sel[h, h, :] = 1.0
    m["sel"] = sel
    lg = np.log1p(-(2.0 ** (-5.0 - np.arange(HR, dtype=np.float64))))
    rel = (s_[None, :] - s_[:, None]).astype(np.float64)
    intra = np.where(rel[:, None, :] >= 0, np.exp(np.maximum(rel, 0)[:, None, :] * lg[None, :, None]), 0.0) * (256.0 ** -0.5)
    m["intra"] = np.ascontiguousarray(intra.astype(f))
    qd = np.exp((s_[None, :] + 1.0) * lg[:, None])
    m["qdec"] = np.ascontiguousarray(np.broadcast_to(qd[None], (128, HR, 128)).astype(f))
    kd = np.exp((127.0 - s_[:, None]) * lg[None, :]) * (256.0 ** -0.5)
    m["kdec"] = np.ascontiguousarray(kd.astype(f))
    half = 128
    inv_freq = (10000.0 ** (-np.arange(half, dtype=f) / f(half))).astype(f)
    ang = (np.arange(S, dtype=f)[None, :] * inv_freq[:, None]).astype(f)
    m["cos"] = np.cos(ang.astype(np.float64)).astype(f)
    m["sin"] = np.sin(ang.astype(np.float64)).astype(f)
    return m


_CACHE = {}


def run(cfg, inp, ncores, trace=False):
    nc, n_instr = build_program(cfg)
    sh = shared_inputs(cfg, inp)
    in_maps = []
    for c in range(ncores):
        d = dict(sh)
        d.update(host_inputs(cfg, inp, c))
        in_maps.append(d)
    res = run_bass_kernel_spmd(nc, in_maps, core_ids=list(range(ncores)), trace=trace)
    outs = [r["out"].reshape(cfg.NB, cfg.S, cfg.D) for r in res.results]
    return np.concatenate(outs, axis=0), res, n_instr


def kernel(**inputs):
    cfg = mkcfg(True)
    inp = {k: np.asarray(v) for k, v in inputs.items()}
    out, _, _ = run(cfg, inp, 8)
    return out.astype(np.float32)
```

```python
import numpy as np
from contextlib import ExitStack
import concourse.bass as bass
import concourse.mybir as mybir
from concourse.bass_utils import run_bass_kernel_spmd

F32 = mybir.dt.float32
BF16 = mybir.dt.bfloat16
AF = mybir.ActivationFunctionType
ALU = mybir.AluOpType

COMPUTE = ("pe", "act", "dve", "pool")
ENGINES = ("pe", "act", "dve", "pool", "sp")


class Buf:
    __slots__ = ("name", "w", "r")

    def __init__(self, name=""):
        self.name = name
        self.w = {}
        self.r = {}


class Prog:
    def __init__(self, nc):
        self.nc = nc
        self.es = ExitStack()
        self.cnt = {e: 0 for e in COMPUTE}
        self.waited = {e: {} for e in ENGINES}
        self.sems = {}
        self.eng = {"pe": nc.tensor, "act": nc.scalar, "dve": nc.vector, "pool": nc.gpsimd, "sp": nc.sync}
        for e in COMPUTE:
            self.sems[e] = self.es.enter_context(nc.semaphore("s_" + e))
        self.dma_pool, self.dma_cnt, self.dma_rr = {}, {}, {}
        for q, n in (("sp", 16), ("act", 4), ("pool", 8)):
            ks = []
            for i in range(n):
                k = "d_%s_%d" % (q, i)
                self.sems[k] = self.es.enter_context(nc.semaphore(k))
                self.dma_cnt[k] = 0
                ks.append(k)
            self.dma_pool[q] = ks
            self.dma_rr[q] = 0
        self.n_instr = 0

    def _deps(self, eng, reads, writes, nowaw=()):
        deps = {}

        def add(ev):
            if deps.get(ev[0], 0) < ev[1]:
                deps[ev[0]] = ev[1]
        for b in reads:
            for kv in b.w.items():
                add(kv)
        for b in writes:
            for kv in b.w.items():
                add(kv)
            for kv in b.r.items():
                add(kv)
        for b in nowaw:
            for kv in b.r.items():
                add(kv)
        waits = []
        wd = self.waited[eng]
        for k, v in deps.items():
            if k == eng and (eng == "pe" or v > self.cnt[eng]):
                continue
            if wd.get(k, 0) >= v:
                continue
            wd[k] = v
            waits.append((k, v))
        return waits

    @staticmethod
    def _mark(ev, reads, writes, nowaw=()):
        k, v = ev
        for b in reads:
            if b.r.get(k, 0) < v:
                b.r[k] = v
        for b in writes:
            b.w = {k: v}
            b.r = {}
        for b in nowaw:
            if b.w.get(k, 0) < v:
                b.w[k] = v

    def _emit(self, eng, waits, fn, ev):
        e = self.eng[eng]
        for k, v in waits:
            e.wait_ge(self.sems[k], v)
        if fn is None:
            return
        ins = fn(e)
        if ev is not None:
            ins.then_inc(self.sems[ev[0]], 1 if ev[0] in COMPUTE else 16)
        self.n_instr += 1

    def op(self, eng, fn, reads=(), writes=(), inc=True):
        waits = self._deps(eng, reads, writes)
        ev = (eng, self.cnt[eng] + 1)
        if inc:
            self.cnt[eng] += 1
        self._mark(ev, reads, writes)
        self._emit(eng, waits, fn, ev if inc else None)
        return ev

    def dma(self, q, out, in_, reads=(), writes=(), nowaw=()):
        pool = self.dma_pool[q]
        k = pool[self.dma_rr[q] % len(pool)]
        self.dma_rr[q] += 1
        waits = self._deps(q, reads, writes, nowaw)
        prev = self.dma_cnt[k] * 16
        if prev > 0 and self.waited[q].get(k, 0) < prev:
            self.waited[q][k] = prev
            waits.append((k, prev))
        self.dma_cnt[k] += 1
        ev = (k, self.dma_cnt[k] * 16)
        self._mark(ev, reads, writes, nowaw)
        self._emit(q, waits, lambda e: e.dma_start(out=out, in_=in_), ev)
        return ev

    def barrier(self):
        evs = [(e, self.cnt[e]) for e in COMPUTE if self.cnt[e] > 0]
        evs += [(k, c * 16) for k, c in self.dma_cnt.items() if c > 0]
        for eng in ENGINES:
            waits = []
            for k, v in evs:
                if k == eng and eng == "pe":
                    continue
                if self.waited[eng].get(k, 0) >= v:
                    continue
                self.waited[eng][k] = v
                waits.append((k, v))
            self._emit(eng, waits, None, None)

    def close(self):
        self.es.close()


class Cfg:
    pass


def mkcfg(full=True):
    c = Cfg()
    if full is True:
        c.D, c.DFF, c.S, c.NB, c.HM, c.HF, c.HR, c.L = 2048, 5632, 2048, 2, 4, 8, 4, 4
    elif full == "medium":
        c.D, c.DFF, c.S, c.NB, c.HM, c.HF, c.HR, c.L = 256, 768, 1024, 2, 4, 8, 4, 2
    else:
        c.D, c.DFF, c.S, c.NB, c.HM, c.HF, c.HR, c.L = 256, 512, 1024, 1, 1, 2, 1, 2
    c.T = 512
    c.N = c.NB * c.S
    c.DC, c.FC = c.D // 128, c.DFF // 128
    c.NT = c.N // c.T
    c.TPS = c.S // c.T
    c.MW, c.FW, c.RW = c.HM * 256, c.HF * 128, c.HR * 256
    c.NCH = c.N // 128
    c.CPS = c.S // 128
    widths = (2 * c.MW, c.MW, c.MW, c.HM, c.HM, c.FW, c.FW, c.FW, c.HF, c.RW, c.RW, c.RW, c.RW, c.D, c.D, c.D)
    names = ("m_qk", "m_v", "m_o", "m_i", "m_f", "f_q", "f_k", "f_v", "f_f", "r_q", "r_k", "r_v", "r_g", "g_m", "g_f", "g_r")
    st = np.cumsum((0,) + widths[:-1])
    c.col = {n: (int(s), int(w)) for n, s, w in zip(names, st, widths)}
    c.INC = int(sum(widths))
    c.frange = ("m_qk", "f_q", "f_k", "r_q", "r_k", "g_m", "g_f", "g_r")
    c.foff = {}
    o = 0
    for n in c.frange:
        c.foff[n] = o
        o += c.col[n][1] // 128
    c.NCF = o
    c.krange = ("m_v", "m_o", "f_v", "r_v", "r_g")
    return c


EPS = 1e-6
NEG = -60000.0


def build_program(cfg):
    D, DFF, S, NB, HM, HF, HR, L, T, N = cfg.D, cfg.DFF, cfg.S, cfg.NB, cfg.HM, cfg.HF, cfg.HR, cfg.L, cfg.T, cfg.N
    DC, FC, NT, TPS, MW, FW, RW, NCH, CPS = cfg.DC, cfg.FC, cfg.NT, cfg.TPS, cfg.MW, cfg.FW, cfg.RW, cfg.NCH, cfg.CPS
    KMAX = max(FC, 2 * DC)
    nc = bass.Bass("TRN2", target_bir_lowering=False)

    def din(name, shape, dt=F32):
        return nc.dram_tensor(name, list(shape), dt, kind="ExternalInput").ap()

    def dscr(name, shape, dt):
        return nc.dram_tensor(name, list(shape), dt).ap()

    x_in = din("x", [N, D])
    c_pc = din("c_pc", [128, DC, NB])
    w_ada = din("w_ada", [L, D, 9 * D])
    bada_pc = din("bada_pc", [128, L, 9 * DC])
    nw_pc = din("nw_pc", [128, L, 6, DC])
    ffn_w = {}
    for nm in ("ffn1_gate", "ffn1_up", "ffn2_gate", "ffn2_up"):
        ffn_w[nm] = din(nm, [L, D, DFF])
    for nm in ("ffn1_down", "ffn2_down"):
        ffn_w[nm] = din(nm, [L, DFF, D])
    w_in = din("w_in", [L, D, cfg.INC])
    b_in = din("b_in", [L, cfg.INC])
    bin_pc = din("bin_pc", [128, L, cfg.NCF])
    bsm_i = din("bsm_i", [HM, L])
    bsm_f = din("bsm_f", [HM, L])
    bsm_ff = din("bsm_ff", [HF, L])
    convw_pc = din("convw_pc", [128, L, 2 * MW // 128, 4])
    convb_pc = din("convb_pc", [128, L, 2 * MW // 128])
    mnw_b = din("mnw_b", [128, L, MW])
    rnw_b = din("rnw_b", [128, L, RW])
    w_bm = din("w_branch_m", [L, MW, D])
    w_bf = din("w_branch_f", [L, FW, D])
    w_br = din("w_branch_r", [L, RW, D])
    w_out = din("w_out", [L, D, D])
    ident_in = din("ident", [128, 128])
    maskT_in = din("maskT", [128, 128])
    sel_in = din("sel", [16, 16, 128])
    intra_in = din("intra", [128, HR, 128])
    qdec_in = din("qdec", [128, HR, 128])
    kdec_in = din("kdec", [128, HR])
    cos_in = din("cos", [128, S])
    sin_in = din("sin", [128, S])
    out = nc.dram_tensor("out", [N, D], F32, kind="ExternalOutput").ap()

    XT = dscr("XT", [D, N], F32)
    QKM = dscr("QKM", [2 * MW, N], BF16)
    VM = dscr("VM", [N, MW], BF16)
    OM = dscr("OM", [N, MW], BF16)
    G_I = dscr("G_I", [HM, N], F32)
    G_F = dscr("G_F", [HM, N], F32)
    G_FF = dscr("G_FF", [HF, N], F32)
    QF = dscr("QF", [FW, N], BF16)
    KF = dscr("KF", [FW, N], BF16)
    VF = dscr("VF", [N, FW], BF16)
    QR = dscr("QR", [RW, N], BF16)
    KR = dscr("KR", [RW, N], BF16)
    VR = dscr("VR", [N, RW], BF16)
    GR = dscr("GR", [N, RW], BF16)
    GATES = dscr("GATES", [3 * D, N], BF16)
    YM = dscr("YM", [MW, N], BF16)
    YF = dscr("YF", [FW, N], BF16)
    YR = dscr("YR", [RW, N], BF16)

    P = Prog(nc)
    glob = ExitStack()
    glob.enter_context(nc.allow_non_contiguous_dma(reason="tiny strided gate/bias loads"))

    uid = [0]

    def sb(es, name, shape, dt):
        uid[0] += 1
        return es.enter_context(nc.sbuf_tensor("s%d_%s" % (uid[0], name), list(shape), dt))

    banks = [glob.enter_context(nc.psum_tensor("pb%d" % i, [128, 512], F32)) for i in range(8)]
    bbuf = [Buf("pb%d" % i) for i in range(8)]
    ring = [0]
    ring_n = [5]

    def ps():
        i = ring[0] % ring_n[0]
        ring[0] += 1
        return banks[i], bbuf[i]

    ident_f = sb(glob, "ident_f", [128, 128], F32)
    ident_b = sb(glob, "ident_b", [128, 128], BF16)
    ones_b = sb(glob, "ones_b", [128, 128], BF16)
    cst = sb(glob, "cst", [128, 4], F32)
    maskT = sb(glob, "maskT", [128, 128], F32)
    sel = sb(glob, "sel", [16, 16, 128], F32)
    MOD = sb(glob, "MOD", [128, 9 * DC, NB], F32)
    SCE = sb(glob, "SCE", [128, 3, DC, NB], F32)
    GTE = sb(glob, "GTE", [128, 3, DC, NB], F32)
    badap = sb(glob, "badap", [128, L, 9 * DC], F32)
    nwp = sb(glob, "nwp", [128, L, 6, DC], F32)
    binp = sb(glob, "binp", [128, L, cfg.NCF], F32)
    bsi = sb(glob, "bsi", [HM, L], F32)
    bsf = sb(glob, "bsf", [HM, L], F32)
    bsff = sb(glob, "bsff", [HF, L], F32)
    cwp = sb(glob, "cwp", [128, L, 2 * MW // 128, 4], F32)
    cbp = sb(glob, "cbp", [128, L, 2 * MW // 128], F32)
    condT = sb(glob, "condT", [128, DC, NB], BF16)
    c32 = sb(glob, "c32", [128, DC, NB], F32)
    b_const = Buf("const")
    b_mod = Buf("mod")
    for dst, src in ((ident_f, ident_in), (maskT, maskT_in), (sel, sel_in), (badap, bada_pc), (nwp, nw_pc),
                     (binp, bin_pc), (bsi, bsm_i), (bsf, bsm_f), (bsff, bsm_ff), (cwp, convw_pc), (cbp, convb_pc),
                     (c32, c_pc)):
        P.dma("sp", dst[:], src, writes=[b_const])
    P.op("dve", lambda e: e.tensor_copy(out=ident_b[:], in_=ident_f[:]), reads=[b_const], writes=[b_const])
    P.op("dve", lambda e: e.memset(ones_b[:], 1.0), writes=[b_const])
    P.op("dve", lambda e: e.memset(cst[:, 0:1], EPS * D), writes=[b_const])
    P.op("dve", lambda e: e.memset(cst[:, 1:2], EPS), writes=[b_const])
    P.op("dve", lambda e: e.memset(cst[:, 2:3], 1.0), writes=[b_const])
    P.op("act", lambda e: e.activation(out=condT[:], in_=c32[:], func=AF.Silu), reads=[b_const], writes=[b_const])

    b_XT = [[Buf("XT%d_%d" % (i, c)) for c in range(DC)] for i in range(NT)]
    b_scr = Buf("scr")
    b_y = Buf("ybranch")
    b_out = Buf("out")

    def tok(tt):
        return slice(tt * T, (tt + 1) * T)

    with ExitStack() as es:
        xin = [sb(es, "xin%d" % i, [128, T // 128, D], F32) for i in range(2)]
        xo = [sb(es, "xo%d" % i, [128, T], F32) for i in range(3)]
        b_xin = [Buf() for _ in range(2)]
        b_xo = [Buf() for _ in range(3)]
        k = 0
        for tt in range(NT):
            s = tt % 2
            P.dma("sp", xin[s][:], x_in[tok(tt), :].rearrange("(c p) d -> p c d", p=128), writes=[b_xin[s]])
            for dc in range(DC):
                pt, pb = ps()
                for tc in range(T // 128):
                    P.op("pe", lambda e, pt=pt, s=s, tc=tc, dc=dc: e.matmul(
                        pt[:, tc * 128:(tc + 1) * 128], lhsT=xin[s][:, tc, dc * 128:(dc + 1) * 128], rhs=ident_f[:],
                        start=True, stop=True), reads=[b_xin[s], b_const], writes=[pb], inc=(tc == T // 128 - 1))
                o = k % 3
                eng = "act" if k % 2 == 0 else "dve"
                if eng == "act":
                    P.op("act", lambda e, o=o, pt=pt: e.copy(out=xo[o][:], in_=pt[:]), reads=[pb], writes=[b_xo[o]])
                else:
                    P.op("dve", lambda e, o=o, pt=pt: e.tensor_copy(out=xo[o][:], in_=pt[:]), reads=[pb], writes=[b_xo[o]])
                P.dma("sp", XT[dc * 128:(dc + 1) * 128, tok(tt)], xo[o][:], reads=[b_xo[o]], writes=[b_XT[tt][dc]])
                k += 1
        P.barrier()

    class G:
        pass

    def alloc_gemm(es, kmax):
        g = G()
        g.xy = sb(es, "xy", [128, DC, T], F32)
        g.hT = sb(es, "hT", [128, DC, T], BF16)
        g.wb = [sb(es, "wb%d" % i, [128, kmax, 256], BF16) for i in range(3)]
        g.b_wb = [[Buf("wb%d_%d" % (i, p_)) for p_ in range(8)] for i in range(3)]
        g.wi = [0]
        g.sq = [sb(es, "sq%d" % i, [128, T], BF16) for i in range(2)]
        g.b_sq = [Buf() for _ in range(2)]
        g.tmp = [sb(es, "tmp%d" % i, [128, T], F32) for i in range(3)]
        g.b_tmp = [Buf() for _ in range(3)]
        g.ti = [0]
        g.rstd = sb(es, "rstd", [128, T], F32)
        g.b_rstd = Buf()
        g.xr = [sb(es, "xr%d" % i, [128, T], F32) for i in range(3)]
        g.b_xr = [Buf() for _ in range(3)]
        g.xo = [sb(es, "xo%d" % i, [128, T], F32) for i in range(3)]
        g.b_xo = [Buf() for _ in range(3)]
        g.ob = [sb(es, "ob%d" % i, [128, T], BF16) for i in range(4)]
        g.b_ob = [Buf() for _ in range(4)]
        g.oi = [0]
        g.b_xy = Buf("xy")
        g.b_hT = Buf("hT")
        return g

    def wslot(g):
        s = g.wi[0] % 3
        g.wi[0] += 1
        return s

    def tmpslot(g):
        s = g.ti[0] % 3
        g.ti[0] += 1
        return s

    def obslot(g):
        s = g.oi[0] % 4
        g.oi[0] += 1
        return s

    def load_w(g, s, W2d, k0, kc_n, c0, ncols, kdst=0):
        for a in range(0, kc_n, 8):
            n = min(8, kc_n - a)
            src = W2d[(k0 + a) * 128:(k0 + a + n) * 128, c0:c0 + ncols].rearrange("(kc p) n -> p kc n", p=128)
            P.dma("pool", g.wb[s][:, kdst + a:kdst + a + n, 0:ncols], src, writes=[g.b_wb[s][(kdst + a) // 8]])

    def sumsq_rstd(g, src_fn, nchunks, rbuf):
        pt, pb = ps()
        for c in range(nchunks):
            q = c % 2
            P.op("act", lambda e, q=q, c=c: e.activation(out=g.sq[q][:], in_=src_fn(c), func=AF.Square),
                 reads=[rbuf], writes=[g.b_sq[q]])
            P.op("pe", lambda e, q=q, c=c, pt=pt: e.matmul(pt[:], lhsT=ones_b[:], rhs=g.sq[q][:], start=(c == 0),
                                                         stop=(c == nchunks - 1)),
                 reads=[g.b_sq[q], b_const], writes=[pb], inc=True)
        P.op("act", lambda e, pt=pt: e.activation(out=g.rstd[:], in_=pt[:], func=AF.Sqrt, bias=cst[:, 0:1]),
             reads=[pb, b_const], writes=[g.b_rstd])
        P.op("dve", lambda e: e.reciprocal(out=g.rstd[:], in_=g.rstd[:]), reads=[g.b_rstd], writes=[g.b_rstd])

    def load_norm(g, tt, j):
        b = tt // TPS
        P.dma("sp", g.xy[:], XT[:, tok(tt)].rearrange("(c p) t -> p c t", p=128), reads=b_XT[tt], writes=[g.b_xy])
        import os
        LNP = os.environ.get('LN_PARTS', 'sa')
        if 's' in LNP:
            sumsq_rstd(g, lambda c: g.xy[:, c, :], DC, g.b_xy)
        for c in range(DC if 'a' in LNP else 0):
            s = tmpslot(g)
            P.op("dve", lambda e, s=s, c=c: e.tensor_tensor(out=g.tmp[s][:], in0=g.xy[:, c, :], in1=g.rstd[:], op=ALU.mult),
                 reads=[g.b_xy, g.b_rstd], writes=[g.b_tmp[s]])
            P.op("act", lambda e, s=s, c=c: e.activation(out=g.hT[:, c, :], in_=g.tmp[s][:], func=AF.Identity,
                                                          scale=SCE[:, j, c, b:b + 1], bias=MOD[:, (3 * j) * DC + c, b:b + 1]),
                 reads=[g.b_tmp[s], b_mod], writes=[g.b_hT])

    def post_res(g, tt, j):
        b = tt // TPS
        sumsq_rstd(g, lambda c: g.xy[:, c, :], DC, g.b_xy)
        for c in range(DC):
            r = c % 3
            P.dma("sp", g.xr[r][:], XT[c * 128:(c + 1) * 128, tok(tt)], reads=[b_XT[tt][c]], writes=[g.b_xr[r]])
            s = tmpslot(g)
            P.op("dve", lambda e, s=s, c=c: e.tensor_tensor(out=g.tmp[s][:], in0=g.xy[:, c, :], in1=g.rstd[:], op=ALU.mult),
                 reads=[g.b_xy, g.b_rstd], writes=[g.b_tmp[s]])
            P.op("dve", lambda e, s=s, c=c, r=r: e.scalar_tensor_tensor(
                out=g.xo[r][:], in0=g.tmp[s][:], scalar=GTE[:, j, c, b:b + 1], in1=g.xr[r][:], op0=ALU.mult, op1=ALU.add),
                reads=[g.b_tmp[s], g.b_xr[r], b_mod], writes=[g.b_xo[r]])
            P.dma("sp", XT[c * 128:(c + 1) * 128, tok(tt)], g.xo[r][:], reads=[g.b_xo[r]], writes=[b_XT[tt][c]])

    def gemm_F(g, W2d, c0, ncols, KCn, rhs_fn, rbufs, epi, M=128):
        for t0 in range(0, ncols, 256):
            nw = min(256, ncols - t0)
            s = wslot(g)
            load_w(g, s, W2d, 0, KCn, c0 + t0, nw)
            for jn in range(0, nw, 128):
                m = min(M, nw - jn)
                pt, pb = ps()
                for kc in range(KCn):
                    P.op("pe", lambda e, pt=pt, s=s, kc=kc, jn=jn, m=m: e.matmul(
                        pt[0:m, :], lhsT=g.wb[s][:, kc, jn:jn + m], rhs=rhs_fn(kc), start=(kc == 0), stop=(kc == KCn - 1)),
                        reads=[g.b_wb[s][kc // 8]] + rbufs, writes=[pb], inc=(kc == KCn - 1))
                epi((t0 + jn) // 128, pt, pb)

    def compute_mod(l, g):
        pt, pb = banks[5], bbuf[5]
        ncols = 9 * D
        for t0 in range(0, ncols, 256):
            s = wslot(g)
            load_w(g, s, w_ada[l], 0, DC, t0, 256)
            for jn in (0, 128):
                ci = (t0 + jn) // 128
                for kc in range(DC):
                    P.op("pe", lambda e, s=s, kc=kc, jn=jn, ci=ci: e.matmul(
                        pt[:, ci * NB:(ci + 1) * NB], lhsT=g.wb[s][:, kc, jn:jn + 128], rhs=condT[:, kc, :],
                        start=(kc == 0), stop=(kc == DC - 1)),
                        reads=[g.b_wb[s][kc // 8], b_const], writes=[pb], inc=(kc == DC - 1 and jn == 128))
        pv = pt[:, 0:9 * DC * NB].rearrange("p (c b) -> p c b", b=NB)
        for b in range(NB):
            P.op("dve", lambda e, b=b: e.tensor_tensor(out=MOD[:, :, b], in0=pv[:, :, b], in1=badap[:, l, :], op=ALU.add),
                 reads=[pb, b_const], writes=[b_mod])
        sqD = float(np.sqrt(D))
        for j in range(3):
            step = 1.0 if j == 1 else 0.5
            for b in range(NB):
                P.op("dve", lambda e, j=j, b=b: e.scalar_tensor_tensor(
                    out=SCE[:, j, :, b], in0=MOD[:, (3 * j + 1) * DC:(3 * j + 2) * DC, b], scalar=1.0,
                    in1=nwp[:, l, 2 * j, :], op0=ALU.add, op1=ALU.mult), reads=[b_mod, b_const], writes=[b_mod])
                P.op("dve", lambda e, j=j, b=b: e.tensor_scalar(
                    out=SCE[:, j, :, b], in0=SCE[:, j, :, b], scalar1=sqD, scalar2=None, op0=ALU.mult),
                    reads=[b_mod], writes=[b_mod])
                P.op("dve", lambda e, j=j, b=b, step=step: e.scalar_tensor_tensor(
                    out=GTE[:, j, :, b], in0=MOD[:, (3 * j + 2) * DC:(3 * j + 3) * DC, b], scalar=step * sqD,
                    in1=nwp[:, l, 2 * j + 1, :], op0=ALU.mult, op1=ALU.mult), reads=[b_mod, b_const], writes=[b_mod])

    def ffn(g, aT, b_aT, l, j, Wg, Wu, Wd):
        import os
        PARTS = os.environ.get('FFN_PARTS', 'ngdp')
        for tt in range(int(os.environ.get('FFN_NT', NT))):
            if 'n' in PARTS:
                load_norm(g, tt, j)
            for ft in range(DFF // 256 if 'g' in PARTS else 0):
                s = wslot(g)
                load_w(g, s, Wg[l], 0, DC, ft * 256, 256, kdst=0)
                load_w(g, s, Wu[l], 0, DC, ft * 256, 256, kdst=DC)
                for jn in (0, 128):
                    pa, ba = ps()
                    pu, bu = ps()
                    for (pt, pb, ko) in ((pa, ba, 0), (pu, bu, DC)):
                        for kc in range(DC):
                            P.op("pe", lambda e, pt=pt, s=s, kc=kc, ko=ko, jn=jn: e.matmul(
                                pt[:], lhsT=g.wb[s][:, ko + kc, jn:jn + 128], rhs=g.hT[:, kc, :],
                                start=(kc == 0), stop=(kc == DC - 1)),
                                reads=[g.b_wb[s][(ko + kc) // 8], g.b_hT], writes=[pb], inc=(kc == DC - 1))
                    ts_ = tmpslot(g)
                    fc = ft * 2 + jn // 128
                    P.op("act", lambda e, ts_=ts_, pa=pa: e.activation(out=g.tmp[ts_][:], in_=pa[:], func=AF.Silu),
                         reads=[ba], writes=[g.b_tmp[ts_]])
                    P.op("dve", lambda e, ts_=ts_, pu=pu, fc=fc: e.tensor_tensor(out=aT[:, fc, :], in0=g.tmp[ts_][:], in1=pu[:],
                                                                                op=ALU.mult),
                         reads=[g.b_tmp[ts_], bu], writes=[b_aT])

            kk = [0]

            def epi(ci, pt, pb):
                if kk[0] % 2 == 0:
                    P.op("act", lambda e: e.copy(out=g.xy[:, ci, :], in_=pt[:]), reads=[pb], writes=[g.b_xy])
                else:
                    P.op("dve", lambda e: e.tensor_copy(out=g.xy[:, ci, :], in_=pt[:]), reads=[pb], writes=[g.b_xy])
                kk[0] += 1
            if 'd' in PARTS:
                gemm_F(g, Wd[l], 0, D, FC, lambda kc: aT[:, kc, :], [b_aT], epi)
            if 'p' in PARTS:
                post_res(g, tt, j)

    def mixer_in(g, l, zb, b_zb, halo, b_halo, brow, b_brow, wsm, b_wsm, osm, b_osm, cs, sn):
        W = w_in[l]
        for i, nm in enumerate(cfg.krange):
            c0, wdt = cfg.col[nm]
            P.dma("pool", brow[0:1, i, 0:wdt], b_in[l:l + 1, c0:c0 + wdt], writes=[b_brow])
        for i, (nm, h) in enumerate((("m_i", HM), ("m_f", HM), ("f_f", HF))):
            c0, wdt = cfg.col[nm]
            for k0 in range(0, DC, 8):
                k1 = min(DC, k0 + 8)
                P.dma("pool", wsm[:, k0:k1, i, 0:wdt], W[k0 * 128:k1 * 128, c0:c0 + wdt].rearrange("(kc p) n -> p kc n", p=128),
                      writes=[b_wsm])
        for tt in range(NT):
            b = tt // TPS
            first = (tt % TPS == 0)
            load_norm(g, tt, 1)
            hfn = lambda kc: g.hT[:, kc, :]
            c0, wdt = cfg.col["m_qk"]

            def epi_qk(ci, pt, pb):
                q = ci % 2
                if first:
                    P.op("dve", lambda e: e.memset(zb[q][:, 0:3], 0.0), writes=[b_zb[q]])
                else:
                    P.op("dve", lambda e: e.tensor_copy(out=zb[q][:, 0:3], in_=halo[:, ci, :]), reads=[b_halo], writes=[b_zb[q]])
                P.op("act", lambda e: e.activation(out=zb[q][:, 3:3 + T], in_=pt[:], func=AF.Identity,
                                                   bias=binp[:, l, cfg.foff["m_qk"] + ci:cfg.foff["m_qk"] + ci + 1]),
                     reads=[pb, b_const], writes=[b_zb[q]])
                P.op("dve", lambda e: e.tensor_copy(out=halo[:, ci, :], in_=zb[q][:, T:T + 3]), reads=[b_zb[q]], writes=[b_halo])
                s = tmpslot(g)
                P.op("dve", lambda e: e.tensor_scalar(out=g.tmp[s][:], in0=zb[q][:, 0:T], scalar1=cwp[:, l, ci, 0:1],
                                                      scalar2=None, op0=ALU.mult), reads=[b_zb[q], b_const], writes=[g.b_tmp[s]])
                for jj in (1, 2, 3):
                    P.op("dve", lambda e, jj=jj: e.scalar_tensor_tensor(
                        out=g.tmp[s][:], in0=zb[q][:, jj:jj + T], scalar=cwp[:, l, ci, jj:jj + 1], in1=g.tmp[s][:],
                        op0=ALU.mult, op1=ALU.add), reads=[b_zb[q], b_const, g.b_tmp[s]], writes=[g.b_tmp[s]])
                o = obslot(g)
                P.op("act", lambda e: e.activation(out=g.ob[o][:], in_=g.tmp[s][:], func=AF.Silu, bias=cbp[:, l, ci:ci + 1]),
                     reads=[g.b_tmp[s], b_const], writes=[g.b_ob[o]])
                P.dma("sp", QKM[ci * 128:(ci + 1) * 128, tok(tt)], g.ob[o][:], reads=[g.b_ob[o]], nowaw=[b_scr])
            gemm_F(g, W, c0, wdt, DC, hfn, [g.b_hT], epi_qk)

            def mk_epi(nm, dst, func, rowoff=0):
                def epi(ci, pt, pb):
                    o = obslot(g)
                    P.op("act", lambda e: e.activation(out=g.ob[o][:], in_=pt[:], func=func,
                                                       bias=binp[:, l, cfg.foff[nm] + ci:cfg.foff[nm] + ci + 1]),
                         reads=[pb, b_const], writes=[g.b_ob[o]])
                    P.dma("sp", dst[rowoff + ci * 128:rowoff + (ci + 1) * 128, tok(tt)], g.ob[o][:], reads=[g.b_ob[o]],
                          nowaw=[b_scr])
                return epi
            for nm, dst, func, ro in (("f_q", QF, AF.Identity, 0), ("f_k", KF, AF.Identity, 0),
                                      ("g_m", GATES, AF.Sigmoid, 0), ("g_f", GATES, AF.Sigmoid, D),
                                      ("g_r", GATES, AF.Sigmoid, 2 * D)):
                c0, wdt = cfg.col[nm]
                gemm_F(g, W, c0, wdt, DC, hfn, [g.b_hT], mk_epi(nm, dst, func, ro))

            pos = slice((tt % TPS) * T, (tt % TPS + 1) * T)
            for nm, dst in (("r_q", QR), ("r_k", KR)):
                c0, wdt = cfg.col[nm]
                st = {}

                def epi_rot(ci, pt, pb, nm=nm, dst=dst, st=st):
                    s = tmpslot(g)
                    P.op("act", lambda e: e.activation(out=g.tmp[s][:], in_=pt[:], func=AF.Identity,
                                                       bias=binp[:, l, cfg.foff[nm] + ci:cfg.foff[nm] + ci + 1]),
                         reads=[pb, b_const], writes=[g.b_tmp[s]])
                    if ci % 2 == 0:
                        st["a"] = s
                        return
                    s1, s2 = st["a"], s
                    for which in (0, 1):
                        s3 = tmpslot(g)
                        o = obslot(g)
                        ta, tb = (cs, sn) if which == 0 else (sn, cs)
                        P.op("dve", lambda e, ta=ta, s3=s3: e.tensor_tensor(out=g.tmp[s3][:], in0=g.tmp[s1][:], in1=ta[:, pos],
                                                                               op=ALU.mult),
                             reads=[g.b_tmp[s1], b_const], writes=[g.b_tmp[s3]])
                        P.op("dve", lambda e, tb=tb, o=o: e.tensor_tensor(out=g.ob[o][:], in0=g.tmp[s2][:], in1=tb[:, pos],
                                                                          op=ALU.mult),
                             reads=[g.b_tmp[s2], b_const], writes=[g.b_ob[o]])
                        P.op("dve", lambda e, o=o, s3=s3, which=which: e.tensor_tensor(
                            out=g.ob[o][:], in0=g.tmp[s3][:], in1=g.ob[o][:],
                            op=(ALU.subtract if which == 0 else ALU.add)),
                            reads=[g.b_tmp[s3], g.b_ob[o]], writes=[g.b_ob[o]])
                        cio = ci - 1 + which
                        P.dma("sp", dst[cio * 128:(cio + 1) * 128, tok(tt)], g.ob[o][:], reads=[g.b_ob[o]], nowaw=[b_scr])
                gemm_F(g, W, c0, wdt, DC, hfn, [g.b_hT], epi_rot)

            for i, (nm, hn, dst, bias) in enumerate((("m_i", HM, G_I, bsi), ("m_f", HM, G_F, bsf), ("f_f", HF, G_FF, bsff))):
                pt, pb = ps()
                for kc in range(DC):
                    P.op("pe", lambda e, pt=pt, kc=kc, i=i, hn=hn: e.matmul(
                        pt[0:hn, :], lhsT=wsm[:, kc, i, 0:hn], rhs=g.hT[:, kc, :], start=(kc == 0), stop=(kc == DC - 1)),
                        reads=[b_wsm, g.b_hT], writes=[pb], inc=(kc == DC - 1))
                q = i
                P.op("act", lambda e, pt=pt, hn=hn, bias=bias, q=q: e.activation(
                    out=osm[q][0:hn, :], in_=pt[0:hn, :], func=AF.Identity, bias=bias[0:hn, l:l + 1]),
                    reads=[pb, b_const], writes=[b_osm[q]])
                P.dma("sp", dst[0:hn, tok(tt)], osm[q][0:hn, :], reads=[b_osm[q]], nowaw=[b_scr])

            for i, (nm, dst, func) in enumerate((("m_v", VM, AF.Identity), ("m_o", OM, AF.Sigmoid), ("f_v", VF, AF.Identity),
                                                 ("r_v", VR, AF.Identity), ("r_g", GR, AF.Silu))):
                c0, wdt = cfg.col[nm]
                for t0 in range(0, wdt, 256):
                    s = wslot(g)
                    load_w(g, s, W, 0, DC, c0 + t0, 256)
                    for tc in range(T // 128):
                        pt, pb = ps()
                        for kc in range(DC):
                            P.op("pe", lambda e, pt=pt, s=s, kc=kc, tc=tc: e.matmul(
                                pt[:, 0:256], lhsT=g.hT[:, kc, tc * 128:(tc + 1) * 128], rhs=g.wb[s][:, kc, 0:256],
                                start=(kc == 0), stop=False), reads=[g.b_wb[s][kc // 8], g.b_hT], writes=[pb], inc=False)
                        P.op("pe", lambda e, pt=pt, i=i, t0=t0: e.matmul(
                            pt[:, 0:256], lhsT=ones_b[0:1, :], rhs=brow[0:1, i, t0:t0 + 256], start=False, stop=True),
                            reads=[b_brow, b_const], writes=[pb], inc=True)
                        o = obslot(g)
                        P.op("act", lambda e, o=o, pt=pt, func=func: e.activation(out=g.ob[o][:, 0:256], in_=pt[:, 0:256], func=func),
                             reads=[pb], writes=[g.b_ob[o]])
                        r0 = tt * T + tc * 128
                        P.dma("sp", dst[r0:r0 + 128, t0:t0 + 256], g.ob[o][:, 0:256], reads=[g.b_ob[o]], nowaw=[b_scr])

    def mixer_out(g, l, yb, b_yb, macc, b_macc, gt, b_gt):
        for tt in range(NT):
            for i, (Y, Wd_) in enumerate(((YM, MW), (YF, FW), (YR, RW))):
                P.dma("sp", yb[i][:, 0:Wd_ // 128, :], Y[:, tok(tt)].rearrange("(c p) t -> p c t", p=128),
                      reads=[b_y], writes=[b_yb[i]])
            for i, (Wb, Wd_) in enumerate(((w_bm, MW), (w_bf, FW), (w_br, RW))):
                def epi(ci, pt, pb, i=i):
                    q = (i * DC + ci) % 6
                    P.dma("sp", gt[q][:], GATES[i * D + ci * 128:i * D + (ci + 1) * 128, tok(tt)], reads=[b_scr], writes=[b_gt[q]])
                    if i == 0:
                        P.op("dve", lambda e: e.tensor_tensor(out=macc[:, ci, :], in0=pt[:], in1=gt[q][:], op=ALU.mult),
                             reads=[pb, b_gt[q]], writes=[b_macc])
                    else:
                        s = tmpslot(g)
                        P.op("dve", lambda e: e.tensor_tensor(out=g.tmp[s][:], in0=pt[:], in1=gt[q][:], op=ALU.mult),
                             reads=[pb, b_gt[q]], writes=[g.b_tmp[s]])
                        if i == 1:
                            P.op("dve", lambda e: e.tensor_tensor(out=macc[:, ci, :], in0=macc[:, ci, :], in1=g.tmp[s][:], op=ALU.add),
                                 reads=[g.b_tmp[s], b_macc], writes=[b_macc])
                        else:
                            P.op("dve", lambda e: e.tensor_tensor(out=g.hT[:, ci, :], in0=macc[:, ci, :], in1=g.tmp[s][:], op=ALU.add),
                                 reads=[g.b_tmp[s], b_macc], writes=[g.b_hT])
                gemm_F(g, Wb[l], 0, D, Wd_ // 128, lambda kc, i=i: yb[i][:, kc, :], [b_yb[i]], epi)
            kk = [0]

            def epi_o(ci, pt, pb):
                if kk[0] % 2 == 0:
                    P.op("act", lambda e: e.copy(out=g.xy[:, ci, :], in_=pt[:]), reads=[pb], writes=[g.b_xy])
                else:
                    P.op("dve", lambda e: e.tensor_copy(out=g.xy[:, ci, :], in_=pt[:]), reads=[pb], writes=[g.b_xy])
                kk[0] += 1
            gemm_F(g, w_out[l], 0, D, DC, lambda kc: g.hT[:, kc, :], [g.b_hT], epi_o)
            post_res(g, tt, 1)

    def seq_mixers(l):
        RM, RF = NB * HM, NB * HF
        with ExitStack() as es:
            GMN = sb(es, "GMN", [RM, S], F32)
            FS = sb(es, "FS", [RF, S], F32)
            MS = sb(es, "MS", [128, CPS, 4, RM], F32)
            DECB = sb(es, "DECB", [128, RM, CPS], F32)
            FNT = sb(es, "FNT", [128, CPS, RF], F32)
            br = Buf("rows")
            bt = Buf("tokmaj")
            with ExitStack() as es1:
                UU = sb(es1, "UU", [RM, S], F32)
                GM = sb(es1, "GM", [RM, S], F32)
                BN = sb(es1, "BN", [RM, S], F32)
                RA = sb(es1, "RA", [RM, S], F32)
                RK = sb(es1, "RK", [RM, S], F32)
                GC = sb(es1, "GC", [RM, CPS], F32)
                FFp = sb(es1, "FFp", [RF, S], F32)
                FN = sb(es1, "FN", [RF, S], F32)
                onesr = sb(es1, "onesr", [max(RM, RF), S], F32)
                for b in range(NB):
                    sq_ = slice(b * S, (b + 1) * S)
                    P.dma("sp", UU[b * HM:(b + 1) * HM, :], G_I[:, sq_], reads=[b_scr], writes=[br])
                    P.dma("sp", GM[b * HM:(b + 1) * HM, :], G_F[:, sq_], reads=[b_scr], writes=[br])
                    P.dma("sp", FFp[b * HF:(b + 1) * HF, :], G_FF[:, sq_], reads=[b_scr], writes=[br])
                P.op("dve", lambda e: e.memset(onesr[:], 1.0), writes=[br])
                for tl in (GM, FFp):
                    P.op("act", lambda e, tl=tl: e.activation(out=tl[:], in_=tl[:], func=AF.Exp, scale=-1.0), reads=[br], writes=[br])
                    P.op("act", lambda e, tl=tl: e.activation(out=tl[:], in_=tl[:], func=AF.Ln, bias=cst[0:tl.shape[0], 2:3]), reads=[br], writes=[br])
                P.op("dve", lambda e: e.tensor_tensor_scan(out=BN[:], data0=onesr[0:RM, :], data1=GM[:], initial=0.0,
                                                           op0=ALU.mult, op1=ALU.add), reads=[br], writes=[br])
                P.op("dve", lambda e: e.tensor_tensor_scan(out=FN[:], data0=onesr[0:RF, :], data1=FFp[:], initial=0.0,
                                                           op0=ALU.mult, op1=ALU.add), reads=[br], writes=[br])
                P.op("dve", lambda e: e.tensor_tensor(out=UU[:], in0=UU[:], in1=BN[:], op=ALU.add), reads=[br], writes=[br])
                P.op("dve", lambda e: e.tensor_tensor_scan(out=GM[:], data0=UU[:], data1=UU[:], initial=0.0,
                                                           op0=ALU.max, op1=ALU.max), reads=[br], writes=[br])
                P.op("dve", lambda e: e.tensor_scalar(out=GMN[:], in0=GM[:], scalar1=-1.0, scalar2=None, op0=ALU.mult), reads=[br], writes=[br])
                P.op("dve", lambda e: e.tensor_scalar(out=FS[:], in0=FN[:], scalar1=-1.0, scalar2=None, op0=ALU.mult),
                     reads=[br], writes=[br])
                GMv = GM[:].rearrange("p (c l) -> p c l", l=128)
                P.op("dve", lambda e: e.memset(GC[:, 0:1], 0.0), reads=[br], writes=[br])
                P.op("dve", lambda e: e.tensor_copy(out=GC[:, 1:CPS], in_=GMv[:, 0:CPS - 1, 127]), reads=[br], writes=[br])
                P.op("dve", lambda e: e.tensor_tensor(out=BN[:], in0=BN[:], in1=GM[:], op=ALU.subtract), reads=[br], writes=[br])
                P.op("act", lambda e: e.activation(out=BN[:], in_=BN[:], func=AF.Exp), reads=[br], writes=[br])
                for c in range(CPS):
                    cl = slice(c * 128, (c + 1) * 128)
                    P.op("dve", lambda e, c=c, cl=cl: e.tensor_scalar(out=RA[:, cl], in0=GMN[:, cl], scalar1=GC[:, c:c + 1], scalar2=None,
                                                                      op0=ALU.add), reads=[br], writes=[br])
                    P.op("dve", lambda e, c=c, cl=cl: e.tensor_scalar(out=RK[:, cl], in0=UU[:, cl], scalar1=GM[:, c * 128 + 127:c * 128 + 128],
                                                                      scalar2=float(np.log(16.0)), op0=ALU.subtract, op1=ALU.subtract),
                         reads=[br], writes=[br])
                P.op("act", lambda e: e.activation(out=RA[:], in_=RA[:], func=AF.Exp), reads=[br], writes=[br])
                P.op("act", lambda e: e.activation(out=RK[:], in_=RK[:], func=AF.Exp), reads=[br], writes=[br])
                pms, bms = banks[5], bbuf[5]
                pdc, bdc = banks[6], bbuf[6]
                pfn, bfn = banks[7], bbuf[7]
                nq = 4 * RM
                for c in range(CPS):
                    cl = slice(c * 128, (c + 1) * 128)
                    for qi, R_ in enumerate((UU, RA, BN, RK)):
                        last = (c == CPS - 1 and qi == 3)
                        P.op("pe", lambda e, c=c, qi=qi, R_=R_, cl=cl: e.matmul(
                            pms[:, c * nq + qi * RM:c * nq + (qi + 1) * RM], lhsT=R_[:, cl], rhs=ident_f[0:RM, 0:RM], start=True, stop=True),
                            reads=[br, b_const], writes=[bms], inc=last)
                    P.op("pe", lambda e, c=c, cl=cl: e.matmul(pfn[:, c * RF:(c + 1) * RF], lhsT=FN[:, cl], rhs=ident_f[0:RF, 0:RF],
                                                              start=True, stop=True), reads=[br, b_const], writes=[bfn], inc=(c == CPS - 1))
                RAv = RA[:].rearrange("p (c l) -> p c l", l=128)
                for h in range(RM):
                    P.op("pe", lambda e, h=h: e.matmul(pdc[:, h * CPS:(h + 1) * CPS], lhsT=sel[0:RM, h, :], rhs=RAv[:, :, 127],
                                                       start=True, stop=True), reads=[br, b_const], writes=[bdc], inc=(h == RM - 1))
                P.op("dve", lambda e: e.tensor_copy(out=MS[:].rearrange("p c q h -> p (c q h)"), in_=pms[:, 0:CPS * nq]),
                     reads=[bms], writes=[bt])
                P.op("dve", lambda e: e.tensor_copy(out=DECB[:].rearrange("p h c -> p (h c)"), in_=pdc[:, 0:RM * CPS]), reads=[bdc], writes=[bt])
                P.op("dve", lambda e: e.tensor_copy(out=FNT[:].rearrange("p c h -> p (c h)"), in_=pfn[:, 0:CPS * RF]), reads=[bfn], writes=[bt])
                P.barrier()

            with ExitStack() as es2:
                kT = [sb(es2, "fkT%d" % i, [128, S], BF16) for i in range(2)]
                qT = [sb(es2, "fqT%d" % i, [128, S], BF16) for i in range(2)]
                Vt = [sb(es2, "fV%d" % i, [128, CPS, 128], BF16) for i in range(2)]
                b_in_ = [Buf() for _ in range(2)]
                PT = [sb(es2, "fPT%d" % i, [128, T], BF16) for i in range(4)]
                b_PT = [Buf() for _ in range(4)]
                rden = sb(es2, "frden", [128, T], F32)
                b_rden = Buf()
                Fb = [sb(es2, "fFb%d" % i, [128, T], F32) for i in range(2)]
                b_Fb = [Buf() for _ in range(2)]
                ftmp = [sb(es2, "ftmp%d" % i, [128, T], F32) for i in range(4)]
                b_ftmp = [Buf() for _ in range(4)]
                fbi = 0
                yo = [sb(es2, "fyo%d" % i, [128, T], BF16) for i in range(2)]
                b_yo = [Buf() for _ in range(2)]
                pO, bO = banks[5], bbuf[5]
                pD, bD = banks[6], bbuf[6]
                scale = float(128.0 ** -0.5)
                it = 0
                pi = 0
                oi = 0
                for b in range(NB):
                    for h in range(HF):
                        s = it % 2
                        it += 1
                        bh = b * HF + h
                        sq_ = slice(b * S, (b + 1) * S)
                        P.dma("sp", kT[s][:], KF[h * 128:(h + 1) * 128, sq_], reads=[b_scr], writes=[b_in_[s]])
                        P.dma("sp", qT[s][:], QF[h * 128:(h + 1) * 128, sq_], reads=[b_scr], writes=[b_in_[s]])
                        P.dma("sp", Vt[s][:], VF[sq_, h * 128:(h + 1) * 128].rearrange("(c p) e -> p c e", p=128),
                              reads=[b_scr], writes=[b_in_[s]])
                        for gq in range(TPS):
                            nkb = 4 * gq + 4
                            pF, bF = ps()
                            P.op("pe", lambda e: e.matmul(pF[:, 0:T], lhsT=sel[0:RF, bh, :], rhs=FS[:, gq * T:(gq + 1) * T],
                                                          start=True, stop=True), reads=[br, b_const], writes=[bF], inc=True)
                            fb = fbi % 2
                            fbi += 1
                            P.op("act", lambda e: e.copy(out=Fb[fb][:], in_=pF[:, 0:T]), reads=[bF], writes=[b_Fb[fb]])
                            pend = []
                            for j in range(nkb):
                                lo = max(0, j - 4 * gq) * 128
                                q0, q1 = gq * T + lo, (gq + 1) * T
                                ncol = T - lo
                                pt, pb = ps()
                                diag = (j >= 4 * gq)
                                P.op("pe", lambda e: e.matmul(
                                    pt[:, 0:ncol], lhsT=kT[s][:, j * 128:(j + 1) * 128], rhs=qT[s][:, q0:q1], start=True, stop=(not diag)),
                                    reads=[b_in_[s]], writes=[pb], inc=(not diag))
                                if diag:
                                    P.op("pe", lambda e: e.matmul(pt[:, 0:128], lhsT=ident_f[:], rhs=maskT[:], start=False, stop=True),
                                         reads=[b_const], writes=[pb], inc=True)
                                if len(pend) >= 2:
                                    pend.pop(0)()
                                p3 = pi % 4
                                pi += 1
                                P.op("dve", lambda e: e.scalar_tensor_tensor(
                                    out=ftmp[p3][:, 0:ncol], in0=pt[:, 0:ncol], scalar=scale, in1=Fb[fb][:, lo:T], op0=ALU.mult, op1=ALU.add),
                                    reads=[pb, b_Fb[fb]], writes=[b_ftmp[p3]])
                                P.op("act", lambda e: e.activation(
                                    out=PT[p3][:, 0:ncol], in_=ftmp[p3][:, 0:ncol], func=AF.Exp, bias=FNT[:, j, bh:bh + 1]),
                                    reads=[b_ftmp[p3], bt], writes=[b_PT[p3]])
                                def pv(j=j, p3=p3, lo=lo, ncol=ncol, s=s, nkb=nkb):
                                    P.op("pe", lambda e: e.matmul(
                                        pO[:, lo:T], lhsT=Vt[s][:, j, :], rhs=PT[p3][:, 0:ncol], start=(j == 0), stop=(j == nkb - 1)),
                                        reads=[b_PT[p3], b_in_[s]], writes=[bO], inc=(j == nkb - 1))
                                    P.op("pe", lambda e: e.matmul(
                                        pD[:, lo:T], lhsT=ones_b[:], rhs=PT[p3][:, 0:ncol], start=(j == 0), stop=(j == nkb - 1)),
                                        reads=[b_PT[p3], b_const], writes=[bD], inc=(j == nkb - 1))
                                pend.append(pv)
                            for f_ in pend:
                                f_()
                            P.op("dve", lambda e: e.reciprocal(out=rden[:], in_=pD[:]), reads=[bD], writes=[b_rden])
                            o = oi % 2
                            oi += 1
                            P.op("dve", lambda e: e.tensor_tensor(out=yo[o][:], in0=pO[:], in1=rden[:], op=ALU.mult),
                                 reads=[bO, b_rden], writes=[b_yo[o]])
                            P.dma("sp", YF[h * 128:(h + 1) * 128, b * S + gq * T:b * S + (gq + 1) * T], yo[o][:],
                                  reads=[b_yo[o]], nowaw=[b_y])
                P.barrier()

            def chunk_branch(kind):
                H = HM if kind == "m" else HR
                QT_src, KT_src = (QKM, QKM) if kind == "m" else (QR, KR)
                koff = MW if kind == "m" else 0
                Vsrc = VM if kind == "m" else VR
                Gsrc = OM if kind == "m" else GR
                NWsrc = mnw_b if kind == "m" else rnw_b
                Ydst = YM if kind == "m" else YR
                VW = 257 if kind == "m" else 256
                ring_n[0] = 8
                HG = min(4, H)
                HALF = CPS // 2
                SH = HALF * 128
                with ExitStack() as es2:
                    nwb = sb(es2, "nwb", [128, H * 256], F32)
                    b_nwb = Buf()
                    P.dma("sp", nwb[:], NWsrc[:, l, :], writes=[b_nwb])
                    if kind == "r":
                        intra = sb(es2, "intra", [128, HR, 128], F32)
                        qdec = sb(es2, "qdec", [128, HR, 128], F32)
                        kdec = sb(es2, "kdec", [128, HR], F32)
                        for d_, s_ in ((intra, intra_in), (qdec, qdec_in), (kdec, kdec_in)):
                            P.dma("sp", d_[:], s_, writes=[b_nwb])
                    qT = [sb(es2, "cq%d" % h, [128, 2, SH], BF16) for h in range(HG)]
                    kT = [sb(es2, "ck%d" % h, [128, 2, SH], BF16) for h in range(HG)]
                    Va = [sb(es2, "cv%d" % h, [128, HALF, VW], BF16) for h in range(HG)]
                    Gt = [sb(es2, "cg%d" % h, [128, HALF, 256], BF16) for h in range(HG)]
                    Cst = [sb(es2, "cC%d" % h, [128, 2, VW], F32) for h in range(HG)]
                    Cbf = [sb(es2, "cCb%d" % h, [128, 2, VW], BF16) for h in range(HG)]
                    b_ld = [Buf() for _ in range(HG)]
                    b_C = [Buf() for _ in range(HG)]
                    b_Cb = [Buf() for _ in range(HG)]
                    NR = 2 * HG
                    wT = [sb(es2, "cw%d" % i, [128, 128], BF16) for i in range(NR)]
                    Dt = [sb(es2, "cD%d" % i, [128, 128], F32) for i in range(NR)]
                    qd = [sb(es2, "cqd%d" % i, [128, 2, 128], BF16) for i in range(NR)]
                    kw = [sb(es2, "ckw%d" % i, [128, 256], BF16) for i in range(NR)]
                    o1 = [sb(es2, "co1%d" % i, [128, VW], F32) for i in range(NR)]
                    na = [sb(es2, "cna%d" % i, [128, VW], F32) for i in range(NR)]
                    hh = [sb(es2, "chh%d" % i, [128, 256], F32) for i in range(NR)]
                    yb_ = [sb(es2, "cyb%d" % i, [128, 256], BF16) for i in range(NR)]
                    st6 = [sb(es2, "cst%d" % i, [128, 8], F32) for i in range(NR)]
                    mv = [sb(es2, "cmv%d" % i, [128, 4], F32) for i in range(NR)]
                    sc1 = [sb(es2, "csc%d" % i, [128, 4], F32) for i in range(NR)]
                    yT = [sb(es2, "cyT%d" % h, [128, 2, T], BF16) for h in range(HG)]
                    b_r = [Buf() for _ in range(NR)]
                    b_yT = [Buf() for _ in range(HG)]
                    ri = [0]
                    for b, hf, h0 in [(b_, hf_, h0_) for b_ in range(NB) for h0_ in range(0, H, HG) for hf_ in range(2)]:
                        if True:
                            sq_ = slice(b * S + hf * SH, b * S + (hf + 1) * SH)
                            for hi in range(HG):
                                h = h0 + hi
                                P.dma("sp", qT[hi][:], QT_src[h * 256:(h + 1) * 256, sq_].rearrange("(c p) t -> p c t", p=128),
                                      reads=[b_scr], writes=[b_ld[hi]])
                                P.dma("sp", kT[hi][:], KT_src[koff + h * 256:koff + (h + 1) * 256, sq_].rearrange("(c p) t -> p c t", p=128),
                                      reads=[b_scr], writes=[b_ld[hi]])
                                P.dma("sp", Va[hi][:, :, 0:256], Vsrc[sq_, h * 256:(h + 1) * 256].rearrange("(c p) e -> p c e", p=128),
                                      reads=[b_scr], writes=[b_ld[hi]])
                                if kind == "m":
                                    P.op("dve", lambda e: e.memset(Va[hi][:, :, 256:257], 1.0), writes=[b_ld[hi]])
                                P.dma("sp", Gt[hi][:], Gsrc[sq_, h * 256:(h + 1) * 256].rearrange("(c p) e -> p c e", p=128),
                                      reads=[b_scr], writes=[b_ld[hi]])
                                if hf == 0:
                                    P.op("dve", lambda e: e.memset(Cst[hi][:], 0.0), writes=[b_C[hi]])
                                    P.op("dve", lambda e: e.memset(Cbf[hi][:], 0.0), writes=[b_Cb[hi]])
                            for cc in range(HALF):
                                c = hf * HALF + cc
                                cl = slice(cc * 128, (cc + 1) * 128)
                                gl = slice(c * 128, (c + 1) * 128)
                                def item(hi):
                                    h = h0 + hi
                                    bh = b * H + h
                                    r = ri[0] % NR
                                    ri[0] += 1
                                    R = [b_r[r]]
                                    pS, bS = ps()
                                    for dc in (0, 1):
                                        P.op("pe", lambda e: e.matmul(
                                            pS[:, 0:128], lhsT=kT[hi][:, dc, cl], rhs=qT[hi][:, dc, cl], start=(dc == 0), stop=(dc == 1)),
                                            reads=[b_ld[hi]], writes=[bS], inc=(dc == 1))
                                        yield
                                    if kind == "m":
                                        pDm, bDm = ps()
                                        P.op("pe", lambda e: e.matmul(
                                            pDm[:, 0:128], lhsT=sel[0:RM, bh, :], rhs=GMN[:, gl], start=True, stop=False),
                                            reads=[br, b_const], writes=[bDm], inc=False)
                                        yield
                                        P.op("pe", lambda e: e.matmul(pDm[:, 0:128], lhsT=ident_f[:], rhs=maskT[:], start=False, stop=True),
                                             reads=[b_const], writes=[bDm], inc=True)
                                        yield
                                        P.op("act", lambda e: e.activation(
                                            out=Dt[r][:], in_=pDm[:, 0:128], func=AF.Exp, bias=MS[:, c, 0, bh:bh + 1]),
                                            reads=[bDm, bt], writes=R)
                                        yield
                                        P.op("dve", lambda e: e.scalar_tensor_tensor(
                                            out=wT[r][:], in0=pS[:, 0:128], scalar=0.0625, in1=Dt[r][:], op0=ALU.mult, op1=ALU.mult),
                                            reads=[bS] + R, writes=R)
                                        yield
                                    else:
                                        P.op("dve", lambda e: e.tensor_tensor(
                                            out=wT[r][:], in0=pS[:, 0:128], in1=intra[:, h, :], op=ALU.mult),
                                            reads=[bS, b_nwb], writes=R)
                                        yield
                                        for dc in (0, 1):
                                            P.op("pool", lambda e: e.tensor_tensor(
                                                out=qd[r][:, dc, :], in0=qT[hi][:, dc, cl], in1=qdec[:, h, :], op=ALU.mult),
                                                reads=[b_ld[hi], b_nwb], writes=R)
                                            yield
                                    pO_, bO_ = ps()
                                    if kind == "m":
                                        P.op("pe", lambda e: e.matmul(
                                            pO_[:, 0:VW], lhsT=wT[r][:], rhs=Va[hi][:, cc, :], start=True, stop=True),
                                            reads=R + [b_ld[hi]], writes=[bO_], inc=True)
                                        yield
                                        pI, bI = ps()
                                        for dc in (0, 1):
                                            P.op("pe", lambda e: e.matmul(
                                                pI[:, 0:VW], lhsT=qT[hi][:, dc, cl], rhs=Cbf[hi][:, dc, :], start=(dc == 0), stop=(dc == 1)),
                                                reads=[b_ld[hi], b_Cb[hi]], writes=[bI], inc=(dc == 1))
                                            yield
                                        P.op("act", lambda e: e.copy(out=o1[r][:], in_=pO_[:, 0:VW]), reads=[bO_], writes=R)
                                        yield
                                        P.op("dve", lambda e: e.scalar_tensor_tensor(
                                            out=na[r][:], in0=pI[:, 0:VW], scalar=MS[:, c, 1, bh:bh + 1], in1=o1[r][:], op0=ALU.mult, op1=ALU.add),
                                            reads=[bI, bt] + R, writes=R)
                                        yield
                                        P.op("dve", lambda e: e.tensor_scalar(
                                            out=sc1[r][:, 2:3], in0=na[r][:, 256:257], scalar1=-1.0, scalar2=None, op0=ALU.mult),
                                            reads=R, writes=R)
                                        yield
                                        P.op("dve", lambda e: e.tensor_tensor(
                                            out=sc1[r][:, 0:1], in0=na[r][:, 256:257], in1=sc1[r][:, 2:3], op=ALU.max), reads=R, writes=R)
                                        yield
                                        P.op("dve", lambda e: e.tensor_scalar(
                                            out=sc1[r][:, 0:1], in0=sc1[r][:, 0:1], scalar1=MS[:, c, 2, bh:bh + 1], scalar2=None, op0=ALU.max),
                                            reads=R + [bt], writes=R)
                                        yield
                                        P.op("dve", lambda e: e.reciprocal(out=sc1[r][:, 1:2], in_=sc1[r][:, 0:1]), reads=R, writes=R)
                                        yield
                                        P.op("dve", lambda e: e.tensor_scalar(out=hh[r][:], in0=na[r][:, 0:256], scalar1=sc1[r][:, 1:2],
                                                                              scalar2=None, op0=ALU.mult), reads=R, writes=R)
                                        yield
                                    else:
                                        P.op("pe", lambda e: e.matmul(
                                            pO_[:, 0:256], lhsT=wT[r][:], rhs=Va[hi][:, cc, :], start=True, stop=False),
                                            reads=R + [b_ld[hi]], writes=[bO_], inc=False)
                                        yield
                                        for dc in (0, 1):
                                            P.op("pe", lambda e: e.matmul(
                                                pO_[:, 0:256], lhsT=qd[r][:, dc, :], rhs=Cbf[hi][:, dc, :], start=False, stop=(dc == 1)),
                                                reads=R + [b_Cb[hi]], writes=[bO_], inc=(dc == 1))
                                            yield
                                        P.op("act", lambda e: e.copy(out=hh[r][:], in_=pO_[:, 0:256]), reads=[bO_], writes=R)
                                        yield
                                    P.op("dve", lambda e: e.bn_stats(out=st6[r][:, 0:6], in_=hh[r][:]), reads=R, writes=R)
                                    yield
                                    P.op("dve", lambda e: e.bn_aggr(out=mv[r][:, 0:2], in_=st6[r][:, 0:6]), reads=R, writes=R)
                                    yield
                                    P.op("act", lambda e: e.activation(out=mv[r][:, 2:3], in_=mv[r][:, 1:2], func=AF.Sqrt, bias=cst[:, 1:2]),
                                         reads=R + [b_const], writes=R)
                                    yield
                                    P.op("dve", lambda e: e.reciprocal(out=mv[r][:, 2:3], in_=mv[r][:, 2:3]), reads=R, writes=R)
                                    yield
                                    P.op("dve", lambda e: e.tensor_scalar(out=hh[r][:], in0=hh[r][:], scalar1=mv[r][:, 0:1],
                                                                          scalar2=mv[r][:, 2:3], op0=ALU.subtract, op1=ALU.mult),
                                         reads=R, writes=R)
                                    yield
                                    P.op("pool", lambda e: e.tensor_tensor(out=hh[r][:], in0=hh[r][:], in1=nwb[:, h * 256:(h + 1) * 256],
                                                                            op=ALU.mult), reads=R + [b_nwb], writes=R)
                                    yield
                                    P.op("dve", lambda e: e.tensor_tensor(out=yb_[r][:], in0=hh[r][:], in1=Gt[hi][:, cc, :], op=ALU.mult),
                                         reads=R + [b_ld[hi]], writes=R)
                                    yield
                                    tq = c % 4
                                    for dc in (0, 1):
                                        pY, bY = ps()
                                        P.op("pe", lambda e: e.matmul(
                                            pY[:, 0:128], lhsT=yb_[r][:, dc * 128:(dc + 1) * 128], rhs=ident_b[:], start=True, stop=True),
                                            reads=R + [b_const], writes=[bY], inc=True)
                                        yield
                                        P.op("act", lambda e: e.copy(out=yT[hi][:, dc, tq * 128:(tq + 1) * 128], in_=pY[:, 0:128]),
                                             reads=[bY], writes=[b_yT[hi]])
                                        yield
                                    if tq == 3:
                                        t0_ = b * S + (c - 3) * 128
                                        P.dma("sp", Ydst[h * 256:(h + 1) * 256, t0_:t0_ + T].rearrange("(c p) t -> p c t", p=128), yT[hi][:],
                                              reads=[b_yT[hi]], nowaw=[b_y])
                                        yield
                                    pK, bK = ps()
                                    for dc in (0, 1):
                                        P.op("pe", lambda e: e.matmul(
                                            pK[:, dc * 128:(dc + 1) * 128], lhsT=kT[hi][:, dc, cl], rhs=ident_b[:], start=True, stop=True),
                                            reads=[b_ld[hi], b_const], writes=[bK], inc=(dc == 1))
                                        yield
                                    if kind == "m":
                                        P.op("dve", lambda e: e.tensor_scalar(
                                            out=kw[r][:], in0=pK[:, 0:256], scalar1=MS[:, c, 3, bh:bh + 1], scalar2=None, op0=ALU.mult),
                                            reads=[bK, bt], writes=R)
                                        yield
                                    else:
                                        P.op("dve", lambda e: e.tensor_scalar(
                                            out=kw[r][:], in0=pK[:, 0:256], scalar1=kdec[:, h:h + 1], scalar2=None, op0=ALU.mult),
                                            reads=[bK, b_nwb], writes=R)
                                        yield
                                    for dc in (0, 1):
                                        pC, bC = ps()
                                        P.op("pe", lambda e: e.matmul(
                                            pC[:, 0:VW], lhsT=kw[r][:, dc * 128:(dc + 1) * 128], rhs=Va[hi][:, cc, :], start=True, stop=True),
                                            reads=R + [b_ld[hi]], writes=[bC], inc=True)
                                        yield
                                        if kind == "m":
                                            dscal = DECB[:, bh, c:c + 1]
                                            rd = [bt]
                                        else:
                                            dscal = float((1.0 - 2.0 ** (-5.0 - h)) ** 128)
                                            rd = []
                                        P.op("dve", lambda e: e.scalar_tensor_tensor(
                                            out=Cst[hi][:, dc, :], in0=Cst[hi][:, dc, :], scalar=dscal, in1=pC[:, 0:VW], op0=ALU.mult, op1=ALU.add),
                                            reads=[bC, b_C[hi]] + rd, writes=[b_C[hi]])
                                        yield
                                        P.op("act", lambda e: e.copy(out=Cbf[hi][:, dc, :], in_=Cst[hi][:, dc, :]),
                                             reads=[b_C[hi]], writes=[b_Cb[hi]])
                                        yield
                                gens = [item(hi_) for hi_ in range(HG)]
                                while gens:
                                    nxt = []
                                    for g_ in gens:
                                        try:
                                            next(g_)
                                            nxt.append(g_)
                                        except StopIteration:
                                            pass
                                    gens = nxt
                    P.barrier()
                    ring_n[0] = 5

            chunk_branch("r")
            chunk_branch("m")

    PH = getattr(cfg, 'phases', (1, 2, 3, 4, 5))
    for l in range(L):
        with ExitStack() as es:
            g = alloc_gemm(es, KMAX)
            aT = sb(es, "aT", [128, FC, T], BF16)
            b_aT = Buf("aT")
            compute_mod(l, g)
            if 1 in PH:
                ffn(g, aT, b_aT, l, 0, ffn_w["ffn1_gate"], ffn_w["ffn1_up"], ffn_w["ffn1_down"])
            P.barrier()
        with ExitStack() as es:
            g = alloc_gemm(es, max(DC, MW // 128, FW // 128, RW // 128))
            zb = [sb(es, "zb%d" % i, [128, T + 3], F32) for i in range(2)]
            b_zb = [Buf() for _ in range(2)]
            halo = sb(es, "halo", [128, 2 * MW // 128, 3], F32)
            b_halo = Buf()
            brow = sb(es, "brow", [1, 5, max(MW, FW, RW)], BF16)
            b_brow = Buf()
            wsm = sb(es, "wsm", [128, DC, 3, 8], BF16)
            b_wsm = Buf()
            osm = [sb(es, "osm%d" % i, [8, T], F32) for i in range(3)]
            b_osm = [Buf() for _ in range(3)]
            cs = sb(es, "cs", [128, S], F32)
            sn = sb(es, "sn", [128, S], F32)
            P.dma("sp", cs[:], cos_in, writes=[b_const])
            P.dma("sp", sn[:], sin_in, writes=[b_const])
            if 2 in PH:
                mixer_in(g, l, zb, b_zb, halo, b_halo, brow, b_brow, wsm, b_wsm, osm, b_osm, cs, sn)
            P.barrier()
        if 3 in PH:
            seq_mixers(l)
        with ExitStack() as es:
            g = alloc_gemm(es, max(DC, MW // 128, FW // 128, RW // 128))
            yb = [sb(es, "yb%d" % i, [128, max(MW, FW, RW) // 128, T], BF16) for i in range(3)]
            b_yb = [Buf() for _ in range(3)]
            macc = sb(es, "macc", [128, DC, T], F32)
            b_macc = Buf()
            gt = [sb(es, "gt%d" % i, [128, T], BF16) for i in range(6)]
            b_gt = [Buf() for _ in range(6)]
            if 4 in PH:
                mixer_out(g, l, yb, b_yb, macc, b_macc, gt, b_gt)
            P.barrier()
        with ExitStack() as es:
            g = alloc_gemm(es, KMAX)
            aT = sb(es, "aT", [128, FC, T], BF16)
            b_aT = Buf("aT")
            if 5 in PH:
                ffn(g, aT, b_aT, l, 2, ffn_w["ffn2_gate"], ffn_w["ffn2_up"], ffn_w["ffn2_down"])
            P.barrier()

    with ExitStack() as es:
        xt = [sb(es, "fxt%d" % i, [128, DC, T], F32) for i in range(2)]
        b_xt = [Buf() for _ in range(2)]
        ot = [sb(es, "fot%d" % i, [128, 512], F32) for i in range(4)]
        b_ot = [Buf() for _ in range(4)]
        k = 0
        for tt in range(NT):
            s = tt % 2
            P.dma("sp", xt[s][:], XT[:, tok(tt)].rearrange("(c p) t -> p c t", p=128), reads=b_XT[tt], writes=[b_xt[s]])
            for tc in range(T // 128):
                for d0 in range(0, DC, 4):
                    nd = min(4, DC - d0)
                    pt, pb = ps()
                    for dd in range(nd):
                        P.op("pe", lambda e, pt=pt, s=s, tc=tc, d0=d0, dd=dd: e.matmul(
                            pt[:, dd * 128:(dd + 1) * 128], lhsT=xt[s][:, d0 + dd, tc * 128:(tc + 1) * 128], rhs=ident_f[:],
                            start=True, stop=True), reads=[b_xt[s], b_const], writes=[pb], inc=(dd == nd - 1))
                    o = k % 4
                    if k % 2 == 0:
                        P.op("act", lambda e, o=o, pt=pt, nd=nd: e.copy(out=ot[o][:, 0:nd * 128], in_=pt[:, 0:nd * 128]), reads=[pb], writes=[b_ot[o]])
                    else:
                        P.op("dve", lambda e, o=o, pt=pt, nd=nd: e.tensor_copy(out=ot[o][:, 0:nd * 128], in_=pt[:, 0:nd * 128]), reads=[pb], writes=[b_ot[o]])
                    r0 = tt * T + tc * 128
                    P.dma("sp", out[r0:r0 + 128, d0 * 128:(d0 + nd) * 128], ot[o][:, 0:nd * 128], reads=[b_ot[o]], nowaw=[b_out])
                    k += 1
    P.barrier()
    glob.close()
    P.close()
    return nc, P.n_instr


def host_inputs(cfg, inp, core):
    D, S, NB, L, HM, HF, HR = cfg.D, cfg.S, cfg.NB, cfg.L, cfg.HM, cfg.HF, cfg.HR
    f = np.float32
    m = {}
    xs = inp["x"][core * NB:(core + 1) * NB]
    m["x"] = np.ascontiguousarray(xs.reshape(NB * S, D))
    cc = inp["c"][core * NB:(core + 1) * NB]
    m["c_pc"] = np.ascontiguousarray(cc.reshape(NB, cfg.DC, 128).transpose(2, 1, 0))
    return m


def shared_inputs(cfg, inp):
    D, S, NB, L, HM, HF, HR, MW = cfg.D, cfg.S, cfg.NB, cfg.L, cfg.HM, cfg.HF, cfg.HR, cfg.MW
    f = np.float32
    m = {}
    for k in ("w_ada", "ffn1_gate", "ffn1_up", "ffn1_down", "ffn2_gate", "ffn2_up", "ffn2_down", "w_in", "b_in",
              "w_branch_m", "w_branch_f", "w_branch_r", "w_out"):
        m[k] = np.ascontiguousarray(inp[k], dtype=f)

    def pc(v):
        v = np.asarray(v, dtype=f)
        sh = v.shape[:-1]
        v = v.reshape(sh + (v.shape[-1] // 128, 128))
        return np.ascontiguousarray(np.moveaxis(v, -1, 0))
    m["bada_pc"] = pc(inp["b_ada"])
    m["nw_pc"] = pc(inp["norm_w"])
    parts = []
    for nm in cfg.frange:
        c0, w = cfg.col[nm]
        parts.append(pc(inp["b_in"][:, c0:c0 + w]))
    m["bin_pc"] = np.ascontiguousarray(np.concatenate(parts, axis=2))
    for key, nm in (("bsm_i", "m_i"), ("bsm_f", "m_f"), ("bsm_ff", "f_f")):
        c0, w = cfg.col[nm]
        m[key] = np.ascontiguousarray(np.asarray(inp["b_in"][:, c0:c0 + w], dtype=f).T)
    cw = np.asarray(inp["conv_w"], dtype=f)
    m["convw_pc"] = np.ascontiguousarray(np.moveaxis(pc(cw), 2, 3))
    m["convb_pc"] = pc(inp["conv_b"])
    m["mnw_b"] = np.ascontiguousarray(np.broadcast_to(np.asarray(inp["mlstm_norm_w"], dtype=f)[None], (128, L, MW)))
    m["rnw_b"] = np.ascontiguousarray(np.broadcast_to(np.asarray(inp["ret_norm_w"], dtype=f)[None], (128, L, cfg.RW)))
    m["ident"] = np.eye(128, dtype=f)
    s_ = np.arange(128)
    m["maskT"] = np.where(s_[:, None] <= s_[None, :], 0.0, NEG).astype(f)
    sel = np.zeros((16, 16, 128), f)
    for h in range(16):
        sel[h, h, :] = 1.0
    m["sel"] = sel
    lg = np.log1p(-(2.0 ** (-5.0 - np.arange(HR, dtype=np.float64))))
    rel = (s_[None, :] - s_[:, None]).astype(np.float64)
    intra = np.where(rel[:, None, :] >= 0, np.exp(np.maximum(rel, 0)[:, None, :] * lg[None, :, None]), 0.0) * (256.0 ** -0.5)
    m["intra"] = np.ascontiguousarray(intra.astype(f))
    qd = np.exp((s_[None, :] + 1.0) * lg[:, None])
    m["qdec"] = np.ascontiguousarray(np.broadcast_to(qd[None], (128, HR, 128)).astype(f))
    kd = np.exp((127.0 - s_[:, None]) * lg[None, :]) * (256.0 ** -0.5)
    m["kdec"] = np.ascontiguousarray(kd.astype(f))
    half = 128
    inv_freq = (10000.0 ** (-np.arange(half, dtype=f) / f(half))).astype(f)
    ang = (np.arange(S, dtype=f)[None, :] * inv_freq[:, None]).astype(f)
    m["cos"] = np.cos(ang.astype(np.float64)).astype(f)
    m["sin"] = np.sin(ang.astype(np.float64)).astype(f)
    return m


_CACHE = {}


def run(cfg, inp, ncores, trace=False):
    nc, n_instr = build_program(cfg)
    sh = shared_inputs(cfg, inp)
    in_maps = []
    for c in range(ncores):
        d = dict(sh)
        d.update(host_inputs(cfg, inp, c))
        in_maps.append(d)
    res = run_bass_kernel_spmd(nc, in_maps, core_ids=list(range(ncores)), trace=trace)
    outs = [r["out"].reshape(cfg.NB, cfg.S, cfg.D) for r in res.results]
    return np.concatenate(outs, axis=0), res, n_instr


def kernel(**inputs):
    cfg = mkcfg(True)
    inp = {k: np.asarray(v) for k, v in inputs.items()}
    out, _, _ = run(cfg, inp, 8)
    return out.astype(np.float32)
```
